# Optimizing a Trainium2 kernel written in Bass

```python
import math
import jax, jax.numpy as jnp
from jax import lax
import numpy as np

D_MODEL = 2048
BATCH = 8
SEQ = 4096
DEPTH = 2

LRU_WIDTH = 1024
LRU_BLOCKS = 16
LRU_BLOCK = LRU_WIDTH // LRU_BLOCKS
CONV_WIDTH = 4
LRU_C = 8.0
RET_HEADS = 8
RET_HEAD_DIM = 128
RET_WIDTH = RET_HEADS * RET_HEAD_DIM
RET_CHUNK = 128
RET_ROT_BASE = 10000.0
DIFF_HEADS = 8
DIFF_HEAD_DIM = 64
DIFF_V_DIM = 2 * DIFF_HEAD_DIM
DIFF_WIDTH = DIFF_HEADS * DIFF_V_DIM
Q_BLOCK = 128
ROPE_THETA = 500000.0
ROPE_DIM = DIFF_HEAD_DIM // 4
N_BRANCH = 3
EPS = 1e-6
IN_SPLITS = (LRU_WIDTH, LRU_WIDTH,
             RET_WIDTH, RET_WIDTH, RET_WIDTH, RET_WIDTH,
             DIFF_WIDTH, DIFF_WIDTH, DIFF_WIDTH, DIFF_WIDTH,
             N_BRANCH * D_MODEL)
D_IN = 2 * LRU_WIDTH + 4 * RET_WIDTH + 4 * DIFF_WIDTH + N_BRANCH * D_MODEL

kernel_name = "hybrid_rglru_retention_diffattn_block"


def rms_norm(x, g):
    xf = x.astype(jnp.float32)
    y = xf * lax.rsqrt(jnp.mean(xf * xf, axis=-1, keepdims=True) + EPS)
    return (y * g.astype(jnp.float32)).astype(x.dtype)


def causal_dwconv(x, w, b):
    y = lax.conv_general_dilated(
        x, w[:, None, :].astype(x.dtype), window_strides=(1,),
        padding=[(CONV_WIDTH - 1, 0)], dimension_numbers=('NWC', 'WIO', 'NWC'),
        feature_group_count=x.shape[-1])
    return y + b.astype(x.dtype)


def rg_lru(x, w_a, b_a, w_x, b_x, lam):
    Bsz, S, W = x.shape
    xb = x.reshape(Bsz, S, LRU_BLOCKS, LRU_BLOCK)
    r = jax.nn.sigmoid(jnp.einsum('bsnd,nde->bsne', xb, w_a).reshape(Bsz, S, W) + b_a)
    i = jax.nn.sigmoid(jnp.einsum('bsnd,nde->bsne', xb, w_x).reshape(Bsz, S, W) + b_x)
    log_a = (-LRU_C * r.astype(jnp.float32)) * jax.nn.softplus(-lam.astype(jnp.float32))
    a = jnp.exp(log_a)
    mult = jnp.sqrt(-jnp.expm1(2.0 * log_a))
    u = mult * (i * x).astype(jnp.float32)

    def combine(left, right):
        a1, b1 = left
        a2, b2 = right
        return a1 * a2, a2 * b1 + b2

    _, h = lax.associative_scan(combine, (a, u), axis=1)
    return h.astype(x.dtype)


def rotate_interleaved(x, cos, sin):
    x1 = x[..., 0::2]
    x2 = x[..., 1::2]
    c = cos[:, None, :].astype(x.dtype)
    s = sin[:, None, :].astype(x.dtype)
    out = jnp.stack([x1 * c - x2 * s, x1 * s + x2 * c], axis=-1)
    return out.reshape(x.shape)


def partial_rope(x, cos, sin):
    half = ROPE_DIM // 2
    x1 = x[..., :half]
    x2 = x[..., half:ROPE_DIM]
    xp = x[..., ROPE_DIM:]
    c = cos[:, None, None, :].astype(x.dtype)
    s = sin[:, None, None, :].astype(x.dtype)
    return jnp.concatenate([x1 * c - x2 * s, x1 * s + x2 * c, xp], axis=-1)


def retention(q, k, v):
    Bsz, S, H, dk = q.shape
    dv = v.shape[-1]
    C = RET_CHUNK
    N = S // C
    f32 = jnp.float32
    log_g = jnp.log1p(-jnp.exp2(-5.0 - jnp.arange(H, dtype=f32)))
    qc = q.astype(f32).reshape(Bsz, N, C, H, dk)
    kc = (k.astype(f32) * (dk ** -0.5)).reshape(Bsz, N, C, H, dk)
    vc = v.astype(f32).reshape(Bsz, N, C, H, dv)
    idx = jnp.arange(C, dtype=f32)
    rel = idx[:, None] - idx[None, :]
    intra = jnp.where(rel[None] >= 0,
                      jnp.exp(log_g[:, None, None] * jnp.maximum(rel, 0.0)[None]), 0.0)
    scores = jnp.einsum('bnihd,bnjhd->bnhij', qc, kc) * intra
    inner = jnp.einsum('bnhij,bnjhe->bnihe', scores, vc)
    k_decay = jnp.exp(log_g[:, None] * (C - 1.0 - idx)[None, :])
    kv = jnp.einsum('bnjhd,hj,bnjhe->nbhde', kc, k_decay, vc)
    chunk_decay = jnp.exp(log_g * C)[None, :, None, None]

    def step(state, kv_n):
        return state * chunk_decay + kv_n, state

    _, prev = lax.scan(step, jnp.zeros((Bsz, H, dk, dv), f32), kv)
    q_decay = jnp.exp(log_g[:, None] * (idx + 1.0)[None, :])
    cross = jnp.einsum('bnihd,hi,nbhde->bnihe', qc, q_decay, prev)
    out = (inner + cross).reshape(Bsz, S, H, dv)
    mu = jnp.mean(out, axis=-1, keepdims=True)
    var = jnp.mean(jnp.square(out - mu), axis=-1, keepdims=True)
    return (out - mu) * lax.rsqrt(var + EPS)


def diff_attention(q, k, v, lam, sub_g, lam_init):
    Bsz, S, H, _, d = q.shape
    nb = S // Q_BLOCK
    qb = q.reshape(Bsz, nb, Q_BLOCK, H, 2, d).transpose(1, 0, 3, 4, 2, 5)
    kt = k.transpose(0, 2, 3, 1, 4)
    vt = v.transpose(0, 2, 1, 3)
    key_pos = jnp.arange(S)
    scale = d ** -0.5

    def block(args):
        qblk, start = args
        s = jnp.einsum('bhcqd,bhckd->bhcqk', qblk, kt).astype(jnp.float32) * scale
        q_pos = start + jnp.arange(Q_BLOCK)
        mask = key_pos[None, :] <= q_pos[:, None]
        s = jnp.where(mask, s, -jnp.inf)
        p = jax.nn.softmax(s, axis=-1)
        w = p[:, :, 0] - lam * p[:, :, 1]
        return jnp.einsum('bhqk,bhke->bhqe', w.astype(vt.dtype), vt)

    starts = jnp.arange(nb) * Q_BLOCK
    o = lax.map(block, (qb, starts))
    o = o.transpose(1, 0, 3, 2, 4).reshape(Bsz, S, H, 2 * d)
    of = o.astype(jnp.float32)
    of = of * lax.rsqrt(jnp.mean(of * of, axis=-1, keepdims=True) + EPS) * sub_g.astype(jnp.float32)
    return (of * (1.0 - lam_init)).astype(v.dtype)


def hybrid_layer(x, layer_idx, cos_r, sin_r, cos_d, sin_d, pre_g, post_g, w_in, conv_w, conv_b,
                 lru_wa, lru_ba, lru_wx, lru_bx, lru_lambda, diff_lam, diff_sub,
                 w_br_a, w_br_b, w_br_c, w_out):
    Bsz, S, _ = x.shape
    h = rms_norm(x, pre_g)
    proj = jnp.einsum('bsd,de->bse', h, w_in)
    offsets = tuple(int(o) for o in np.cumsum(IN_SPLITS)[:-1])
    xa, ga, qr, kr, vr, gr, qd, kd, vd, gd, gm = jnp.split(proj, offsets, axis=-1)

    xa = causal_dwconv(xa, conv_w, conv_b)
    ya = rg_lru(xa, lru_wa, lru_ba, lru_wx, lru_bx, lru_lambda) * jax.nn.silu(ga)

    qr = rotate_interleaved(qr.reshape(Bsz, S, RET_HEADS, RET_HEAD_DIM), cos_r, sin_r)
    kr = rotate_interleaved(kr.reshape(Bsz, S, RET_HEADS, RET_HEAD_DIM), cos_r, sin_r)
    vr = vr.reshape(Bsz, S, RET_HEADS, RET_HEAD_DIM)
    yb = retention(qr, kr, vr).reshape(Bsz, S, RET_WIDTH).astype(x.dtype) * jax.nn.silu(gr)

    lam_init = 0.8 - 0.6 * math.exp(-0.3 * layer_idx)
    dl = diff_lam.astype(jnp.float32)
    lam = jnp.exp(jnp.sum(dl[0] * dl[1])) - jnp.exp(jnp.sum(dl[2] * dl[3])) + lam_init
    qd = partial_rope(qd.reshape(Bsz, S, DIFF_HEADS, 2, DIFF_HEAD_DIM), cos_d, sin_d)
    kd = partial_rope(kd.reshape(Bsz, S, DIFF_HEADS, 2, DIFF_HEAD_DIM), cos_d, sin_d)
    vd = vd.reshape(Bsz, S, DIFF_HEADS, DIFF_V_DIM)
    yc = diff_attention(qd, kd, vd, lam, diff_sub, lam_init).reshape(Bsz, S, DIFF_WIDTH) * jax.nn.silu(gd)

    gm = jax.nn.sigmoid(gm.reshape(Bsz, S, N_BRANCH, D_MODEL))
    m = (gm[:, :, 0] * jnp.einsum('bsw,wd->bsd', ya, w_br_a)
         + gm[:, :, 1] * jnp.einsum('bsw,wd->bsd', yb, w_br_b)
         + gm[:, :, 2] * jnp.einsum('bsw,wd->bsd', yc, w_br_c))
    o = jnp.einsum('bsd,de->bse', m, w_out)
    return x + rms_norm(o, post_g)


def setup_inputs(seed: int = 0) -> dict:
    key = jax.random.key(seed)
    ks = jax.random.split(key, 20)
    f32 = jnp.float32
    nrm = lambda k, shape, scale: jax.random.normal(k, shape, f32) * scale
    u = jax.random.uniform(ks[10], (DEPTH, LRU_WIDTH), f32, 0.9, 0.999)
    s = u ** (1.0 / LRU_C)
    lru_lambda = jnp.log(s) - jnp.log1p(-s)
    return {
        "x": nrm(ks[0], (BATCH, SEQ, D_MODEL), 1.0),
        "pre_norm": 1.0 + nrm(ks[1], (DEPTH, D_MODEL), 0.05),
        "post_norm": 1.0 + nrm(ks[2], (DEPTH, D_MODEL), 0.05),
        "w_in": nrm(ks[3], (DEPTH, D_MODEL, D_IN), D_MODEL ** -0.5),
        "conv_w": nrm(ks[4], (DEPTH, CONV_WIDTH, LRU_WIDTH), CONV_WIDTH ** -0.5),
        "conv_b": nrm(ks[5], (DEPTH, LRU_WIDTH), 0.02),
        "lru_wa": nrm(ks[6], (DEPTH, LRU_BLOCKS, LRU_BLOCK, LRU_BLOCK), LRU_BLOCK ** -0.5),
        "lru_ba": nrm(ks[7], (DEPTH, LRU_WIDTH), 0.02),
        "lru_wx": nrm(ks[8], (DEPTH, LRU_BLOCKS, LRU_BLOCK, LRU_BLOCK), LRU_BLOCK ** -0.5),
        "lru_bx": nrm(ks[9], (DEPTH, LRU_WIDTH), 0.02),
        "lru_lambda": lru_lambda,
        "diff_lambda": nrm(ks[11], (DEPTH, 4, DIFF_HEAD_DIM), 0.1),
        "diff_subln": 1.0 + nrm(ks[12], (DEPTH, DIFF_V_DIM), 0.05),
        "w_branch_a": nrm(ks[13], (DEPTH, LRU_WIDTH, D_MODEL), LRU_WIDTH ** -0.5),
        "w_branch_b": nrm(ks[14], (DEPTH, RET_WIDTH, D_MODEL), RET_WIDTH ** -0.5),
        "w_branch_c": nrm(ks[15], (DEPTH, DIFF_WIDTH, D_MODEL), DIFF_WIDTH ** -0.5),
        "w_out": nrm(ks[16], (DEPTH, D_MODEL, D_MODEL), D_MODEL ** -0.5),
    }


def reference(x, pre_norm, post_norm, w_in, conv_w, conv_b, lru_wa, lru_ba, lru_wx, lru_bx,
              lru_lambda, diff_lambda, diff_subln, w_branch_a, w_branch_b, w_branch_c, w_out):
    S = x.shape[1]
    pos = jnp.arange(S, dtype=jnp.float32)
    ret_freq = 1.0 / (RET_ROT_BASE ** jnp.linspace(0.0, 1.0, RET_HEAD_DIM // 2, dtype=jnp.float32))
    ang_r = pos[:, None] * ret_freq[None, :]
    cos_r, sin_r = jnp.cos(ang_r), jnp.sin(ang_r)
    inv_freq = ROPE_THETA ** (-jnp.arange(0, ROPE_DIM, 2, dtype=jnp.float32) / ROPE_DIM)
    ang_d = pos[:, None] * inv_freq[None, :]
    cos_d, sin_d = jnp.cos(ang_d), jnp.sin(ang_d)
    for l in range(DEPTH):
        x = hybrid_layer(x, l, cos_r, sin_r, cos_d, sin_d, pre_norm[l], post_norm[l], w_in[l],
                         conv_w[l], conv_b[l], lru_wa[l], lru_ba[l], lru_wx[l], lru_bx[l],
                         lru_lambda[l], diff_lambda[l], diff_subln[l], w_branch_a[l],
                         w_branch_b[l], w_branch_c[l], w_out[l])
    return x
```

```python
import math
from contextlib import ExitStack
import numpy as np
import concourse.bass as bass
import concourse.mybir as mybir
from concourse.bass_utils import run_bass_kernel_spmd

F32 = mybir.dt.float32
BF16 = mybir.dt.bfloat16
AF = mybir.ActivationFunctionType
ALU = mybir.AluOpType
AX = mybir.AxisListType

D = 2048
DIN = 16384
EPS = 1e-6
KC = 16


class Res:
    __slots__ = ("w", "rd")

    def __init__(self):
        self.w = None
        self.rd = {}


class TK:
    SEM_LIMIT = 8000

    def __init__(self, nc, es):
        self.nc = nc
        self.E = {"pe": nc.tensor, "act": nc.scalar, "dve": nc.vector, "pool": nc.gpsimd, "sp": nc.sync}
        self.es = es
        self.csem = {}
        self.ccnt = {}
        self.retired = []
        self.nsem = 0
        for e in ("pe", "act", "dve", "pool"):
            self.csem[e] = es.enter_context(nc.semaphore("c_" + e))
            self.ccnt[e] = 0
        self.dsem = {
            "sp": [es.enter_context(nc.semaphore("dsp%d" % i)) for i in range(32)],
            "pool": [es.enter_context(nc.semaphore("dpl%d" % i)) for i in range(16)],
            "act": [es.enter_context(nc.semaphore("dac%d" % i)) for i in range(2)],
        }
        self.didx = {"sp": 0, "pool": 0, "act": 0}
        self.dtot = {}
        self.waited = {e: {} for e in self.E}

    def _wait(self, eng, ev):
        sem, val, _ = ev
        k = id(sem)
        if self.waited[eng].get(k, 0) >= val:
            return
        self.E[eng].wait_ge(sem, val)
        self.waited[eng][k] = val

    def op(self, eng, fn, rd=(), wr=()):
        for r in rd:
            if r.w is not None and not (r.w[2] == "pe" and eng == "pe"):
                self._wait(eng, r.w)
        for w in wr:
            if w.w is not None and not (w.w[2] == "pe" and eng == "pe"):
                self._wait(eng, w.w)
            for ev in w.rd.values():
                if ev[2] != eng:
                    self._wait(eng, ev)
        if self.ccnt[eng] >= self.SEM_LIMIT:
            self.retired.append((self.csem[eng], self.ccnt[eng], eng))
            self.nsem += 1
            self.csem[eng] = self.es.enter_context(self.nc.semaphore("c_%s_%d" % (eng, self.nsem)))
            self.ccnt[eng] = 0
        inst = fn(self.E[eng])
        self.ccnt[eng] += 1
        inst.then_inc(self.csem[eng], 1)
        ev = (self.csem[eng], self.ccnt[eng], eng)
        for r in rd:
            r.rd[eng] = ev
        for w in wr:
            w.w = ev
            w.rd = {}
        return ev

    def dma(self, q, out, in_, rd=(), wr=()):
        for r in rd:
            if r.w is not None:
                self._wait(q, r.w)
        for w in wr:
            if w.w is not None:
                self._wait(q, w.w)
            for ev in w.rd.values():
                self._wait(q, ev)
        pool = self.dsem[q]
        i = self.didx[q]
        self.didx[q] = (i + 1) % len(pool)
        sem = pool[i]
        tot = self.dtot.get(id(sem), 0)
        if tot > 0:
            self._wait(q, (sem, tot, "dma"))
        self.E[q].dma_start(out=out, in_=in_).then_inc(sem, 16)
        tot += 16
        self.dtot[id(sem)] = tot
        ev = (sem, tot, "dma")
        for r in rd:
            r.rd[("d", id(sem))] = ev
        for w in wr:
            w.w = ev
            w.rd = {}
        return ev

    def barrier(self):
        for e in self.E:
            for ev in self.retired:
                self._wait(e, ev)
            for pe, s in self.csem.items():
                if self.ccnt[pe] > 0:
                    self._wait(e, (s, self.ccnt[pe], pe))
            for q, pool in self.dsem.items():
                for s in pool:
                    t = self.dtot.get(id(s), 0)
                    if t > 0:
                        self._wait(e, (s, t, "dma"))


def build(S, depth, dbg=(), stop=99, kinds=None):
    NT = S // 128
    NB = S // 512
    PASS = min(S, 2048)
    NPASS = S // PASS
    PT = PASS // 128
    PB = PASS // 512
    nc = bass.Bass("TRN2", target_bir_lowering=False)

    def din(name, shape, dt=F32):
        return nc.dram_tensor(name, list(shape), dt, kind="ExternalInput").ap()

    def dscr(name, shape, dt=BF16):
        kind = "ExternalOutput" if name in dbg else "Internal"
        return nc.dram_tensor(name, list(shape), dt, kind=kind).ap()

    x_in = din("x", [S, D])
    w_in = din("w_in", [depth, D, DIN])
    w_br = din("w_br", [depth, 3, 1024, D])
    w_out = din("w_out", [depth, D, D])
    cols = din("cols", [depth, 128, 80])
    postg = din("postg", [depth, 128, D])
    subg = din("subg", [depth, 128, 128])
    dlam = din("dlam", [depth, 128, 256])
    lruw = din("lruw", [depth, 2, 8, 128, 128])
    ident = din("ident", [128, 128])
    tri = din("tri", [128, 128])
    ropeR = din("ropeR", [4, S, 128])
    ropeD = din("ropeD", [3, S, 256])
    intraT = din("intraT", [128, 8, 128])
    kdec = din("kdec", [128, 8])
    qdec = din("qdec", [128, 8, 512])
    y_out = nc.dram_tensor("y", [S, D], F32, kind="ExternalOutput").ap()
    xmid = dscr("xmid", [S, D], F32)
    xaT = dscr("xaT", [8, 128, S])
    gaT = dscr("gaT", [8, 128, S])
    gmT = dscr("gmT", [48, 128, S])
    QrT = dscr("QrT", [8, 128, S])
    KrT = dscr("KrT", [8, 128, S])
    Kr = dscr("Kr", [S, 1024])
    Vr = dscr("Vr", [S, 1024])
    Gr = dscr("Gr", [S, 1024])
    QdT = dscr("QdT", [8, 128, S])
    KdT = dscr("KdT", [8, 128, S])
    Vd = dscr("Vd", [S, 1024])
    Gd = dscr("Gd", [S, 1024])
    yT = dscr("yT", [3, 8, 128, S])
    mT = dscr("mT", [16, 128, S])

    with ExitStack() as es:
        tk = TK(nc, es)
        op = tk.op
        dma = tk.dma

        uid = [0]

        def sb(stack, name, shape, dt):
            uid[0] += 1
            return stack.enter_context(nc.sbuf_tensor("%s_u%d" % (name, uid[0]), list(shape), dt))

        psb = [es.enter_context(nc.psum_tensor("ps%d" % i, [128, 512], F32)) for i in range(8)]
        psr = [Res() for _ in range(8)]

        ident_f = sb(es, "ident_f", [128, 128], F32)
        ident_b = sb(es, "ident_b", [128, 128], BF16)
        tri_f = sb(es, "tri_f", [128, 128], F32)
        tri_b = sb(es, "tri_b", [128, 128], BF16)
        r_c = Res()
        dma("sp", ident_f[:], ident, wr=[r_c])
        dma("sp", tri_f[:], tri, wr=[r_c])
        op("dve", lambda e: e.tensor_copy(out=ident_b[:], in_=ident_f[:]), rd=[r_c], wr=[r_c])
        op("dve", lambda e: e.tensor_copy(out=tri_b[:], in_=tri_f[:]), rd=[r_c], wr=[r_c])
        tk.barrier()

        for l in range(depth):
            lam_init = 0.8 - 0.6 * math.exp(-0.3 * l)
            x_src = x_in if l == 0 else xmid
            x_dst = y_out if l == depth - 1 else xmid
            with ExitStack() as ls:
                colst = sb(ls, "colst", [128, 80], F32)
                s8 = sb(ls, "s8", [128, 8], F32)
                s16 = sb(ls, "s16", [128, 8], F32)
                tmp8 = sb(ls, "tmp8", [128, 8], F32)
                dl = sb(ls, "dl", [128, 256], F32)
                dprod = sb(ls, "dprod", [128, 128], F32)
                dsum = sb(ls, "dsum", [128, 4], F32)
                neglam = sb(ls, "neglam", [128, 1], F32)
                subgs = sb(ls, "subgs", [128, 128], F32)
                r_p = Res()
                dma("sp", colst[:], cols[l], wr=[r_p])
                dma("sp", dl[:], dlam[l], wr=[r_p])
                dma("sp", subgs[:], subg[l], wr=[r_p])
                op("act", lambda e: e.activation(out=tmp8[:], in_=colst[:, 72:80], func=AF.Exp, scale=-1.0), rd=[r_p], wr=[r_p])
                op("act", lambda e: e.activation(out=tmp8[:], in_=tmp8[:], func=AF.Ln, bias=1.0), rd=[r_p], wr=[r_p])
                op("dve", lambda e: e.tensor_scalar(out=s8[:], in0=tmp8[:], scalar1=-8.0, scalar2=None, op0=ALU.mult), rd=[r_p], wr=[r_p])
                op("dve", lambda e: e.tensor_scalar(out=s16[:], in0=tmp8[:], scalar1=-16.0, scalar2=None, op0=ALU.mult), rd=[r_p], wr=[r_p])
                op("dve", lambda e: e.tensor_tensor(out=dprod[:, 0:64], in0=dl[:, 0:64], in1=dl[:, 64:128], op=ALU.mult), rd=[r_p], wr=[r_p])
                op("dve", lambda e: e.tensor_tensor(out=dprod[:, 64:128], in0=dl[:, 128:192], in1=dl[:, 192:256], op=ALU.mult), rd=[r_p], wr=[r_p])
                op("dve", lambda e: e.tensor_reduce(out=dsum[:, 0:1], in_=dprod[:, 0:64], axis=AX.X, op=ALU.add), rd=[r_p], wr=[r_p])
                op("dve", lambda e: e.tensor_reduce(out=dsum[:, 1:2], in_=dprod[:, 64:128], axis=AX.X, op=ALU.add), rd=[r_p], wr=[r_p])
                op("act", lambda e: e.activation(out=dsum[:, 2:4], in_=dsum[:, 0:2], func=AF.Exp), rd=[r_p], wr=[r_p])
                op("dve", lambda e: e.tensor_tensor(out=neglam[:], in0=dsum[:, 3:4], in1=dsum[:, 2:3], op=ALU.subtract), rd=[r_p], wr=[r_p])
                op("dve", lambda e: e.tensor_scalar(out=neglam[:], in0=neglam[:], scalar1=-lam_init, scalar2=None, op0=ALU.add), rd=[r_p], wr=[r_p])
                op("dve", lambda e: e.tensor_scalar(out=subgs[:], in0=subgs[:], scalar1=1.0 - lam_init, scalar2=None, op0=ALU.mult), rd=[r_p], wr=[r_p])
                tk.barrier()

                for p in range(NPASS):
                    tok_p = p * PASS
                    with ExitStack() as s1:
                        hT = sb(s1, "hT", [128, KC, PASS], BF16)
                        r_hT = [Res() for _ in range(PT)]
                        with ExitStack() as s0:
                            xt = [sb(s0, "xt%d" % i, [128, D], F32) for i in range(2)]
                            xn = [sb(s0, "xn%d" % i, [128, D], BF16) for i in range(2)]
                            junk = sb(s0, "junk", [128, D], BF16)
                            st = [sb(s0, "st%d" % i, [128, 4], F32) for i in range(2)]
                            r_xt = [Res(), Res()]
                            r_xn = [Res(), Res()]
                            r_junk = Res()
                            r_st = [Res(), Res()]
                            dma("sp", xt[0][:], x_src[tok_p:tok_p + 128, :], wr=[r_xt[0]])
                            for tt in range(PT):
                                b = tt % 2
                                if tt + 1 < PT:
                                    t1 = tok_p + (tt + 1) * 128
                                    dma("sp", xt[1 - b][:], x_src[t1:t1 + 128, :], wr=[r_xt[1 - b]])
                                op("act", lambda e, b=b: e.activation(out=junk[:], in_=xt[b][:], func=AF.Square, accum_out=st[b][:, 0:1]),
                                   rd=[r_xt[b]], wr=[r_junk, r_st[b]])
                                op("act", lambda e, b=b: e.activation(out=st[b][:, 1:2], in_=st[b][:, 0:1], func=AF.Sqrt, scale=1.0 / D, bias=EPS),
                                   rd=[r_st[b]], wr=[r_st[b]])
                                op("dve", lambda e, b=b: e.reciprocal(out=st[b][:, 2:3], in_=st[b][:, 1:2]), rd=[r_st[b]], wr=[r_st[b]])
                                op("dve", lambda e, b=b: e.tensor_scalar(out=xn[b][:], in0=xt[b][:], scalar1=st[b][:, 2:3], scalar2=None, op0=ALU.mult),
                                   rd=[r_xt[b], r_st[b]], wr=[r_xn[b]])
                                for half in range(2):
                                    pbk = 6 + half
                                    ptv = psb[pbk][:].bitcast(BF16)
                                    for j in range(8):
                                        kc = half * 8 + j
                                        op("pe", lambda e, b=b, kc=kc, j=j, ptv=ptv: e.transpose(out=ptv[:, j * 128:(j + 1) * 128], in_=xn[b][:, kc * 128:(kc + 1) * 128], identity=ident_b[:]),
                                           rd=[r_xn[b]], wr=[psr[pbk]])
                                    src = ptv.rearrange("p (j t) -> p j t", j=8)
                                    dst = hT[:, half * 8:(half + 1) * 8, tt * 128:(tt + 1) * 128]
                                    if half == 0:
                                        op("act", lambda e, src=src, dst=dst: e.activation(out=dst, in_=src, func=AF.Copy), rd=[psr[pbk]], wr=[r_hT[tt]])
                                    else:
                                        op("dve", lambda e, src=src, dst=dst: e.tensor_copy(out=dst, in_=src), rd=[psr[pbk]], wr=[r_hT[tt]])
                            tk.barrier()
                            if stop == 1:
                                return nc
                        with ExitStack() as s2:
                            wst = [sb(s2, "wst%d" % i, [128, KC, 256], F32) for i in range(2)]
                            wbf = [sb(s2, "wbf%d" % i, [128, KC, 256], BF16) for i in range(2)]
                            r_wst = [Res(), Res()]
                            r_wbf = [Res(), Res()]
                            fst = [sb(s2, "fst%d" % i, [128, 512], BF16) for i in range(2)]
                            r_fst = [Res(), Res()]
                            tmb = [sb(s2, "tmb%d" % i, [128, 256], BF16) for i in range(2)]
                            r_tmb = [Res(), Res()]
                            ra = sb(s2, "ra", [128, 256], F32)
                            rb = sb(s2, "rb", [128, 256], F32)
                            r_ra = Res()
                            r_rb = Res()
                            tst = [sb(s2, "tst%d" % i, [128, 2, 512], BF16) for i in range(2)]
                            r_tst = [Res(), Res()]
                            rtab = sb(s2, "rtab", [128, 4, PT, 128], F32)
                            dtt = [sb(s2, "dtt%d" % i, [128, 3, 256], F32) for i in range(2)]
                            r_dtt = [Res(), Res()]
                            rc = sb(s2, "rc", [128, 256], F32)
                            r_rc = Res()
                            rbd = sb(s2, "rbd", [128, 256], F32)
                            r_rbd = Res()
                            op("pool", lambda e: e.memset(rbd[:], 0.0), wr=[r_rbd])
                            op("pool", lambda e: e.memset(rc[:], 0.0), wr=[r_rc])
                            r_tab = Res()
                            for i in range(4):
                                dma("sp", rtab[:, i, :, :], ropeR[i, tok_p:tok_p + PASS, :].rearrange("(n p) f -> p n f", p=128), wr=[r_tab])
                            groups = []
                            for g in range(8):
                                groups.append(("xa", g * 128, 128, g))
                            for kind, base in (("qr", 2048), ("kr", 3072), ("vr", 4096), ("qd", 6144), ("kd", 7168), ("vd", 8192)):
                                for g in range(4):
                                    groups.append((kind, base + g * 256, 256, g))
                            for g in range(8):
                                groups.append(("ga", 1024 + g * 128, 128, g))
                            for kind, base in (("gr", 5120), ("gd", 9216)):
                                for g in range(4):
                                    groups.append((kind, base + g * 256, 256, g))
                            for g in range(48):
                                groups.append(("gm", 10240 + g * 128, 128, g))
                            if kinds is not None:
                                groups = [g_ for g_ in groups if g_[0] in kinds]
                            wv = w_in[l].rearrange("(kc p) n -> p kc n", p=128)

                            def wload(gi):
                                _, c0, ncol, _ = groups[gi]
                                b = gi % 2
                                dma("sp", wst[b][:, :, 0:ncol], wv[:, :, c0:c0 + ncol], wr=[r_wst[b]])

                            def wcast(gi):
                                _, c0, ncol, _ = groups[gi]
                                b = gi % 2
                                for kc in range(KC):
                                    if kc % 2 == 0:
                                        op("dve", lambda e, b=b, kc=kc, ncol=ncol: e.tensor_scalar(out=wbf[b][:, kc, 0:ncol], in0=wst[b][:, kc, 0:ncol], scalar1=colst[:, kc:kc + 1], scalar2=None, op0=ALU.mult),
                                           rd=[r_wst[b]], wr=[r_wbf[b]])
                                    else:
                                        op("act", lambda e, b=b, kc=kc, ncol=ncol: e.activation(out=wbf[b][:, kc, 0:ncol], in_=wst[b][:, kc, 0:ncol], func=AF.Copy, scale=colst[:, kc:kc + 1]),
                                           rd=[r_wst[b]], wr=[r_wbf[b]])

                            cnt = {"ps": 0, "fst": 0, "tmb": 0, "tst": 0, "pt": 0, "dtt": 0}

                            def compute(gi):
                                kind, c0, ncol, g = groups[gi]
                                b = gi % 2
                                if ncol == 128:
                                    dst = {"xa": xaT, "ga": gaT, "gm": gmT}[kind]
                                    func = {"xa": AF.Copy, "ga": AF.Silu, "gm": AF.Sigmoid}[kind]
                                    for tb in range(PB):
                                        pk = cnt["ps"] % 6
                                        cnt["ps"] += 1
                                        for kc in range(KC):
                                            op("pe", lambda e, pk=pk, kc=kc, tb=tb, b=b: e.matmul(psb[pk][:, 0:512], lhsT=wbf[b][:, kc, 0:128], rhs=hT[:, kc, tb * 512:(tb + 1) * 512], start=(kc == 0), stop=(kc == KC - 1)),
                                               rd=[r_wbf[b]] + r_hT[tb * 4:tb * 4 + 4], wr=[psr[pk]])
                                        fb = cnt["fst"] % 2
                                        cnt["fst"] += 1
                                        op("act", lambda e, pk=pk, fb=fb, func=func: e.activation(out=fst[fb][:], in_=psb[pk][:, 0:512], func=func), rd=[psr[pk]], wr=[r_fst[fb]])
                                        t0 = tok_p + tb * 512
                                        dma("pool", dst[g, :, t0:t0 + 512], fst[fb][:], rd=[r_fst[fb]])
                                    return
                                for tb in range(PB):
                                    need_t = kind in ("qr", "kr", "qd", "kd")
                                    if need_t:
                                        ptk = 6 + cnt["pt"] % 2
                                        cnt["pt"] += 1
                                        ptv = psb[ptk][:].bitcast(BF16).rearrange("p (h t d) -> p h t d", h=2, t=4)
                                    for t4 in range(4):
                                        tt = tb * 4 + t4
                                        tok0 = tok_p + tt * 128
                                        pk = cnt["ps"] % 6
                                        cnt["ps"] += 1
                                        for kc in range(KC):
                                            op("pe", lambda e, pk=pk, kc=kc, tt=tt, b=b: e.matmul(psb[pk][:, 0:256], lhsT=hT[:, kc, tt * 128:(tt + 1) * 128], rhs=wbf[b][:, kc, 0:256], start=(kc == 0), stop=(kc == KC - 1)),
                                               rd=[r_wbf[b], r_hT[tt]], wr=[psr[pk]])
                                        mb = cnt["tmb"] % 2
                                        cnt["tmb"] += 1
                                        pv = psb[pk][:, 0:256]
                                        if kind in ("vr", "vd", "gr", "gd"):
                                            func = AF.Silu if kind[0] == "g" else AF.Copy
                                            op("act", lambda e, pv=pv, mb=mb, func=func: e.activation(out=tmb[mb][:], in_=pv, func=func), rd=[psr[pk]], wr=[r_tmb[mb]])
                                            dst = {"vr": Vr, "vd": Vd, "gr": Gr, "gd": Gd}[kind]
                                            dma("pool", dst[tok0:tok0 + 128, g * 256:(g + 1) * 256], tmb[mb][:], rd=[r_tmb[mb]])
                                            continue
                                        if kind in ("qr", "kr"):
                                            ti = 0 if kind == "qr" else 2
                                            for hh in range(2):
                                                hs = slice(hh * 128, (hh + 1) * 128)
                                                op("dve", lambda e, pv=pv, hs=hs, ti=ti, tt=tt: e.tensor_tensor(out=ra[:, hs], in0=pv[:, hs], in1=rtab[:, ti, tt, :], op=ALU.mult),
                                                   rd=[psr[pk], r_tab], wr=[r_ra])
                                                pvp = pv[:, hs].rearrange("p (a two) -> p a two", two=2)
                                                rbp = rb[:, hs].rearrange("p (a two) -> p a two", two=2)
                                                stp = rtab[:, ti + 1, tt, :].rearrange("p (a two) -> p a two", two=2)
                                                op("dve", lambda e, pvp=pvp, rbp=rbp, stp=stp: e.tensor_tensor(out=rbp[:, :, 0], in0=pvp[:, :, 1], in1=stp[:, :, 0], op=ALU.mult),
                                                   rd=[psr[pk], r_tab], wr=[r_rb])
                                                op("dve", lambda e, pvp=pvp, rbp=rbp, stp=stp: e.tensor_tensor(out=rbp[:, :, 1], in0=pvp[:, :, 0], in1=stp[:, :, 1], op=ALU.mult),
                                                   rd=[psr[pk], r_tab], wr=[r_rb])
                                            op("dve", lambda e, mb=mb: e.tensor_tensor(out=tmb[mb][:], in0=ra[:], in1=rb[:], op=ALU.add), rd=[r_ra, r_rb], wr=[r_tmb[mb]])
                                            if kind == "kr":
                                                dma("pool", Kr[tok0:tok0 + 128, g * 256:(g + 1) * 256], tmb[mb][:], rd=[r_tmb[mb]])
                                        else:
                                            db = cnt["dtt"] % 2
                                            cnt["dtt"] += 1
                                            dma("sp", dtt[db][:], ropeD[:, tok0:tok0 + 128, :].rearrange("i p f -> p i f"), wr=[r_dtt[db]])
                                            op("dve", lambda e, pv=pv, db=db: e.tensor_tensor(out=ra[:], in0=pv, in1=dtt[db][:, 0, :], op=ALU.mult),
                                               rd=[psr[pk], r_dtt[db]], wr=[r_ra])
                                            op("dve", lambda e, pv=pv, db=db: e.tensor_tensor(out=rbd[:, 0:248], in0=pv[:, 8:256], in1=dtt[db][:, 1, 0:248], op=ALU.mult),
                                               rd=[psr[pk], r_dtt[db]], wr=[r_rbd])
                                            op("dve", lambda e, pv=pv, db=db: e.tensor_tensor(out=rc[:, 8:256], in0=pv[:, 0:248], in1=dtt[db][:, 2, 8:256], op=ALU.mult),
                                               rd=[psr[pk], r_dtt[db]], wr=[r_rc])
                                            op("dve", lambda e: e.tensor_tensor(out=ra[:], in0=ra[:], in1=rbd[:], op=ALU.add), rd=[r_ra, r_rbd], wr=[r_ra])
                                            op("dve", lambda e, mb=mb: e.tensor_tensor(out=tmb[mb][:], in0=ra[:], in1=rc[:], op=ALU.add), rd=[r_ra, r_rc], wr=[r_tmb[mb]])
                                        for hh in range(2):
                                            op("pe", lambda e, ptv=ptv, hh=hh, t4=t4, mb=mb: e.transpose(out=ptv[:, hh, t4, :], in_=tmb[mb][:, hh * 128:(hh + 1) * 128], identity=ident_b[:]),
                                               rd=[r_tmb[mb]], wr=[psr[ptk]])
                                    if need_t:
                                        sbk = cnt["tst"] % 2
                                        cnt["tst"] += 1
                                        src = psb[ptk][:].bitcast(BF16).rearrange("p (h s) -> p h s", h=2)
                                        op("act", lambda e, src=src, sbk=sbk: e.activation(out=tst[sbk][:], in_=src, func=AF.Copy), rd=[psr[ptk]], wr=[r_tst[sbk]])
                                        dst = {"qr": QrT, "kr": KrT, "qd": QdT, "kd": KdT}[kind]
                                        t0 = tok_p + tb * 512
                                        dma("pool", dst[2 * g:2 * g + 2, :, t0:t0 + 512].rearrange("h d s -> d h s"), tst[sbk][:], rd=[r_tst[sbk]])

                            ng = len(groups)
                            wload(0)
                            wload(1)
                            wcast(0)
                            for gi in range(ng):
                                if gi + 1 < ng:
                                    wcast(gi + 1)
                                compute(gi)
                                if gi + 2 < ng:
                                    wload(gi + 2)
                            tk.barrier()

                if stop == 2:
                    tk.barrier()
                    return nc
                with ExitStack() as s3:
                    xpad = [sb(s3, "xpad%d" % i, [128, S + 4], BF16) for i in range(2)]
                    sga = [sb(s3, "sga%d" % i, [128, S], BF16) for i in range(2)]
                    r_in = [Res(), Res()]
                    wlf = sb(s3, "wlf", [128, 2, 128], F32)
                    wlb = [sb(s3, "wlb%d" % i, [128, 2, 128], BF16) for i in range(2)]
                    r_wlf = Res()
                    r_wlb = [Res(), Res()]
                    dg = [sb(s3, "dg%d" % i, [128, 4, 128], BF16) for i in range(2)]
                    r_dg = [Res(), Res()]
                    xc = sb(s3, "xc", [128, S], BF16)
                    rr = sb(s3, "rr", [128, S], F32)
                    ii = sb(s3, "ii", [128, S], F32)
                    aa = sb(s3, "aa", [128, S], F32)
                    a2 = sb(s3, "a2", [128, S], F32)
                    ya = [sb(s3, "ya%d" % i, [128, S], BF16) for i in range(2)]
                    r_xc, r_rr, r_ii, r_aa, r_a2 = Res(), Res(), Res(), Res(), Res()
                    r_ya = [Res(), Res()]
                    for i in range(2):
                        op("pool", lambda e, i=i: e.memset(xpad[i][:, 0:4], 0.0), wr=[r_in[i]])

                    def lru_load(cc):
                        b = cc % 2
                        dma("sp", xpad[b][:, 3:3 + S], xaT[cc], wr=[r_in[b]])
                        dma("sp", sga[b][:], gaT[cc], wr=[r_in[b]])

                    lru_load(0)
                    for cc in range(8):
                        b = cc % 2
                        if cc + 1 < 8:
                            lru_load(cc + 1)
                        dma("sp", wlf[:, 0, :], lruw[l, 0, cc], wr=[r_wlf])
                        dma("sp", wlf[:, 1, :], lruw[l, 1, cc], wr=[r_wlf])
                        op("dve", lambda e, b=b: e.tensor_copy(out=wlb[b][:], in_=wlf[:]), rd=[r_wlf], wr=[r_wlb[b]])
                        for k in range(4):
                            op("dve", lambda e, b=b, k=k, cc=cc: e.tensor_scalar(out=dg[b][:, k, :], in0=ident_f[:], scalar1=colst[:, 16 + k * 8 + cc:17 + k * 8 + cc], scalar2=None, op0=ALU.mult),
                               wr=[r_dg[b]])
                        pc = 0
                        for tb in range(NB):
                            pk = pc % 6
                            pc += 1
                            for k in range(4):
                                op("pe", lambda e, pk=pk, k=k, tb=tb, b=b: e.matmul(psb[pk][:, 0:512], lhsT=dg[b][:, k, :], rhs=xpad[b][:, tb * 512 + k:tb * 512 + k + 512], start=(k == 0), stop=(k == 3)),
                                   rd=[r_dg[b], r_in[b]], wr=[psr[pk]])
                            op("act", lambda e, pk=pk, tb=tb, cc=cc: e.activation(out=xc[:, tb * 512:(tb + 1) * 512], in_=psb[pk][:, 0:512], func=AF.Identity, bias=colst[:, 48 + cc:49 + cc]),
                               rd=[psr[pk]], wr=[r_xc])
                        for tb in range(NB):
                            for gi_, (dstt, r_d, bo) in enumerate(((rr, r_rr, 56), (ii, r_ii, 64))):
                                pk = pc % 6
                                pc += 1
                                op("pe", lambda e, pk=pk, tb=tb, b=b, gi_=gi_: e.matmul(psb[pk][:, 0:512], lhsT=wlb[b][:, gi_, :], rhs=xc[:, tb * 512:(tb + 1) * 512], start=True, stop=True),
                                   rd=[r_wlb[b], r_xc], wr=[psr[pk]])
                                op("act", lambda e, pk=pk, tb=tb, dstt=dstt, bo=bo, cc=cc: e.activation(out=dstt[:, tb * 512:(tb + 1) * 512], in_=psb[pk][:, 0:512], func=AF.Sigmoid, bias=colst[:, bo + cc:bo + cc + 1]),
                                   rd=[psr[pk]], wr=[r_d])
                        op("act", lambda e, cc=cc: e.activation(out=aa[:], in_=rr[:], func=AF.Exp, scale=s8[:, cc:cc + 1]), rd=[r_rr], wr=[r_aa])
                        op("act", lambda e, cc=cc: e.activation(out=a2[:], in_=rr[:], func=AF.Exp, scale=s16[:, cc:cc + 1]), rd=[r_rr], wr=[r_a2])
                        op("dve", lambda e: e.tensor_scalar(out=a2[:], in0=a2[:], scalar1=-1.0, scalar2=1.0, op0=ALU.mult, op1=ALU.add), rd=[r_a2], wr=[r_a2])
                        op("act", lambda e: e.activation(out=a2[:], in_=a2[:], func=AF.Sqrt), rd=[r_a2], wr=[r_a2])
                        op("dve", lambda e: e.tensor_tensor(out=ii[:], in0=ii[:], in1=xc[:], op=ALU.mult), rd=[r_ii, r_xc], wr=[r_ii])
                        op("dve", lambda e: e.tensor_tensor(out=ii[:], in0=ii[:], in1=a2[:], op=ALU.mult), rd=[r_ii, r_a2], wr=[r_ii])
                        op("dve", lambda e: e.tensor_tensor_scan(out=rr[:], data0=aa[:], data1=ii[:], initial=0.0, op0=ALU.mult, op1=ALU.add), rd=[r_aa, r_ii], wr=[r_rr])
                        op("dve", lambda e, b=b: e.tensor_tensor(out=ya[b][:], in0=rr[:], in1=sga[b][:], op=ALU.mult), rd=[r_rr, r_in[b]], wr=[r_ya[b]])
                        dma("pool", yT[0, cc], ya[b][:], rd=[r_ya[b]])
                    tk.barrier()

                if stop == 3:
                    tk.barrier()
                    return nc
                with ExitStack() as s4:
                    itT = sb(s4, "itT", [128, 8, 128], F32)
                    kdt = sb(s4, "kdt", [128, 8], F32)
                    qdt = sb(s4, "qdt", [128, 8, 512], F32)
                    r_k = Res()
                    dma("sp", itT[:], intraT, wr=[r_k])
                    dma("sp", kdt[:], kdec, wr=[r_k])
                    dma("sp", qdt[:], qdec, wr=[r_k])
                    qT = [sb(s4, "qT%d" % i, [128, 8, 512], BF16) for i in range(2)]
                    kT = [sb(s4, "kT%d" % i, [128, 8, 512], BF16) for i in range(2)]
                    kM = [sb(s4, "kM%d" % i, [128, 4, 1024], BF16) for i in range(2)]
                    vM = [sb(s4, "vM%d" % i, [128, 4, 1024], BF16) for i in range(2)]
                    gM = [sb(s4, "gM%d" % i, [128, 4, 1024], BF16) for i in range(2)]
                    r_q = [Res(), Res()]
                    r_kt = [Res(), Res()]
                    r_km = [Res(), Res()]
                    r_vm = [Res(), Res()]
                    r_gm = [Res(), Res()]
                    qTd = sb(s4, "qTd", [128, 8, 512], BF16)
                    kMd = sb(s4, "kMd", [128, 4, 1024], BF16)
                    r_qTd, r_kMd = Res(), Res()
                    Sf = sb(s4, "Sf", [128, 8, 128], F32)
                    Sb = sb(s4, "Sb", [128, 8, 128], BF16)
                    r_Sf, r_Sb = Res(), Res()
                    sT = [sb(s4, "sT%d" % i, [128, 512], BF16) for i in range(2)]
                    r_sT = [Res(), Res()]
                    bst = sb(s4, "bst", [128, 8, 6], F32)
                    mv = sb(s4, "mv", [128, 8, 2], F32)
                    rs = sb(s4, "rs", [128, 8], F32)
                    r_bst, r_mv, r_rs = Res(), Res(), Res()
                    yn = sb(s4, "yn", [128, 1024], F32)
                    ybm = sb(s4, "ybm", [128, 1024], BF16)
                    r_yn, r_ybm = Res(), Res()
                    ybt = [sb(s4, "ybt%d" % i, [128, 8, 512], BF16) for i in range(2)]
                    r_ybt = [Res(), Res()]
                    op("dve", lambda e: e.memset(Sf[:], 0.0), wr=[r_Sf])
                    op("pool", lambda e: e.memset(Sb[:], 0.0), wr=[r_Sb])

                    def ret_load(tb):
                        b = tb % 2
                        t0 = tb * 512
                        dma("sp", qT[b][:], QrT[:, :, t0:t0 + 512].rearrange("h d s -> d h s"), wr=[r_q[b]])
                        dma("sp", kT[b][:], KrT[:, :, t0:t0 + 512].rearrange("h d s -> d h s"), wr=[r_kt[b]])
                        dma("sp", kM[b][:], Kr[t0:t0 + 512, :].rearrange("(n p) f -> p n f", p=128), wr=[r_km[b]])
                        dma("sp", vM[b][:], Vr[t0:t0 + 512, :].rearrange("(n p) f -> p n f", p=128), wr=[r_vm[b]])
                        dma("sp", gM[b][:], Gr[t0:t0 + 512, :].rearrange("(n p) f -> p n f", p=128), wr=[r_gm[b]])

                    ret_load(0)
                    pc = 0
                    for tb in range(NB):
                        b = tb % 2
                        if tb + 1 < NB:
                            ret_load(tb + 1)
                        op("dve", lambda e, b=b: e.tensor_tensor(out=qTd[:], in0=qT[b][:], in1=qdt[:], op=ALU.mult), rd=[r_q[b], r_k], wr=[r_qTd])
                        for h in range(8):
                            op("dve", lambda e, b=b, h=h: e.tensor_scalar(out=kMd[:, :, h * 128:(h + 1) * 128], in0=kM[b][:, :, h * 128:(h + 1) * 128], scalar1=kdt[:, h:h + 1], scalar2=None, op0=ALU.mult),
                               rd=[r_km[b], r_k], wr=[r_kMd])
                        for n in range(4):
                            cs = slice(n * 128, (n + 1) * 128)
                            obank = []
                            for hg in range(2):
                                pk = pc % 6
                                pc += 1
                                for h4 in range(4):
                                    h = hg * 4 + h4
                                    op("pe", lambda e, pk=pk, h4=h4, h=h, b=b, cs=cs: e.matmul(psb[pk][:, h4 * 128:(h4 + 1) * 128], lhsT=kT[b][:, h, cs], rhs=qT[b][:, h, cs], start=True, stop=True),
                                       rd=[r_kt[b], r_q[b]], wr=[psr[pk]])
                                sbk = (2 * n + hg) % 2
                                op("dve", lambda e, pk=pk, sbk=sbk, hg=hg: e.tensor_tensor(out=sT[sbk][:].rearrange("p (h i) -> p h i", h=4), in0=psb[pk][:, 0:512].rearrange("p (h i) -> p h i", h=4), in1=itT[:, hg * 4:(hg + 1) * 4, :], op=ALU.mult),
                                   rd=[psr[pk], r_k], wr=[r_sT[sbk]])
                                po = pc % 6
                                pc += 1
                                obank.append(po)
                                for h4 in range(4):
                                    h = hg * 4 + h4
                                    op("pe", lambda e, po=po, h4=h4, h=h, b=b, n=n, sbk=sbk: e.matmul(psb[po][:, h4 * 128:(h4 + 1) * 128], lhsT=sT[sbk][:, h4 * 128:(h4 + 1) * 128], rhs=vM[b][:, n, h * 128:(h + 1) * 128], start=True, stop=False),
                                       rd=[r_sT[sbk], r_vm[b]], wr=[psr[po]])
                                    op("pe", lambda e, po=po, h4=h4, h=h, cs=cs: e.matmul(psb[po][:, h4 * 128:(h4 + 1) * 128], lhsT=qTd[:, h, cs], rhs=Sb[:, h, :], start=False, stop=True),
                                       rd=[r_qTd, r_Sb], wr=[psr[po]])
                            for hg in range(2):
                                pk = pc % 6
                                pc += 1
                                for h4 in range(4):
                                    h = hg * 4 + h4
                                    op("pe", lambda e, pk=pk, h4=h4, h=h, b=b, n=n: e.matmul(psb[pk][:, h4 * 128:(h4 + 1) * 128], lhsT=kMd[:, n, h * 128:(h + 1) * 128], rhs=vM[b][:, n, h * 128:(h + 1) * 128], start=True, stop=True),
                                       rd=[r_kMd, r_vm[b]], wr=[psr[pk]])
                                for h4 in range(4):
                                    h = hg * 4 + h4
                                    cd = float(np.exp(np.float32(128.0) * np.log1p(-np.exp2(np.float32(-5.0 - h)))))
                                    op("dve", lambda e, pk=pk, h4=h4, h=h, cd=cd: e.scalar_tensor_tensor(out=Sf[:, h, :], in0=Sf[:, h, :], scalar=cd, in1=psb[pk][:, h4 * 128:(h4 + 1) * 128], op0=ALU.mult, op1=ALU.add),
                                       rd=[psr[pk], r_Sf], wr=[r_Sf])
                            op("act", lambda e: e.activation(out=Sb[:], in_=Sf[:], func=AF.Copy), rd=[r_Sf], wr=[r_Sb])
                            for hg in range(2):
                                po = obank[hg]
                                for h4 in range(4):
                                    h = hg * 4 + h4
                                    op("dve", lambda e, po=po, h4=h4, h=h: e.bn_stats(out=bst[:, h, :], in_=psb[po][:, h4 * 128:(h4 + 1) * 128]), rd=[psr[po]], wr=[r_bst])
                                    op("dve", lambda e, h=h: e.bn_aggr(out=mv[:, h, :], in_=bst[:, h, :]), rd=[r_bst], wr=[r_mv])
                            op("act", lambda e: e.activation(out=rs[:], in_=mv[:, :, 1], func=AF.Sqrt, bias=EPS), rd=[r_mv], wr=[r_rs])
                            op("dve", lambda e: e.reciprocal(out=rs[:], in_=rs[:]), rd=[r_rs], wr=[r_rs])
                            for hg in range(2):
                                po = obank[hg]
                                for h4 in range(4):
                                    h = hg * 4 + h4
                                    op("dve", lambda e, po=po, h4=h4, h=h: e.tensor_scalar(out=yn[:, h * 128:(h + 1) * 128], in0=psb[po][:, h4 * 128:(h4 + 1) * 128], scalar1=mv[:, h, 0:1], scalar2=rs[:, h:h + 1], op0=ALU.subtract, op1=ALU.mult),
                                       rd=[psr[po], r_mv, r_rs], wr=[r_yn])
                            op("dve", lambda e, b=b, n=n: e.tensor_tensor(out=ybm[:], in0=yn[:], in1=gM[b][:, n, :], op=ALU.mult), rd=[r_yn, r_gm[b]], wr=[r_ybm])
                            ptk = 6 + (tb * 4 + n) % 2
                            ptv = psb[ptk][:].bitcast(BF16).rearrange("p (h t) -> p h t", h=8)
                            for h in range(8):
                                op("pe", lambda e, ptv=ptv, h=h: e.transpose(out=ptv[:, h, :], in_=ybm[:, h * 128:(h + 1) * 128], identity=ident_b[:]), rd=[r_ybm], wr=[psr[ptk]])
                            op("act", lambda e, ptv=ptv, b=b, cs=cs: e.activation(out=ybt[b][:, :, cs], in_=ptv, func=AF.Copy), rd=[psr[ptk]], wr=[r_ybt[b]])
                        t0 = tb * 512
                        dma("pool", yT[1, :, :, t0:t0 + 512].rearrange("h d s -> d h s"), ybt[b][:], rd=[r_ybt[b]])
                    tk.barrier()

                if stop == 4:
                    tk.barrier()
                    return nc
                with ExitStack() as s5:
                    qh = [sb(s5, "qh%d" % i, [128, S], BF16) for i in range(2)]
                    kh = [sb(s5, "kh%d" % i, [128, S], BF16) for i in range(2)]
                    vh = [sb(s5, "vh%d" % i, [128, NT, 132], BF16) for i in range(2)]
                    gh = [sb(s5, "gh%d" % i, [128, NT, 128], BF16) for i in range(2)]
                    r_qh = [Res(), Res()]
                    r_kh = [Res(), Res()]
                    r_vh = [Res(), Res()]
                    r_gh = [Res(), Res()]
                    NE = 3
                    Eb = [[sb(s5, "E%d_%d" % (c, i), [128, 512], BF16) for i in range(NE)] for c in range(2)]
                    r_E = [[Res() for _ in range(NE)] for _ in range(2)]
                    sm = sb(s5, "sm", [128, 8], F32)
                    r_sm = Res()
                    o1 = sb(s5, "o1", [128, 128], F32)
                    o2 = sb(s5, "o2", [128, 128], F32)
                    jk = sb(s5, "jk", [128, 128], BF16)
                    ycm = sb(s5, "ycm", [128, 128], BF16)
                    r_o1, r_o2, r_jk, r_ycm = Res(), Res(), Res(), Res()
                    yct = [sb(s5, "yct%d" % i, [128, 512], BF16) for i in range(2)]
                    r_yct = [Res(), Res()]
                    for i in range(2):
                        op("pool", lambda e, i=i: e.memset(vh[i][:, :, 128:129], 1.0), wr=[r_vh[i]])

                    def da_load(h):
                        b = h % 2
                        dma("sp", qh[b][:], QdT[h], wr=[r_qh[b]])
                        dma("sp", kh[b][:], KdT[h], wr=[r_kh[b]])
                        dma("sp", vh[b][:, :, 0:128], Vd[:, h * 128:(h + 1) * 128].rearrange("(n p) e -> p n e", p=128), wr=[r_vh[b]])
                        dma("sp", gh[b][:], Gd[:, h * 128:(h + 1) * 128].rearrange("(n p) e -> p n e", p=128), wr=[r_gh[b]])

                    def acc_ap(c, qs):
                        if qs < 3:
                            return c, psb[c][:, qs * 129:qs * 129 + 129]
                        return 2, psb[2][:, c * 129:c * 129 + 129]

                    da_load(0)
                    sc = 0
                    ec = 0
                    yc_i = 0
                    for h in range(8):
                        b = h % 2
                        if h + 1 < 8:
                            da_load(h + 1)
                        for qb in range(NB):
                            nkt = 4 * qb + 4
                            started = [False, False, False]
                            for kt in range(nkt):
                                r = kt - 4 * qb
                                c0 = r * 128 if r > 0 else 0
                                for c in range(2):
                                    pk = 3 + sc % 4
                                    sc += 1
                                    ps_ = slice(c * 64, (c + 1) * 64)
                                    op("pe", lambda e, pk=pk, ps_=ps_, kt=kt, qb=qb, c0=c0, b=b: e.matmul(psb[pk][:, c0:512], lhsT=kh[b][ps_, kt * 128:(kt + 1) * 128], rhs=qh[b][ps_, qb * 512 + c0:(qb + 1) * 512], start=True, stop=True),
                                       rd=[r_kh[b], r_qh[b]], wr=[psr[pk]])
                                    ei = ec % NE
                                    ec += 1
                                    Et = Eb[c][ei]
                                    rE = r_E[c][ei]
                                    op("act", lambda e, pk=pk, Et=Et, c0=c0: e.activation(out=Et[:, c0:512], in_=psb[pk][:, c0:512], func=AF.Exp, scale=0.125), rd=[psr[pk]], wr=[rE])
                                    if r >= 0:
                                        op("pool", lambda e, Et=Et, c0=c0: e.tensor_tensor(out=Et[:, c0:c0 + 128], in0=Et[:, c0:c0 + 128], in1=tri_b[:], op=ALU.mult), rd=[rE], wr=[rE])
                                    for qs in range(max(r, 0), 4):
                                        bk, aap = acc_ap(c, qs)
                                        first = not started[bk]
                                        started[bk] = True
                                        last = (kt == 4 * qb + qs)
                                        op("pe", lambda e, aap=aap, Et=Et, qs=qs, kt=kt, b=b, first=first, last=last: e.matmul(aap, lhsT=Et[:, qs * 128:(qs + 1) * 128], rhs=vh[b][:, kt, 0:129], start=first, stop=last, skip_group_check=True),
                                           rd=[rE, r_vh[b]], wr=[psr[bk]])
                            yb_ = yc_i % 2
                            yc_i += 1
                            ptk = 7
                            ptv = psb[ptk][:].bitcast(BF16)
                            for qs in range(4):
                                bk0, a0 = acc_ap(0, qs)
                                bk1, a1 = acc_ap(1, qs)
                                tile_i = qb * 4 + qs
                                op("dve", lambda e, a0=a0: e.reciprocal(out=sm[:, 0:1], in_=a0[:, 128:129]), rd=[psr[bk0]], wr=[r_sm])
                                op("dve", lambda e, a1=a1: e.reciprocal(out=sm[:, 1:2], in_=a1[:, 128:129]), rd=[psr[bk1]], wr=[r_sm])
                                op("dve", lambda e: e.tensor_tensor(out=sm[:, 2:3], in0=sm[:, 1:2], in1=neglam[:], op=ALU.mult), rd=[r_sm], wr=[r_sm])
                                op("act", lambda e, a0=a0: e.activation(out=o1[:], in_=a0[:, 0:128], func=AF.Copy, scale=sm[:, 0:1]), rd=[psr[bk0], r_sm], wr=[r_o1])
                                op("dve", lambda e, a1=a1: e.scalar_tensor_tensor(out=o2[:], in0=a1[:, 0:128], scalar=sm[:, 2:3], in1=o1[:], op0=ALU.mult, op1=ALU.add), rd=[psr[bk1], r_sm, r_o1], wr=[r_o2])
                                op("act", lambda e: e.activation(out=jk[:], in_=o2[:], func=AF.Square, accum_out=sm[:, 3:4]), rd=[r_o2], wr=[r_jk, r_sm])
                                op("act", lambda e: e.activation(out=sm[:, 4:5], in_=sm[:, 3:4], func=AF.Sqrt, scale=1.0 / 128, bias=EPS), rd=[r_sm], wr=[r_sm])
                                op("dve", lambda e: e.reciprocal(out=sm[:, 5:6], in_=sm[:, 4:5]), rd=[r_sm], wr=[r_sm])
                                op("dve", lambda e: e.scalar_tensor_tensor(out=o1[:], in0=o2[:], scalar=sm[:, 5:6], in1=subgs[:], op0=ALU.mult, op1=ALU.mult), rd=[r_o2, r_sm], wr=[r_o1])
                                op("dve", lambda e, b=b, tile_i=tile_i: e.tensor_tensor(out=ycm[:], in0=o1[:], in1=gh[b][:, tile_i, :], op=ALU.mult), rd=[r_o1, r_gh[b]], wr=[r_ycm])
                                op("pe", lambda e, ptv=ptv, qs=qs: e.transpose(out=ptv[:, qs * 128:(qs + 1) * 128], in_=ycm[:], identity=ident_b[:]), rd=[r_ycm], wr=[psr[ptk]])
                            op("act", lambda e, ptv=ptv, yb_=yb_: e.activation(out=yct[yb_][:], in_=ptv[:, 0:512], func=AF.Copy), rd=[psr[ptk]], wr=[r_yct[yb_]])
                            dma("pool", yT[2, h, :, qb * 512:(qb + 1) * 512], yct[yb_][:], rd=[r_yct[yb_]])
                    tk.barrier()

                if stop == 5:
                    tk.barrier()
                    return nc
                with ExitStack() as s6:
                    WB = sb(s6, "WB", [128, 3, 8, D], BF16)
                    r_WB = Res()
                    wsf = [sb(s6, "wsf%d" % i, [128, D], F32) for i in range(2)]
                    r_wsf = [Res(), Res()]
                    i_ = 0
                    for br in range(3):
                        for kc in range(8):
                            b = i_ % 2
                            dma("sp", wsf[b][:], w_br[l, br, kc * 128:(kc + 1) * 128, :], wr=[r_wsf[b]])
                            if i_ % 2 == 0:
                                op("dve", lambda e, b=b, br=br, kc=kc: e.tensor_copy(out=WB[:, br, kc, :], in_=wsf[b][:]), rd=[r_wsf[b]], wr=[r_WB])
                            else:
                                op("act", lambda e, b=b, br=br, kc=kc: e.activation(out=WB[:, br, kc, :], in_=wsf[b][:], func=AF.Copy), rd=[r_wsf[b]], wr=[r_WB])
                            i_ += 1
                    yTb = [sb(s6, "yTb%d" % i, [128, 3, 8, 512], BF16) for i in range(2)]
                    r_yTb = [Res(), Res()]
                    gmb = [sb(s6, "gmb%d" % i, [128, 3, 512], BF16) for i in range(2)]
                    r_gmb = [Res(), Res()]
                    t0b = sb(s6, "t0b", [128, 512], F32)
                    t1b = sb(s6, "t1b", [128, 512], F32)
                    t2b = sb(s6, "t2b", [128, 512], F32)
                    r_t0, r_t1, r_t2 = Res(), Res(), Res()
                    mst = [sb(s6, "mst%d" % i, [128, 512], BF16) for i in range(2)]
                    r_mst = [Res(), Res()]

                    def y_load(tb):
                        b = tb % 2
                        t0 = tb * 512
                        for br in range(3):
                            dma("sp", yTb[b][:, br, :, :], yT[br, :, :, t0:t0 + 512].rearrange("c p s -> p c s"), wr=[r_yTb[b]])

                    def gm_load(tb, dc, gi):
                        t0 = tb * 512
                        b = gi % 2
                        dma("sp", gmb[b][:], gmT[:, :, t0:t0 + 512].rearrange("(br dc) p s -> dc p br s", br=3)[dc], wr=[r_gmb[b]])

                    y_load(0)
                    gi = 0
                    gm_load(0, 0, 0)
                    pc = 0
                    for tb in range(NB):
                        b = tb % 2
                        if tb + 1 < NB:
                            y_load(tb + 1)
                        for dc in range(16):
                            nxt = tb * 16 + dc + 1
                            if nxt < NB * 16:
                                gm_load(nxt // 16, nxt % 16, gi + 1)
                            gb = gi % 2
                            gi += 1
                            pks = []
                            for br in range(3):
                                pk = pc % 8
                                pc += 1
                                pks.append(pk)
                                for kc in range(8):
                                    op("pe", lambda e, pk=pk, br=br, kc=kc, dc=dc, b=b: e.matmul(psb[pk][:, 0:512], lhsT=WB[:, br, kc, dc * 128:(dc + 1) * 128], rhs=yTb[b][:, br, kc, :], start=(kc == 0), stop=(kc == 7)),
                                       rd=[r_WB, r_yTb[b]], wr=[psr[pk]])
                            op("dve", lambda e, pk=pks[0], gb=gb: e.tensor_tensor(out=t0b[:], in0=psb[pk][:, 0:512], in1=gmb[gb][:, 0, :], op=ALU.mult), rd=[psr[pks[0]], r_gmb[gb]], wr=[r_t0])
                            op("dve", lambda e, pk=pks[1], gb=gb: e.tensor_tensor(out=t1b[:], in0=psb[pk][:, 0:512], in1=gmb[gb][:, 1, :], op=ALU.mult), rd=[psr[pks[1]], r_gmb[gb]], wr=[r_t1])
                            op("dve", lambda e, pk=pks[2], gb=gb: e.tensor_tensor(out=t2b[:], in0=psb[pk][:, 0:512], in1=gmb[gb][:, 2, :], op=ALU.mult), rd=[psr[pks[2]], r_gmb[gb]], wr=[r_t2])
                            op("pool", lambda e: e.tensor_tensor(out=t0b[:], in0=t0b[:], in1=t1b[:], op=ALU.add), rd=[r_t0, r_t1], wr=[r_t0])
                            mb = (tb * 16 + dc) % 2
                            op("pool", lambda e, mb=mb: e.tensor_tensor(out=mst[mb][:], in0=t0b[:], in1=t2b[:], op=ALU.add), rd=[r_t0, r_t2], wr=[r_mst[mb]])
                            dma("pool", mT[dc, :, tb * 512:(tb + 1) * 512], mst[mb][:], rd=[r_mst[mb]])
                    tk.barrier()

                if stop == 6:
                    tk.barrier()
                    return nc
                with ExitStack() as s7:
                    WO = sb(s7, "WO", [128, KC, D], BF16)
                    r_WO = Res()
                    wsf = [sb(s7, "wsf%d" % i, [128, D], F32) for i in range(2)]
                    r_wsf = [Res(), Res()]
                    for kc in range(KC):
                        b = kc % 2
                        dma("sp", wsf[b][:], w_out[l, kc * 128:(kc + 1) * 128, :], wr=[r_wsf[b]])
                        if kc % 2 == 0:
                            op("dve", lambda e, b=b, kc=kc: e.tensor_copy(out=WO[:, kc, :], in_=wsf[b][:]), rd=[r_wsf[b]], wr=[r_WO])
                        else:
                            op("act", lambda e, b=b, kc=kc: e.activation(out=WO[:, kc, :], in_=wsf[b][:], func=AF.Copy), rd=[r_wsf[b]], wr=[r_WO])
                    pg = sb(s7, "pg", [128, D], F32)
                    r_pg = Res()
                    dma("sp", pg[:], postg[l], wr=[r_pg])
                    mtb = [sb(s7, "mtb%d" % i, [128, KC, 128], BF16) for i in range(2)]
                    xr = [sb(s7, "xr%d" % i, [128, D], F32) for i in range(2)]
                    r_mtb = [Res(), Res()]
                    r_xr = [Res(), Res()]
                    ot = [sb(s7, "ot%d" % i, [128, D], F32) for i in range(2)]
                    r_ot = [Res(), Res()]
                    jq = sb(s7, "jq", [128, 512], BF16)
                    r_jq = Res()
                    q4 = [sb(s7, "q4_%d" % i, [128, 8], F32) for i in range(2)]
                    r_q4 = [Res(), Res()]

                    def o_load(tt):
                        b = tt % 2
                        t0 = tt * 128
                        dma("sp", mtb[b][:], mT[:, :, t0:t0 + 128].rearrange("c p s -> p c s"), wr=[r_mtb[b]])
                        dma("sp", xr[b][:], x_src[t0:t0 + 128, :], wr=[r_xr[b]])

                    o_load(0)
                    for tt in range(NT):
                        b = tt % 2
                        if tt + 1 < NT:
                            o_load(tt + 1)
                        base = 4 * (tt % 2)
                        for eb in range(4):
                            pk = base + eb
                            for dc in range(KC):
                                op("pe", lambda e, pk=pk, dc=dc, eb=eb, b=b: e.matmul(psb[pk][:, 0:512], lhsT=mtb[b][:, dc, :], rhs=WO[:, dc, eb * 512:(eb + 1) * 512], start=(dc == 0), stop=(dc == KC - 1)),
                                   rd=[r_mtb[b], r_WO], wr=[psr[pk]])
                            op("act", lambda e, pk=pk, eb=eb, b=b: e.activation(out=jq[:], in_=psb[pk][:, 0:512], func=AF.Square, accum_out=q4[b][:, eb:eb + 1]), rd=[psr[pk]], wr=[r_jq, r_q4[b]])
                        op("dve", lambda e, b=b: e.tensor_reduce(out=q4[b][:, 4:5], in_=q4[b][:, 0:4], axis=AX.X, op=ALU.add), rd=[r_q4[b]], wr=[r_q4[b]])
                        op("act", lambda e, b=b: e.activation(out=q4[b][:, 5:6], in_=q4[b][:, 4:5], func=AF.Sqrt, scale=1.0 / D, bias=EPS), rd=[r_q4[b]], wr=[r_q4[b]])
                        op("dve", lambda e, b=b: e.reciprocal(out=q4[b][:, 6:7], in_=q4[b][:, 5:6]), rd=[r_q4[b]], wr=[r_q4[b]])
                        for eb in range(4):
                            pk = base + eb
                            es_ = slice(eb * 512, (eb + 1) * 512)
                            op("dve", lambda e, pk=pk, es_=es_, b=b: e.scalar_tensor_tensor(out=ot[b][:, es_], in0=psb[pk][:, 0:512], scalar=q4[b][:, 6:7], in1=pg[:, es_], op0=ALU.mult, op1=ALU.mult),
                               rd=[psr[pk], r_q4[b], r_pg], wr=[r_ot[b]])
                        op("pool", lambda e, b=b: e.tensor_tensor(out=ot[b][:], in0=ot[b][:], in1=xr[b][:], op=ALU.add), rd=[r_ot[b], r_xr[b]], wr=[r_ot[b]])
                        dma("pool", x_dst[tt * 128:(tt + 1) * 128, :], ot[b][:], rd=[r_ot[b]])
                    tk.barrier()
        tk.barrier()
    return nc


def _const_tables(S):
    f32 = np.float32
    pos = np.arange(S, dtype=f32)
    ret_freq = (1.0 / (f32(10000.0) ** np.linspace(0.0, 1.0, 64, dtype=f32))).astype(f32)
    ang = (pos[:, None] * ret_freq[None, :]).astype(f32)
    c, s = np.cos(ang).astype(f32), np.sin(ang).astype(f32)
    Cq = np.repeat(c, 2, axis=1)
    Sq = np.stack([-s, s], axis=-1).reshape(S, 128)
    ks = f32(128.0 ** -0.5)
    ropeR = np.stack([Cq, Sq, Cq * ks, Sq * ks]).astype(f32)
    inv = (f32(500000.0) ** (-np.arange(0, 16, 2, dtype=f32) / f32(16))).astype(f32)
    angd = (pos[:, None] * inv[None, :]).astype(f32)
    cd, sd = np.cos(angd).astype(f32), np.sin(angd).astype(f32)
    Cf = np.ones((S, 256), f32)
    Sa = np.zeros((S, 256), f32)
    Sb = np.zeros((S, 256), f32)
    for blk in range(4):
        o = blk * 64
        Cf[:, o:o + 8] = cd
        Cf[:, o + 8:o + 16] = cd
        Sa[:, o:o + 8] = -sd
        Sb[:, o + 8:o + 16] = sd
    ropeD = np.stack([Cf, Sa, Sb]).astype(f32)
    H = 8
    log_g = np.log1p(-np.exp2(-5.0 - np.arange(H, dtype=f32))).astype(f32)
    idx = np.arange(128, dtype=f32)
    rel = idx[:, None] - idx[None, :]
    intra = np.where(rel[None] >= 0, np.exp(log_g[:, None, None] * np.maximum(rel, 0.0)[None]), 0.0).astype(f32)
    intraT = np.ascontiguousarray(intra.transpose(2, 0, 1))
    kdec = np.ascontiguousarray(np.exp(log_g[:, None] * (127.0 - idx)[None, :]).astype(f32).T)
    qd = np.exp(log_g[:, None] * (idx + 1.0)[None, :]).astype(f32)
    qdec = np.ascontiguousarray(np.broadcast_to(np.tile(qd, (1, 4))[None], (128, 8, 512))).astype(f32)
    ident = np.eye(128, dtype=f32)
    tri = (idx[None, :] >= idx[:, None]).astype(f32)
    return dict(ropeR=ropeR, ropeD=ropeD, intraT=intraT, kdec=kdec, qdec=qdec, ident=ident, tri=tri)


def _prep_weights(inp, depth):
    f32 = np.float32
    cols = np.zeros((depth, 128, 80), f32)
    lruw = np.zeros((depth, 2, 8, 128, 128), f32)
    for l in range(depth):
        cols[l, :, 0:16] = inp["pre_norm"][l].reshape(16, 128).T
        cols[l, :, 16:48] = inp["conv_w"][l].reshape(4, 8, 128).transpose(2, 0, 1).reshape(128, 32)
        cols[l, :, 48:56] = inp["conv_b"][l].reshape(8, 128).T
        cols[l, :, 56:64] = inp["lru_ba"][l].reshape(8, 128).T
        cols[l, :, 64:72] = inp["lru_bx"][l].reshape(8, 128).T
        cols[l, :, 72:80] = inp["lru_lambda"][l].reshape(8, 128).T
        for wi, name in enumerate(("lru_wa", "lru_wx")):
            w = inp[name][l]
            for cc in range(8):
                for j in range(2):
                    lruw[l, wi, cc, j * 64:(j + 1) * 64, j * 64:(j + 1) * 64] = w[cc * 2 + j]
    postg = np.ascontiguousarray(np.broadcast_to(inp["post_norm"][:depth, None, :], (depth, 128, D))).astype(f32)
    subg = np.ascontiguousarray(np.broadcast_to(inp["diff_subln"][:depth, None, :], (depth, 128, 128))).astype(f32)
    dlam = np.ascontiguousarray(np.broadcast_to(inp["diff_lambda"][:depth].reshape(depth, 1, 256), (depth, 128, 256))).astype(f32)
    w_br = np.ascontiguousarray(np.stack([inp["w_branch_a"][:depth], inp["w_branch_b"][:depth], inp["w_branch_c"][:depth]], axis=1)).astype(f32)
    return dict(cols=cols, lruw=lruw, postg=postg, subg=subg, dlam=dlam, w_br=w_br,
                w_in=np.ascontiguousarray(inp["w_in"][:depth]), w_out=np.ascontiguousarray(inp["w_out"][:depth]))


def kernel(**inputs):
    x = np.asarray(inputs["x"], dtype=np.float32)
    B, S, _ = x.shape
    depth = inputs["w_in"].shape[0]
    nc = build(S, depth)
    shared = _prep_weights({k: np.asarray(v) for k, v in inputs.items()}, depth)
    shared.update(_const_tables(S))
    in_maps = []
    for b in range(B):
        m = dict(shared)
        m["x"] = np.ascontiguousarray(x[b])
        in_maps.append(m)
    res = run_bass_kernel_spmd(nc, in_maps, core_ids=list(range(B)))
    return np.stack([np.asarray(r["y"], dtype=np.float32) for r in res.results], axis=0)
```

```python
import math
from contextlib import ExitStack
import numpy as np
import concourse.bass as bass
import concourse.mybir as mybir
from concourse.bass_utils import run_bass_kernel_spmd

F32 = mybir.dt.float32
BF16 = mybir.dt.bfloat16
AF = mybir.ActivationFunctionType
ALU = mybir.AluOpType
AX = mybir.AxisListType

D = 2048
DIN = 16384
EPS = 1e-6
KC = 16


class Res:
    __slots__ = ("w", "rd")

    def __init__(self):
        self.w = None
        self.rd = {}


class TK:
    SEM_LIMIT = 8000

    def __init__(self, nc, es):
        self.nc = nc
        self.E = {"pe": nc.tensor, "act": nc.scalar, "dve": nc.vector, "pool": nc.gpsimd, "sp": nc.sync}
        self.es = es
        self.csem = {}
        self.ccnt = {}
        self.retired = []
        self.nsem = 0
        for e in ("pe", "act", "dve", "pool"):
            self.csem[e] = es.enter_context(nc.semaphore("c_" + e))
            self.ccnt[e] = 0
        self.dsem = {
            "sp": [es.enter_context(nc.semaphore("dsp%d" % i)) for i in range(32)],
            "pool": [es.enter_context(nc.semaphore("dpl%d" % i)) for i in range(16)],
            "act": [es.enter_context(nc.semaphore("dac%d" % i)) for i in range(2)],
        }
        self.didx = {"sp": 0, "pool": 0, "act": 0}
        self.dtot = {}
        self.waited = {e: {} for e in self.E}

    def _wait(self, eng, ev):
        sem, val, _ = ev
        k = id(sem)
        if self.waited[eng].get(k, 0) >= val:
            return
        self.E[eng].wait_ge(sem, val)
        self.waited[eng][k] = val

    def op(self, eng, fn, rd=(), wr=()):
        for r in rd:
            if r.w is not None and not (r.w[2] == "pe" and eng == "pe"):
                self._wait(eng, r.w)
        for w in wr:
            if w.w is not None and not (w.w[2] == "pe" and eng == "pe"):
                self._wait(eng, w.w)
            for ev in w.rd.values():
                self._wait(eng, ev)
        if self.ccnt[eng] >= self.SEM_LIMIT:
            self.retired.append((self.csem[eng], self.ccnt[eng], eng))
            self.nsem += 1
            self.csem[eng] = self.es.enter_context(self.nc.semaphore("c_%s_%d" % (eng, self.nsem)))
            self.ccnt[eng] = 0
        inst = fn(self.E[eng])
        self.ccnt[eng] += 1
        inst.then_inc(self.csem[eng], 1)
        ev = (self.csem[eng], self.ccnt[eng], eng)
        for r in rd:
            r.rd[eng] = ev
        for w in wr:
            w.w = ev
            w.rd = {}
        return ev

    def dma(self, q, out, in_, rd=(), wr=()):
        for r in rd:
            if r.w is not None:
                self._wait(q, r.w)
        for w in wr:
            if w.w is not None:
                self._wait(q, w.w)
            for ev in w.rd.values():
                self._wait(q, ev)
        pool = self.dsem[q]
        i = self.didx[q]
        self.didx[q] = (i + 1) % len(pool)
        sem = pool[i]
        tot = self.dtot.get(id(sem), 0)
        if tot > 0:
            self._wait(q, (sem, tot, "dma"))
        self.E[q].dma_start(out=out, in_=in_).then_inc(sem, 16)
        tot += 16
        self.dtot[id(sem)] = tot
        ev = (sem, tot, "dma")
        for r in rd:
            r.rd[("d", id(sem))] = ev
        for w in wr:
            w.w = ev
            w.rd = {}
        return ev

    def barrier(self):
        for e in self.E:
            for ev in self.retired:
                self._wait(e, ev)
            for pe, s in self.csem.items():
                if self.ccnt[pe] > 0:
                    self._wait(e, (s, self.ccnt[pe], pe))
            for q, pool in self.dsem.items():
                for s in pool:
                    t = self.dtot.get(id(s), 0)
                    if t > 0:
                        self._wait(e, (s, t, "dma"))


def build(S, depth, dbg=(), stop=99, kinds=None):
    NT = S // 128
    NB = S // 512
    PASS = min(S, 2048)
    NPASS = S // PASS
    PT = PASS // 128
    PB = PASS // 512
    nc = bass.Bass("TRN2", target_bir_lowering=False)

    def din(name, shape, dt=F32):
        return nc.dram_tensor(name, list(shape), dt, kind="ExternalInput").ap()

    def dscr(name, shape, dt=BF16):
        kind = "ExternalOutput" if name in dbg else "Internal"
        return nc.dram_tensor(name, list(shape), dt, kind=kind).ap()

    x_in = din("x", [S, D])
    w_in = din("w_in", [depth, D, DIN])
    w_br = din("w_br", [depth, 3, 1024, D])
    w_out = din("w_out", [depth, D, D])
    cols = din("cols", [depth, 128, 80])
    postg = din("postg", [depth, 128, D])
    subg = din("subg", [depth, 128, 128])
    dlam = din("dlam", [depth, 128, 256])
    lruw = din("lruw", [depth, 2, 8, 128, 128])
    ident = din("ident", [128, 128])
    tri = din("tri", [128, 128])
    ropeR = din("ropeR", [4, S, 128])
    ropeD = din("ropeD", [3, S, 256])
    intraT = din("intraT", [128, 8, 128])
    kdec = din("kdec", [128, 8])
    qdec = din("qdec", [128, 8, 512])
    y_out = nc.dram_tensor("y", [S, D], F32, kind="ExternalOutput").ap()
    xmid = dscr("xmid", [S, D], F32)
    xaT = dscr("xaT", [8, 128, S])
    gaT = dscr("gaT", [8, 128, S])
    gmT = dscr("gmT", [48, 128, S])
    QrT = dscr("QrT", [8, 128, S])
    KrT = dscr("KrT", [8, 128, S])
    Kr = dscr("Kr", [S, 1024])
    Vr = dscr("Vr", [S, 1024])
    Gr = dscr("Gr", [S, 1024])
    QdT = dscr("QdT", [8, 128, S])
    KdT = dscr("KdT", [8, 128, S])
    Vd = dscr("Vd", [S, 1024])
    Gd = dscr("Gd", [S, 1024])
    yT = dscr("yT", [3, 8, 128, S])
    mT = dscr("mT", [16, 128, S])

    with ExitStack() as es:
        tk = TK(nc, es)
        op = tk.op
        dma = tk.dma

        uid = [0]

        def sb(stack, name, shape, dt):
            uid[0] += 1
            return stack.enter_context(nc.sbuf_tensor("%s_u%d" % (name, uid[0]), list(shape), dt))

        psb = [es.enter_context(nc.psum_tensor("ps%d" % i, [128, 512], F32)) for i in range(8)]
        psr = [Res() for _ in range(8)]

        ident_f = sb(es, "ident_f", [128, 128], F32)
        ident_b = sb(es, "ident_b", [128, 128], BF16)
        tri_f = sb(es, "tri_f", [128, 128], F32)
        tri_b = sb(es, "tri_b", [128, 128], BF16)
        r_c = Res()
        dma("sp", ident_f[:], ident, wr=[r_c])
        dma("sp", tri_f[:], tri, wr=[r_c])
        op("dve", lambda e: e.tensor_copy(out=ident_b[:], in_=ident_f[:]), rd=[r_c], wr=[r_c])
        op("dve", lambda e: e.tensor_copy(out=tri_b[:], in_=tri_f[:]), rd=[r_c], wr=[r_c])
        tk.barrier()

        for l in range(depth):
            lam_init = 0.8 - 0.6 * math.exp(-0.3 * l)
            x_src = x_in if l == 0 else xmid
            x_dst = y_out if l == depth - 1 else xmid
            with ExitStack() as ls:
                colst = sb(ls, "colst", [128, 80], F32)
                s8 = sb(ls, "s8", [128, 8], F32)
                s16 = sb(ls, "s16", [128, 8], F32)
                tmp8 = sb(ls, "tmp8", [128, 8], F32)
                dl = sb(ls, "dl", [128, 256], F32)
                dprod = sb(ls, "dprod", [128, 128], F32)
                dsum = sb(ls, "dsum", [128, 4], F32)
                neglam = sb(ls, "neglam", [128, 1], F32)
                subgs = sb(ls, "subgs", [128, 128], F32)
                r_p = Res()
                dma("sp", colst[:], cols[l], wr=[r_p])
                dma("sp", dl[:], dlam[l], wr=[r_p])
                dma("sp", subgs[:], subg[l], wr=[r_p])
                op("act", lambda e: e.activation(out=tmp8[:], in_=colst[:, 72:80], func=AF.Exp, scale=-1.0), rd=[r_p], wr=[r_p])
                op("act", lambda e: e.activation(out=tmp8[:], in_=tmp8[:], func=AF.Ln, bias=1.0), rd=[r_p], wr=[r_p])
                op("dve", lambda e: e.tensor_scalar(out=s8[:], in0=tmp8[:], scalar1=-8.0, scalar2=None, op0=ALU.mult), rd=[r_p], wr=[r_p])
                op("dve", lambda e: e.tensor_scalar(out=s16[:], in0=tmp8[:], scalar1=-16.0, scalar2=None, op0=ALU.mult), rd=[r_p], wr=[r_p])
                op("dve", lambda e: e.tensor_tensor(out=dprod[:, 0:64], in0=dl[:, 0:64], in1=dl[:, 64:128], op=ALU.mult), rd=[r_p], wr=[r_p])
                op("dve", lambda e: e.tensor_tensor(out=dprod[:, 64:128], in0=dl[:, 128:192], in1=dl[:, 192:256], op=ALU.mult), rd=[r_p], wr=[r_p])
                op("dve", lambda e: e.tensor_reduce(out=dsum[:, 0:1], in_=dprod[:, 0:64], axis=AX.X, op=ALU.add), rd=[r_p], wr=[r_p])
                op("dve", lambda e: e.tensor_reduce(out=dsum[:, 1:2], in_=dprod[:, 64:128], axis=AX.X, op=ALU.add), rd=[r_p], wr=[r_p])
                op("act", lambda e: e.activation(out=dsum[:, 2:4], in_=dsum[:, 0:2], func=AF.Exp), rd=[r_p], wr=[r_p])
                op("dve", lambda e: e.tensor_tensor(out=neglam[:], in0=dsum[:, 3:4], in1=dsum[:, 2:3], op=ALU.subtract), rd=[r_p], wr=[r_p])
                op("dve", lambda e: e.tensor_scalar(out=neglam[:], in0=neglam[:], scalar1=-lam_init, scalar2=None, op0=ALU.add), rd=[r_p], wr=[r_p])
                op("dve", lambda e: e.tensor_scalar(out=subgs[:], in0=subgs[:], scalar1=1.0 - lam_init, scalar2=None, op0=ALU.mult), rd=[r_p], wr=[r_p])
                tk.barrier()

                for p in range(NPASS):
                    tok_p = p * PASS
                    with ExitStack() as s1:
                        hT = sb(s1, "hT", [128, KC, PASS], BF16)
                        r_hT = [Res() for _ in range(PT)]
                        with ExitStack() as s0:
                            xt = [sb(s0, "xt%d" % i, [128, D], F32) for i in range(2)]
                            xn = [sb(s0, "xn%d" % i, [128, D], BF16) for i in range(2)]
                            junk = sb(s0, "junk", [128, D], BF16)
                            st = [sb(s0, "st%d" % i, [128, 4], F32) for i in range(2)]
                            r_xt = [Res(), Res()]
                            r_xn = [Res(), Res()]
                            r_junk = Res()
                            r_st = [Res(), Res()]
                            dma("sp", xt[0][:], x_src[tok_p:tok_p + 128, :], wr=[r_xt[0]])
                            for tt in range(PT):
                                b = tt % 2
                                if tt + 1 < PT:
                                    t1 = tok_p + (tt + 1) * 128
                                    dma("sp", xt[1 - b][:], x_src[t1:t1 + 128, :], wr=[r_xt[1 - b]])
                                op("act", lambda e, b=b: e.activation(out=junk[:], in_=xt[b][:], func=AF.Square, accum_out=st[b][:, 0:1]),
                                   rd=[r_xt[b]], wr=[r_junk, r_st[b]])
                                op("act", lambda e, b=b: e.activation(out=st[b][:, 1:2], in_=st[b][:, 0:1], func=AF.Sqrt, scale=1.0 / D, bias=EPS),
                                   rd=[r_st[b]], wr=[r_st[b]])
                                op("dve", lambda e, b=b: e.reciprocal(out=st[b][:, 2:3], in_=st[b][:, 1:2]), rd=[r_st[b]], wr=[r_st[b]])
                                op("dve", lambda e, b=b: e.tensor_scalar(out=xn[b][:], in0=xt[b][:], scalar1=st[b][:, 2:3], scalar2=None, op0=ALU.mult),
                                   rd=[r_xt[b], r_st[b]], wr=[r_xn[b]])
                                for half in range(2):
                                    pbk = 6 + half
                                    ptv = psb[pbk][:].bitcast(BF16)
                                    for j in range(8):
                                        kc = half * 8 + j
                                        op("pe", lambda e, b=b, kc=kc, j=j, ptv=ptv: e.transpose(out=ptv[:, j * 128:(j + 1) * 128], in_=xn[b][:, kc * 128:(kc + 1) * 128], identity=ident_b[:]),
                                           rd=[r_xn[b]], wr=[psr[pbk]])
                                    src = ptv.rearrange("p (j t) -> p j t", j=8)
                                    dst = hT[:, half * 8:(half + 1) * 8, tt * 128:(tt + 1) * 128]
                                    if half == 0:
                                        op("act", lambda e, src=src, dst=dst: e.activation(out=dst, in_=src, func=AF.Copy), rd=[psr[pbk]], wr=[r_hT[tt]])
                                    else:
                                        op("dve", lambda e, src=src, dst=dst: e.tensor_copy(out=dst, in_=src), rd=[psr[pbk]], wr=[r_hT[tt]])
                            tk.barrier()
                            if stop == 1:
                                return nc
                        with ExitStack() as s2:
                            wst = [sb(s2, "wst%d" % i, [128, KC, 256], F32) for i in range(2)]
                            wbf = [sb(s2, "wbf%d" % i, [128, KC, 256], BF16) for i in range(2)]
                            r_wst = [Res(), Res()]
                            r_wbf = [Res(), Res()]
                            fst = [sb(s2, "fst%d" % i, [128, 512], BF16) for i in range(2)]
                            r_fst = [Res(), Res()]
                            tmb = [sb(s2, "tmb%d" % i, [128, 256], BF16) for i in range(2)]
                            r_tmb = [Res(), Res()]
                            ra = sb(s2, "ra", [128, 256], F32)
                            rb = sb(s2, "rb", [128, 256], F32)
                            r_ra = Res()
                            r_rb = Res()
                            tst = [sb(s2, "tst%d" % i, [128, 2, 512], BF16) for i in range(2)]
                            r_tst = [Res(), Res()]
                            rtab = sb(s2, "rtab", [128, 4, PT, 128], F32)
                            dtt = [sb(s2, "dtt%d" % i, [128, 3, 256], F32) for i in range(2)]
                            r_dtt = [Res(), Res()]
                            rc = sb(s2, "rc", [128, 256], F32)
                            r_rc = Res()
                            rbd = sb(s2, "rbd", [128, 256], F32)
                            r_rbd = Res()
                            op("pool", lambda e: e.memset(rbd[:], 0.0), wr=[r_rbd])
                            op("pool", lambda e: e.memset(rc[:], 0.0), wr=[r_rc])
                            r_tab = Res()
                            for i in range(4):
                                dma("sp", rtab[:, i, :, :], ropeR[i, tok_p:tok_p + PASS, :].rearrange("(n p) f -> p n f", p=128), wr=[r_tab])
                            groups = []
                            for g in range(8):
                                groups.append(("xa", g * 128, 128, g))
                            for kind, base in (("qr", 2048), ("kr", 3072), ("vr", 4096), ("qd", 6144), ("kd", 7168), ("vd", 8192)):
                                for g in range(4):
                                    groups.append((kind, base + g * 256, 256, g))
                            for g in range(8):
                                groups.append(("ga", 1024 + g * 128, 128, g))
                            for kind, base in (("gr", 5120), ("gd", 9216)):
                                for g in range(4):
                                    groups.append((kind, base + g * 256, 256, g))
                            for g in range(48):
                                groups.append(("gm", 10240 + g * 128, 128, g))
                            if kinds is not None:
                                groups = [g_ for g_ in groups if g_[0] in kinds]
                            wv = w_in[l].rearrange("(kc p) n -> p kc n", p=128)

                            def wload(gi):
                                _, c0, ncol, _ = groups[gi]
                                b = gi % 2
                                dma("sp", wst[b][:, :, 0:ncol], wv[:, :, c0:c0 + ncol], wr=[r_wst[b]])

                            def wcast(gi):
                                _, c0, ncol, _ = groups[gi]
                                b = gi % 2
                                for kc in range(KC):
                                    if kc % 2 == 0:
                                        op("dve", lambda e, b=b, kc=kc, ncol=ncol: e.tensor_scalar(out=wbf[b][:, kc, 0:ncol], in0=wst[b][:, kc, 0:ncol], scalar1=colst[:, kc:kc + 1], scalar2=None, op0=ALU.mult),
                                           rd=[r_wst[b]], wr=[r_wbf[b]])
                                    else:
                                        op("act", lambda e, b=b, kc=kc, ncol=ncol: e.activation(out=wbf[b][:, kc, 0:ncol], in_=wst[b][:, kc, 0:ncol], func=AF.Copy, scale=colst[:, kc:kc + 1]),
                                           rd=[r_wst[b]], wr=[r_wbf[b]])

                            cnt = {"ps": 0, "fst": 0, "tmb": 0, "tst": 0, "pt": 0, "dtt": 0}

                            def compute(gi):
                                kind, c0, ncol, g = groups[gi]
                                b = gi % 2
                                if ncol == 128:
                                    dst = {"xa": xaT, "ga": gaT, "gm": gmT}[kind]
                                    func = {"xa": AF.Copy, "ga": AF.Silu, "gm": AF.Sigmoid}[kind]
                                    for tb in range(PB):
                                        pk = cnt["ps"] % 6
                                        cnt["ps"] += 1
                                        for kc in range(KC):
                                            op("pe", lambda e, pk=pk, kc=kc, tb=tb, b=b: e.matmul(psb[pk][:, 0:512], lhsT=wbf[b][:, kc, 0:128], rhs=hT[:, kc, tb * 512:(tb + 1) * 512], start=(kc == 0), stop=(kc == KC - 1)),
                                               rd=[r_wbf[b]] + r_hT[tb * 4:tb * 4 + 4], wr=[psr[pk]])
                                        fb = cnt["fst"] % 2
                                        cnt["fst"] += 1
                                        op("act", lambda e, pk=pk, fb=fb, func=func: e.activation(out=fst[fb][:], in_=psb[pk][:, 0:512], func=func), rd=[psr[pk]], wr=[r_fst[fb]])
                                        t0 = tok_p + tb * 512
                                        dma("pool", dst[g, :, t0:t0 + 512], fst[fb][:], rd=[r_fst[fb]])
                                    return
                                trq = []
                                for tb in range(PB):
                                    need_t = kind in ("qr", "kr", "qd", "kd")
                                    if need_t:
                                        ptk = 6 + cnt["pt"] % 2
                                        cnt["pt"] += 1
                                        ptv = psb[ptk][:].bitcast(BF16).rearrange("p (h t d) -> p h t d", h=2, t=4)
                                    for t4 in range(4):
                                        tt = tb * 4 + t4
                                        tok0 = tok_p + tt * 128
                                        pk = cnt["ps"] % 6
                                        cnt["ps"] += 1
                                        for kc in range(KC):
                                            op("pe", lambda e, pk=pk, kc=kc, tt=tt, b=b: e.matmul(psb[pk][:, 0:256], lhsT=hT[:, kc, tt * 128:(tt + 1) * 128], rhs=wbf[b][:, kc, 0:256], start=(kc == 0), stop=(kc == KC - 1)),
                                               rd=[r_wbf[b], r_hT[tt]], wr=[psr[pk]])
                                        while trq:
                                            trq.pop(0)()
                                        mb = cnt["tmb"] % 2
                                        cnt["tmb"] += 1
                                        pv = psb[pk][:, 0:256]
                                        if kind in ("vr", "vd", "gr", "gd"):
                                            func = AF.Silu if kind[0] == "g" else AF.Copy
                                            op("act", lambda e, pv=pv, mb=mb, func=func: e.activation(out=tmb[mb][:], in_=pv, func=func), rd=[psr[pk]], wr=[r_tmb[mb]])
                                            dst = {"vr": Vr, "vd": Vd, "gr": Gr, "gd": Gd}[kind]
                                            dma("pool", dst[tok0:tok0 + 128, g * 256:(g + 1) * 256], tmb[mb][:], rd=[r_tmb[mb]])
                                            continue
                                        if kind in ("qr", "kr"):
                                            ti = 0 if kind == "qr" else 2
                                            for hh in range(2):
                                                hs = slice(hh * 128, (hh + 1) * 128)
                                                op("dve", lambda e, pv=pv, hs=hs, ti=ti, tt=tt: e.tensor_tensor(out=ra[:, hs], in0=pv[:, hs], in1=rtab[:, ti, tt, :], op=ALU.mult),
                                                   rd=[psr[pk], r_tab], wr=[r_ra])
                                                pvp = pv[:, hs].rearrange("p (a two) -> p a two", two=2)
                                                rbp = rb[:, hs].rearrange("p (a two) -> p a two", two=2)
                                                stp = rtab[:, ti + 1, tt, :].rearrange("p (a two) -> p a two", two=2)
                                                op("dve", lambda e, pvp=pvp, rbp=rbp, stp=stp: e.tensor_tensor(out=rbp[:, :, 0], in0=pvp[:, :, 1], in1=stp[:, :, 0], op=ALU.mult),
                                                   rd=[psr[pk], r_tab], wr=[r_rb])
                                                op("dve", lambda e, pvp=pvp, rbp=rbp, stp=stp: e.tensor_tensor(out=rbp[:, :, 1], in0=pvp[:, :, 0], in1=stp[:, :, 1], op=ALU.mult),
                                                   rd=[psr[pk], r_tab], wr=[r_rb])
                                            op("dve", lambda e, mb=mb: e.tensor_tensor(out=tmb[mb][:], in0=ra[:], in1=rb[:], op=ALU.add), rd=[r_ra, r_rb], wr=[r_tmb[mb]])
                                            if kind == "kr":
                                                dma("pool", Kr[tok0:tok0 + 128, g * 256:(g + 1) * 256], tmb[mb][:], rd=[r_tmb[mb]])
                                        else:
                                            db = cnt["dtt"] % 2
                                            cnt["dtt"] += 1
                                            dma("sp", dtt[db][:], ropeD[:, tok0:tok0 + 128, :].rearrange("i p f -> p i f"), wr=[r_dtt[db]])
                                            op("dve", lambda e, pv=pv, db=db: e.tensor_tensor(out=ra[:], in0=pv, in1=dtt[db][:, 0, :], op=ALU.mult),
                                               rd=[psr[pk], r_dtt[db]], wr=[r_ra])
                                            op("dve", lambda e, pv=pv, db=db: e.tensor_tensor(out=rbd[:, 0:248], in0=pv[:, 8:256], in1=dtt[db][:, 1, 0:248], op=ALU.mult),
                                               rd=[psr[pk], r_dtt[db]], wr=[r_rbd])
                                            op("dve", lambda e, pv=pv, db=db: e.tensor_tensor(out=rc[:, 8:256], in0=pv[:, 0:248], in1=dtt[db][:, 2, 8:256], op=ALU.mult),
                                               rd=[psr[pk], r_dtt[db]], wr=[r_rc])
                                            op("dve", lambda e: e.tensor_tensor(out=ra[:], in0=ra[:], in1=rbd[:], op=ALU.add), rd=[r_ra, r_rbd], wr=[r_ra])
                                            op("dve", lambda e, mb=mb: e.tensor_tensor(out=tmb[mb][:], in0=ra[:], in1=rc[:], op=ALU.add), rd=[r_ra, r_rc], wr=[r_tmb[mb]])
                                        def _tr(ptv=ptv, ptk=ptk, t4=t4, mb=mb):
                                            for hh in range(2):
                                                op("pe", lambda e, hh=hh: e.transpose(out=ptv[:, hh, t4, :], in_=tmb[mb][:, hh * 128:(hh + 1) * 128], identity=ident_b[:]),
                                                   rd=[r_tmb[mb]], wr=[psr[ptk]])
                                        trq.append(_tr)
                                    if need_t:
                                        def _ev(ptk=ptk, tb=tb, kind=kind, g=g):
                                            sbk = cnt["tst"] % 2
                                            cnt["tst"] += 1
                                            src = psb[ptk][:].bitcast(BF16).rearrange("p (h s) -> p h s", h=2)
                                            op("act", lambda e: e.activation(out=tst[sbk][:], in_=src, func=AF.Copy), rd=[psr[ptk]], wr=[r_tst[sbk]])
                                            dst = {"qr": QrT, "kr": KrT, "qd": QdT, "kd": KdT}[kind]
                                            t0 = tok_p + tb * 512
                                            dma("pool", dst[2 * g:2 * g + 2, :, t0:t0 + 512].rearrange("h d s -> d h s"), tst[sbk][:], rd=[r_tst[sbk]])
                                        trq.append(_ev)
                                while trq:
                                    trq.pop(0)()

                            ng = len(groups)
                            wload(0)
                            wload(1)
                            wcast(0)
                            for gi in range(ng):
                                if gi + 1 < ng:
                                    wcast(gi + 1)
                                compute(gi)
                                if gi + 2 < ng:
                                    wload(gi + 2)
                            tk.barrier()

                if stop == 2:
                    tk.barrier()
                    return nc
                with ExitStack() as s3:
                    xpad = [sb(s3, "xpad%d" % i, [128, S + 4], BF16) for i in range(2)]
                    sga = [sb(s3, "sga%d" % i, [128, S], BF16) for i in range(2)]
                    r_in = [Res(), Res()]
                    wlf = sb(s3, "wlf", [128, 2, 128], F32)
                    wlb = [sb(s3, "wlb%d" % i, [128, 2, 128], BF16) for i in range(2)]
                    r_wlf = Res()
                    r_wlb = [Res(), Res()]
                    dg = [sb(s3, "dg%d" % i, [128, 4, 128], BF16) for i in range(2)]
                    r_dg = [Res(), Res()]
                    xc = sb(s3, "xc", [128, S], BF16)
                    rr = sb(s3, "rr", [128, S], F32)
                    ii = sb(s3, "ii", [128, S], F32)
                    aa = sb(s3, "aa", [128, S], F32)
                    a2 = sb(s3, "a2", [128, S], F32)
                    ya = [sb(s3, "ya%d" % i, [128, S], BF16) for i in range(2)]
                    r_xc, r_rr, r_ii, r_aa, r_a2 = Res(), Res(), Res(), Res(), Res()
                    r_ya = [Res(), Res()]
                    for i in range(2):
                        op("pool", lambda e, i=i: e.memset(xpad[i][:, 0:4], 0.0), wr=[r_in[i]])

                    def lru_load(cc):
                        b = cc % 2
                        dma("sp", xpad[b][:, 3:3 + S], xaT[cc], wr=[r_in[b]])
                        dma("sp", sga[b][:], gaT[cc], wr=[r_in[b]])

                    lru_load(0)
                    for cc in range(8):
                        b = cc % 2
                        if cc + 1 < 8:
                            lru_load(cc + 1)
                        dma("sp", wlf[:, 0, :], lruw[l, 0, cc], wr=[r_wlf])
                        dma("sp", wlf[:, 1, :], lruw[l, 1, cc], wr=[r_wlf])
                        op("dve", lambda e, b=b: e.tensor_copy(out=wlb[b][:], in_=wlf[:]), rd=[r_wlf], wr=[r_wlb[b]])
                        for k in range(4):
                            op("dve", lambda e, b=b, k=k, cc=cc: e.tensor_scalar(out=dg[b][:, k, :], in0=ident_f[:], scalar1=colst[:, 16 + k * 8 + cc:17 + k * 8 + cc], scalar2=None, op0=ALU.mult),
                               wr=[r_dg[b]])
                        pc = 0
                        for tb in range(NB):
                            pk = pc % 6
                            pc += 1
                            for k in range(4):
                                op("pe", lambda e, pk=pk, k=k, tb=tb, b=b: e.matmul(psb[pk][:, 0:512], lhsT=dg[b][:, k, :], rhs=xpad[b][:, tb * 512 + k:tb * 512 + k + 512], start=(k == 0), stop=(k == 3)),
                                   rd=[r_dg[b], r_in[b]], wr=[psr[pk]])
                            op("act", lambda e, pk=pk, tb=tb, cc=cc: e.activation(out=xc[:, tb * 512:(tb + 1) * 512], in_=psb[pk][:, 0:512], func=AF.Identity, bias=colst[:, 48 + cc:49 + cc]),
                               rd=[psr[pk]], wr=[r_xc])
                        for tb in range(NB):
                            for gi_, (dstt, r_d, bo) in enumerate(((rr, r_rr, 56), (ii, r_ii, 64))):
                                pk = pc % 6
                                pc += 1
                                op("pe", lambda e, pk=pk, tb=tb, b=b, gi_=gi_: e.matmul(psb[pk][:, 0:512], lhsT=wlb[b][:, gi_, :], rhs=xc[:, tb * 512:(tb + 1) * 512], start=True, stop=True),
                                   rd=[r_wlb[b], r_xc], wr=[psr[pk]])
                                op("act", lambda e, pk=pk, tb=tb, dstt=dstt, bo=bo, cc=cc: e.activation(out=dstt[:, tb * 512:(tb + 1) * 512], in_=psb[pk][:, 0:512], func=AF.Sigmoid, bias=colst[:, bo + cc:bo + cc + 1]),
                                   rd=[psr[pk]], wr=[r_d])
                        op("act", lambda e, cc=cc: e.activation(out=aa[:], in_=rr[:], func=AF.Exp, scale=s8[:, cc:cc + 1]), rd=[r_rr], wr=[r_aa])
                        op("act", lambda e, cc=cc: e.activation(out=a2[:], in_=rr[:], func=AF.Exp, scale=s16[:, cc:cc + 1]), rd=[r_rr], wr=[r_a2])
                        op("dve", lambda e: e.tensor_scalar(out=a2[:], in0=a2[:], scalar1=-1.0, scalar2=1.0, op0=ALU.mult, op1=ALU.add), rd=[r_a2], wr=[r_a2])
                        op("act", lambda e: e.activation(out=a2[:], in_=a2[:], func=AF.Sqrt), rd=[r_a2], wr=[r_a2])
                        op("dve", lambda e: e.tensor_tensor(out=ii[:], in0=ii[:], in1=xc[:], op=ALU.mult), rd=[r_ii, r_xc], wr=[r_ii])
                        op("dve", lambda e: e.tensor_tensor(out=ii[:], in0=ii[:], in1=a2[:], op=ALU.mult), rd=[r_ii, r_a2], wr=[r_ii])
                        op("dve", lambda e: e.tensor_tensor_scan(out=rr[:], data0=aa[:], data1=ii[:], initial=0.0, op0=ALU.mult, op1=ALU.add), rd=[r_aa, r_ii], wr=[r_rr])
                        op("dve", lambda e, b=b: e.tensor_tensor(out=ya[b][:], in0=rr[:], in1=sga[b][:], op=ALU.mult), rd=[r_rr, r_in[b]], wr=[r_ya[b]])
                        dma("pool", yT[0, cc], ya[b][:], rd=[r_ya[b]])
                    tk.barrier()

                if stop == 3:
                    tk.barrier()
                    return nc
                with ExitStack() as s4:
                    itT = sb(s4, "itT", [128, 8, 128], F32)
                    kdt = sb(s4, "kdt", [128, 8], F32)
                    qdt = sb(s4, "qdt", [128, 8, 512], F32)
                    r_k = Res()
                    dma("sp", itT[:], intraT, wr=[r_k])
                    dma("sp", kdt[:], kdec, wr=[r_k])
                    dma("sp", qdt[:], qdec, wr=[r_k])
                    qT = [sb(s4, "qT%d" % i, [128, 8, 512], BF16) for i in range(2)]
                    kT = [sb(s4, "kT%d" % i, [128, 8, 512], BF16) for i in range(2)]
                    kM = [sb(s4, "kM%d" % i, [128, 4, 1024], BF16) for i in range(2)]
                    vM = [sb(s4, "vM%d" % i, [128, 4, 1024], BF16) for i in range(2)]
                    gM = [sb(s4, "gM%d" % i, [128, 4, 1024], BF16) for i in range(2)]
                    r_q = [Res(), Res()]
                    r_kt = [Res(), Res()]
                    r_km = [Res(), Res()]
                    r_vm = [Res(), Res()]
                    r_gm = [Res(), Res()]
                    qTd = sb(s4, "qTd", [128, 8, 512], BF16)
                    kMd = sb(s4, "kMd", [128, 4, 1024], BF16)
                    r_qTd, r_kMd = Res(), Res()
                    Sf = sb(s4, "Sf", [128, 8, 128], F32)
                    Sb = sb(s4, "Sb", [128, 8, 128], BF16)
                    r_Sf, r_Sb = Res(), Res()
                    sT = [sb(s4, "sT%d" % i, [128, 512], BF16) for i in range(2)]
                    r_sT = [Res(), Res()]
                    bst = sb(s4, "bst", [128, 8, 6], F32)
                    mv = sb(s4, "mv", [128, 8, 2], F32)
                    rs = sb(s4, "rs", [128, 8], F32)
                    r_bst, r_mv, r_rs = Res(), Res(), Res()
                    yn = sb(s4, "yn", [128, 1024], F32)
                    ybm = sb(s4, "ybm", [128, 1024], BF16)
                    r_yn, r_ybm = Res(), Res()
                    ybt = [sb(s4, "ybt%d" % i, [128, 8, 512], BF16) for i in range(2)]
                    r_ybt = [Res(), Res()]
                    op("dve", lambda e: e.memset(Sf[:], 0.0), wr=[r_Sf])
                    op("pool", lambda e: e.memset(Sb[:], 0.0), wr=[r_Sb])

                    def ret_load(tb):
                        b = tb % 2
                        t0 = tb * 512
                        dma("sp", qT[b][:], QrT[:, :, t0:t0 + 512].rearrange("h d s -> d h s"), wr=[r_q[b]])
                        dma("sp", kT[b][:], KrT[:, :, t0:t0 + 512].rearrange("h d s -> d h s"), wr=[r_kt[b]])
                        dma("sp", kM[b][:], Kr[t0:t0 + 512, :].rearrange("(n p) f -> p n f", p=128), wr=[r_km[b]])
                        dma("sp", vM[b][:], Vr[t0:t0 + 512, :].rearrange("(n p) f -> p n f", p=128), wr=[r_vm[b]])
                        dma("sp", gM[b][:], Gr[t0:t0 + 512, :].rearrange("(n p) f -> p n f", p=128), wr=[r_gm[b]])

                    ret_load(0)
                    pc = 0
                    for tb in range(NB):
                        b = tb % 2
                        if tb + 1 < NB:
                            ret_load(tb + 1)
                        op("dve", lambda e, b=b: e.tensor_tensor(out=qTd[:], in0=qT[b][:], in1=qdt[:], op=ALU.mult), rd=[r_q[b], r_k], wr=[r_qTd])
                        for h in range(8):
                            op("dve", lambda e, b=b, h=h: e.tensor_scalar(out=kMd[:, :, h * 128:(h + 1) * 128], in0=kM[b][:, :, h * 128:(h + 1) * 128], scalar1=kdt[:, h:h + 1], scalar2=None, op0=ALU.mult),
                               rd=[r_km[b], r_k], wr=[r_kMd])
                        for n in range(4):
                            cs = slice(n * 128, (n + 1) * 128)
                            obank = []
                            for hg in range(2):
                                pk = pc % 6
                                pc += 1
                                for h4 in range(4):
                                    h = hg * 4 + h4
                                    op("pe", lambda e, pk=pk, h4=h4, h=h, b=b, cs=cs: e.matmul(psb[pk][:, h4 * 128:(h4 + 1) * 128], lhsT=kT[b][:, h, cs], rhs=qT[b][:, h, cs], start=True, stop=True),
                                       rd=[r_kt[b], r_q[b]], wr=[psr[pk]])
                                sbk = (2 * n + hg) % 2
                                op("dve", lambda e, pk=pk, sbk=sbk, hg=hg: e.tensor_tensor(out=sT[sbk][:].rearrange("p (h i) -> p h i", h=4), in0=psb[pk][:, 0:512].rearrange("p (h i) -> p h i", h=4), in1=itT[:, hg * 4:(hg + 1) * 4, :], op=ALU.mult),
                                   rd=[psr[pk], r_k], wr=[r_sT[sbk]])
                                po = pc % 6
                                pc += 1
                                obank.append(po)
                                for h4 in range(4):
                                    h = hg * 4 + h4
                                    op("pe", lambda e, po=po, h4=h4, h=h, b=b, n=n, sbk=sbk: e.matmul(psb[po][:, h4 * 128:(h4 + 1) * 128], lhsT=sT[sbk][:, h4 * 128:(h4 + 1) * 128], rhs=vM[b][:, n, h * 128:(h + 1) * 128], start=True, stop=False),
                                       rd=[r_sT[sbk], r_vm[b]], wr=[psr[po]])
                                    op("pe", lambda e, po=po, h4=h4, h=h, cs=cs: e.matmul(psb[po][:, h4 * 128:(h4 + 1) * 128], lhsT=qTd[:, h, cs], rhs=Sb[:, h, :], start=False, stop=True),
                                       rd=[r_qTd, r_Sb], wr=[psr[po]])
                            for hg in range(2):
                                pk = pc % 6
                                pc += 1
                                for h4 in range(4):
                                    h = hg * 4 + h4
                                    op("pe", lambda e, pk=pk, h4=h4, h=h, b=b, n=n: e.matmul(psb[pk][:, h4 * 128:(h4 + 1) * 128], lhsT=kMd[:, n, h * 128:(h + 1) * 128], rhs=vM[b][:, n, h * 128:(h + 1) * 128], start=True, stop=True),
                                       rd=[r_kMd, r_vm[b]], wr=[psr[pk]])
                                for h4 in range(4):
                                    h = hg * 4 + h4
                                    cd = float(np.exp(np.float32(128.0) * np.log1p(-np.exp2(np.float32(-5.0 - h)))))
                                    op("dve", lambda e, pk=pk, h4=h4, h=h, cd=cd: e.scalar_tensor_tensor(out=Sf[:, h, :], in0=Sf[:, h, :], scalar=cd, in1=psb[pk][:, h4 * 128:(h4 + 1) * 128], op0=ALU.mult, op1=ALU.add),
                                       rd=[psr[pk], r_Sf], wr=[r_Sf])
                            op("act", lambda e: e.activation(out=Sb[:], in_=Sf[:], func=AF.Copy), rd=[r_Sf], wr=[r_Sb])
                            for hg in range(2):
                                po = obank[hg]
                                for h4 in range(4):
                                    h = hg * 4 + h4
                                    op("dve", lambda e, po=po, h4=h4, h=h: e.bn_stats(out=bst[:, h, :], in_=psb[po][:, h4 * 128:(h4 + 1) * 128]), rd=[psr[po]], wr=[r_bst])
                                    op("dve", lambda e, h=h: e.bn_aggr(out=mv[:, h, :], in_=bst[:, h, :]), rd=[r_bst], wr=[r_mv])
                            op("act", lambda e: e.activation(out=rs[:], in_=mv[:, :, 1], func=AF.Sqrt, bias=EPS), rd=[r_mv], wr=[r_rs])
                            op("dve", lambda e: e.reciprocal(out=rs[:], in_=rs[:]), rd=[r_rs], wr=[r_rs])
                            for hg in range(2):
                                po = obank[hg]
                                for h4 in range(4):
                                    h = hg * 4 + h4
                                    op("dve", lambda e, po=po, h4=h4, h=h: e.tensor_scalar(out=yn[:, h * 128:(h + 1) * 128], in0=psb[po][:, h4 * 128:(h4 + 1) * 128], scalar1=mv[:, h, 0:1], scalar2=rs[:, h:h + 1], op0=ALU.subtract, op1=ALU.mult),
                                       rd=[psr[po], r_mv, r_rs], wr=[r_yn])
                            op("dve", lambda e, b=b, n=n: e.tensor_tensor(out=ybm[:], in0=yn[:], in1=gM[b][:, n, :], op=ALU.mult), rd=[r_yn, r_gm[b]], wr=[r_ybm])
                            ptk = 6 + (tb * 4 + n) % 2
                            ptv = psb[ptk][:].bitcast(BF16).rearrange("p (h t) -> p h t", h=8)
                            for h in range(8):
                                op("pe", lambda e, ptv=ptv, h=h: e.transpose(out=ptv[:, h, :], in_=ybm[:, h * 128:(h + 1) * 128], identity=ident_b[:]), rd=[r_ybm], wr=[psr[ptk]])
                            op("act", lambda e, ptv=ptv, b=b, cs=cs: e.activation(out=ybt[b][:, :, cs], in_=ptv, func=AF.Copy), rd=[psr[ptk]], wr=[r_ybt[b]])
                        t0 = tb * 512
                        dma("pool", yT[1, :, :, t0:t0 + 512].rearrange("h d s -> d h s"), ybt[b][:], rd=[r_ybt[b]])
                    tk.barrier()

                if stop == 4:
                    tk.barrier()
                    return nc
                with ExitStack() as s5:
                    qh = [sb(s5, "qh%d" % i, [128, S], BF16) for i in range(2)]
                    kh = [sb(s5, "kh%d" % i, [128, S], BF16) for i in range(2)]
                    vh = [sb(s5, "vh%d" % i, [128, NT, 132], BF16) for i in range(2)]
                    gh = [sb(s5, "gh%d" % i, [128, NT, 128], BF16) for i in range(2)]
                    r_qh = [Res(), Res()]
                    r_kh = [Res(), Res()]
                    r_vh = [Res(), Res()]
                    r_gh = [Res(), Res()]
                    NE = 3
                    Eb = [[sb(s5, "E%d_%d" % (c, i), [128, 512], BF16) for i in range(NE)] for c in range(2)]
                    r_E = [[Res() for _ in range(NE)] for _ in range(2)]
                    accs = [sb(s5, "accs%d" % i, [128, 3, 512], F32) for i in range(2)]
                    r_accs = [Res(), Res()]
                    ogb = [sb(s5, "ogb%d" % i, [128, 4, 128], F32) for i in range(2)]
                    r_ogb = [Res(), Res()]
                    smb = [sb(s5, "smb%d" % i, [128, 16], F32) for i in range(2)]
                    r_smb = [Res(), Res()]
                    o1 = sb(s5, "o1", [128, 128], F32)
                    o2 = sb(s5, "o2", [128, 128], F32)
                    jk = sb(s5, "jk", [128, 128], F32)
                    r_o1, r_o2, r_jk = Res(), Res(), Res()
                    ycm = [sb(s5, "ycm%d" % i, [128, 4, 128], BF16) for i in range(2)]
                    r_ycm = [Res(), Res()]
                    yct = [sb(s5, "yct%d" % i, [128, 512], BF16) for i in range(2)]
                    r_yct = [Res(), Res()]
                    for i in range(2):
                        op("pool", lambda e, i=i: e.memset(vh[i][:, :, 128:129], 1.0), wr=[r_vh[i]])

                    def da_load(h):
                        b = h % 2
                        dma("sp", qh[b][:], QdT[h], wr=[r_qh[b]])
                        dma("sp", kh[b][:], KdT[h], wr=[r_kh[b]])
                        dma("sp", vh[b][:, :, 0:128], Vd[:, h * 128:(h + 1) * 128].rearrange("(n p) e -> p n e", p=128), wr=[r_vh[b]])
                        dma("sp", gh[b][:], Gd[:, h * 128:(h + 1) * 128].rearrange("(n p) e -> p n e", p=128), wr=[r_gh[b]])

                    def acc_ap(c, qs):
                        if qs < 3:
                            return c, psb[c][:, qs * 129:qs * 129 + 129]
                        return 2, psb[2][:, c * 129:c * 129 + 129]

                    def acc_sb(ab, c, qs):
                        if qs < 3:
                            return accs[ab][:, c, qs * 129:qs * 129 + 129]
                        return accs[ab][:, 2, c * 129:c * 129 + 129]

                    pending = []

                    def defer(n, fn):
                        pending.append([n, fn])

                    def tick():
                        for it in pending:
                            it[0] -= 1
                        while pending and pending[0][0] <= 0:
                            pending.pop(0)[1]()

                    def flush():
                        while pending:
                            pending.pop(0)[1]()

                    LA = 2
                    da_load(0)
                    da_load(1)
                    st_ = {"sc": 0, "ec": [0, 0], "blk": 0}
                    for h in range(8):
                        b = h % 2
                        for qb in range(NB):
                            if qb == min(1, NB - 1) and h >= 1 and h + 1 < 8:
                                da_load(h + 1)
                            nkt = 4 * qb + 4
                            steps = [(kt, c) for kt in range(nkt) for c in range(2)]
                            started = [False, False, False]
                            info = {}

                            def qk(i):
                                kt, c = steps[i]
                                r = kt - 4 * qb
                                c0 = r * 128 if r > 0 else 0
                                pk = 3 + st_["sc"] % 4
                                st_["sc"] += 1
                                ps_ = slice(c * 64, (c + 1) * 64)
                                op("pe", lambda e: e.matmul(psb[pk][:, c0:512], lhsT=kh[b][ps_, kt * 128:(kt + 1) * 128], rhs=qh[b][ps_, qb * 512 + c0:(qb + 1) * 512], start=True, stop=True),
                                   rd=[r_kh[b], r_qh[b]], wr=[psr[pk]])
                                ei = st_["ec"][c] % NE
                                st_["ec"][c] += 1
                                Et = Eb[c][ei]
                                rE = r_E[c][ei]
                                op("act", lambda e: e.activation(out=Et[:, c0:512], in_=psb[pk][:, c0:512], func=AF.Exp, scale=0.125), rd=[psr[pk]], wr=[rE])
                                if r >= 0:
                                    op("pool", lambda e: e.tensor_tensor(out=Et[:, c0:c0 + 128], in0=Et[:, c0:c0 + 128], in1=tri_b[:], op=ALU.mult), rd=[rE], wr=[rE])
                                info[i] = (Et, rE)

                            def pv(i):
                                kt, c = steps[i]
                                r = kt - 4 * qb
                                Et, rE = info[i]
                                for qs in range(max(r, 0), 4):
                                    bk, aap = acc_ap(c, qs)
                                    first = not started[bk]
                                    started[bk] = True
                                    last = (kt == 4 * qb + qs)
                                    op("pe", lambda e, aap=aap, qs=qs, first=first, last=last: e.matmul(aap, lhsT=Et[:, qs * 128:(qs + 1) * 128], rhs=vh[b][:, kt, 0:129], start=first, stop=last, skip_group_check=True),
                                       rd=[rE, r_vh[b]], wr=[psr[bk]])

                            for i in range(len(steps) + LA):
                                if i < len(steps):
                                    qk(i)
                                if i - LA >= 0:
                                    pv(i - LA)
                                tick()
                            ab = st_["blk"] % 2
                            st_["blk"] += 1
                            for bk, ncol in ((0, 387), (1, 387), (2, 258)):
                                op("dve", lambda e, bk=bk, ncol=ncol: e.tensor_copy(out=accs[ab][:, bk, 0:ncol], in_=psb[bk][:, 0:ncol]), rd=[psr[bk]], wr=[r_accs[ab]])
                            sview0 = accs[ab][:, 0, 0:387].rearrange("p (q e) -> p q e", e=129)[:, :, 128]
                            sview1 = accs[ab][:, 1, 0:387].rearrange("p (q e) -> p q e", e=129)[:, :, 128]
                            op("dve", lambda e: e.reciprocal(out=smb[ab][:, 0:3], in_=sview0), rd=[r_accs[ab]], wr=[r_smb[ab]])
                            op("dve", lambda e: e.reciprocal(out=smb[ab][:, 3:4], in_=accs[ab][:, 2, 128:129]), rd=[r_accs[ab]], wr=[r_smb[ab]])
                            op("dve", lambda e: e.reciprocal(out=smb[ab][:, 4:7], in_=sview1), rd=[r_accs[ab]], wr=[r_smb[ab]])
                            op("dve", lambda e: e.reciprocal(out=smb[ab][:, 7:8], in_=accs[ab][:, 2, 257:258]), rd=[r_accs[ab]], wr=[r_smb[ab]])
                            op("dve", lambda e: e.tensor_scalar(out=smb[ab][:, 4:8], in0=smb[ab][:, 4:8], scalar1=neglam[:, 0:1], scalar2=None, op0=ALU.mult), rd=[r_smb[ab]], wr=[r_smb[ab]])
                            for qs in range(4):
                                a0 = acc_sb(ab, 0, qs)
                                a1 = acc_sb(ab, 1, qs)
                                tile_i = qb * 4 + qs
                                op("dve", lambda e, a0=a0, qs=qs: e.tensor_scalar(out=o1[:], in0=a0[:, 0:128], scalar1=smb[ab][:, qs:qs + 1], scalar2=None, op0=ALU.mult), rd=[r_accs[ab], r_smb[ab]], wr=[r_o1])
                                op("dve", lambda e, a1=a1, qs=qs: e.scalar_tensor_tensor(out=o2[:], in0=a1[:, 0:128], scalar=smb[ab][:, 4 + qs:5 + qs], in1=o1[:], op0=ALU.mult, op1=ALU.add), rd=[r_accs[ab], r_smb[ab], r_o1], wr=[r_o2])
                                op("dve", lambda e, qs=qs: e.scalar_tensor_tensor(out=jk[:], in0=o2[:], scalar=1.0, in1=o2[:], op0=ALU.mult, op1=ALU.mult, accum_out=smb[ab][:, 8 + qs:9 + qs]), rd=[r_o2], wr=[r_jk, r_smb[ab]])
                                op("dve", lambda e, qs=qs, tile_i=tile_i: e.tensor_tensor(out=ogb[ab][:, qs, :], in0=o2[:], in1=gh[b][:, tile_i, :], op=ALU.mult), rd=[r_o2, r_gh[b]], wr=[r_ogb[ab]])
                                op("dve", lambda e, qs=qs: e.tensor_tensor(out=ogb[ab][:, qs, :], in0=ogb[ab][:, qs, :], in1=subgs[:], op=ALU.mult), rd=[r_ogb[ab]], wr=[r_ogb[ab]])

                            def stB(ab=ab):
                                op("act", lambda e: e.activation(out=smb[ab][:, 12:16], in_=smb[ab][:, 8:12], func=AF.Sqrt, scale=1.0 / 128, bias=EPS), rd=[r_smb[ab]], wr=[r_smb[ab]])

                            def stC(ab=ab):
                                op("dve", lambda e: e.reciprocal(out=smb[ab][:, 12:16], in_=smb[ab][:, 12:16]), rd=[r_smb[ab]], wr=[r_smb[ab]])
                                for qs in range(4):
                                    op("dve", lambda e, qs=qs: e.tensor_scalar(out=ycm[ab][:, qs, :], in0=ogb[ab][:, qs, :], scalar1=smb[ab][:, 12 + qs:13 + qs], scalar2=None, op0=ALU.mult), rd=[r_ogb[ab], r_smb[ab]], wr=[r_ycm[ab]])

                            def stT(ab=ab, h=h, qb=qb):
                                ptv = psb[7][:].bitcast(BF16)
                                for qs in range(4):
                                    op("pe", lambda e, qs=qs: e.transpose(out=ptv[:, qs * 128:(qs + 1) * 128], in_=ycm[ab][:, qs, :], identity=ident_b[:]), rd=[r_ycm[ab]], wr=[psr[7]])
                                op("dve", lambda e: e.tensor_copy(out=yct[ab][:], in_=ptv[:, 0:512]), rd=[psr[7]], wr=[r_yct[ab]])
                                dma("pool", yT[2, h, :, qb * 512:(qb + 1) * 512], yct[ab][:], rd=[r_yct[ab]])

                            defer(4, stB)
                            defer(5, stC)
                            defer(7, stT)
                    flush()
                    tk.barrier()

                if stop == 5:
                    tk.barrier()
                    return nc
                with ExitStack() as s6:
                    WB = sb(s6, "WB", [128, 3, 8, D], BF16)
                    r_WB = Res()
                    wsf = [sb(s6, "wsf%d" % i, [128, D], F32) for i in range(2)]
                    r_wsf = [Res(), Res()]
                    i_ = 0
                    for br in range(3):
                        for kc in range(8):
                            b = i_ % 2
                            dma("sp", wsf[b][:], w_br[l, br, kc * 128:(kc + 1) * 128, :], wr=[r_wsf[b]])
                            if i_ % 2 == 0:
                                op("dve", lambda e, b=b, br=br, kc=kc: e.tensor_copy(out=WB[:, br, kc, :], in_=wsf[b][:]), rd=[r_wsf[b]], wr=[r_WB])
                            else:
                                op("act", lambda e, b=b, br=br, kc=kc: e.activation(out=WB[:, br, kc, :], in_=wsf[b][:], func=AF.Copy), rd=[r_wsf[b]], wr=[r_WB])
                            i_ += 1
                    yTb = [sb(s6, "yTb%d" % i, [128, 3, 8, 512], BF16) for i in range(2)]
                    r_yTb = [Res(), Res()]
                    gmb = [sb(s6, "gmb%d" % i, [128, 3, 512], BF16) for i in range(2)]
                    r_gmb = [Res(), Res()]
                    t0b = sb(s6, "t0b", [128, 512], F32)
                    t1b = sb(s6, "t1b", [128, 512], F32)
                    t2b = sb(s6, "t2b", [128, 512], F32)
                    r_t0, r_t1, r_t2 = Res(), Res(), Res()
                    mst = [sb(s6, "mst%d" % i, [128, 512], BF16) for i in range(2)]
                    r_mst = [Res(), Res()]

                    def y_load(tb):
                        b = tb % 2
                        t0 = tb * 512
                        for br in range(3):
                            dma("sp", yTb[b][:, br, :, :], yT[br, :, :, t0:t0 + 512].rearrange("c p s -> p c s"), wr=[r_yTb[b]])

                    def gm_load(tb, dc, gi):
                        t0 = tb * 512
                        b = gi % 2
                        dma("sp", gmb[b][:], gmT[:, :, t0:t0 + 512].rearrange("(br dc) p s -> dc p br s", br=3)[dc], wr=[r_gmb[b]])

                    y_load(0)
                    gi = 0
                    gm_load(0, 0, 0)
                    pc = 0
                    for tb in range(NB):
                        b = tb % 2
                        if tb + 1 < NB:
                            y_load(tb + 1)
                        for dc in range(16):
                            nxt = tb * 16 + dc + 1
                            if nxt < NB * 16:
                                gm_load(nxt // 16, nxt % 16, gi + 1)
                            gb = gi % 2
                            gi += 1
                            pks = []
                            for br in range(3):
                                pk = pc % 8
                                pc += 1
                                pks.append(pk)
                                for kc in range(8):
                                    op("pe", lambda e, pk=pk, br=br, kc=kc, dc=dc, b=b: e.matmul(psb[pk][:, 0:512], lhsT=WB[:, br, kc, dc * 128:(dc + 1) * 128], rhs=yTb[b][:, br, kc, :], start=(kc == 0), stop=(kc == 7)),
                                       rd=[r_WB, r_yTb[b]], wr=[psr[pk]])
                            op("dve", lambda e, pk=pks[0], gb=gb: e.tensor_tensor(out=t0b[:], in0=psb[pk][:, 0:512], in1=gmb[gb][:, 0, :], op=ALU.mult), rd=[psr[pks[0]], r_gmb[gb]], wr=[r_t0])
                            op("dve", lambda e, pk=pks[1], gb=gb: e.tensor_tensor(out=t1b[:], in0=psb[pk][:, 0:512], in1=gmb[gb][:, 1, :], op=ALU.mult), rd=[psr[pks[1]], r_gmb[gb]], wr=[r_t1])
                            op("dve", lambda e, pk=pks[2], gb=gb: e.tensor_tensor(out=t2b[:], in0=psb[pk][:, 0:512], in1=gmb[gb][:, 2, :], op=ALU.mult), rd=[psr[pks[2]], r_gmb[gb]], wr=[r_t2])
                            op("pool", lambda e: e.tensor_tensor(out=t0b[:], in0=t0b[:], in1=t1b[:], op=ALU.add), rd=[r_t0, r_t1], wr=[r_t0])
                            mb = (tb * 16 + dc) % 2
                            op("pool", lambda e, mb=mb: e.tensor_tensor(out=mst[mb][:], in0=t0b[:], in1=t2b[:], op=ALU.add), rd=[r_t0, r_t2], wr=[r_mst[mb]])
                            dma("pool", mT[dc, :, tb * 512:(tb + 1) * 512], mst[mb][:], rd=[r_mst[mb]])
                    tk.barrier()

                if stop == 6:
                    tk.barrier()
                    return nc
                with ExitStack() as s7:
                    WO = sb(s7, "WO", [128, KC, D], BF16)
                    r_WO = Res()
                    wsf = [sb(s7, "wsf%d" % i, [128, D], F32) for i in range(2)]
                    r_wsf = [Res(), Res()]
                    for kc in range(KC):
                        b = kc % 2
                        dma("sp", wsf[b][:], w_out[l, kc * 128:(kc + 1) * 128, :], wr=[r_wsf[b]])
                        if kc % 2 == 0:
                            op("dve", lambda e, b=b, kc=kc: e.tensor_copy(out=WO[:, kc, :], in_=wsf[b][:]), rd=[r_wsf[b]], wr=[r_WO])
                        else:
                            op("act", lambda e, b=b, kc=kc: e.activation(out=WO[:, kc, :], in_=wsf[b][:], func=AF.Copy), rd=[r_wsf[b]], wr=[r_WO])
                    pg = sb(s7, "pg", [128, D], F32)
                    r_pg = Res()
                    dma("sp", pg[:], postg[l], wr=[r_pg])
                    mtb = [sb(s7, "mtb%d" % i, [128, KC, 128], BF16) for i in range(2)]
                    xr = [sb(s7, "xr%d" % i, [128, D], F32) for i in range(2)]
                    r_mtb = [Res(), Res()]
                    r_xr = [Res(), Res()]
                    ot = [sb(s7, "ot%d" % i, [128, D], F32) for i in range(2)]
                    r_ot = [Res(), Res()]
                    jq = sb(s7, "jq", [128, 512], BF16)
                    r_jq = Res()
                    q4 = [sb(s7, "q4_%d" % i, [128, 8], F32) for i in range(2)]
                    r_q4 = [Res(), Res()]

                    def o_load(tt):
                        b = tt % 2
                        t0 = tt * 128
                        dma("sp", mtb[b][:], mT[:, :, t0:t0 + 128].rearrange("c p s -> p c s"), wr=[r_mtb[b]])
                        dma("sp", xr[b][:], x_src[t0:t0 + 128, :], wr=[r_xr[b]])

                    o_load(0)
                    for tt in range(NT):
                        b = tt % 2
                        if tt + 1 < NT:
                            o_load(tt + 1)
                        base = 4 * (tt % 2)
                        for eb in range(4):
                            pk = base + eb
                            for dc in range(KC):
                                op("pe", lambda e, pk=pk, dc=dc, eb=eb, b=b: e.matmul(psb[pk][:, 0:512], lhsT=mtb[b][:, dc, :], rhs=WO[:, dc, eb * 512:(eb + 1) * 512], start=(dc == 0), stop=(dc == KC - 1)),
                                   rd=[r_mtb[b], r_WO], wr=[psr[pk]])
                            op("act", lambda e, pk=pk, eb=eb, b=b: e.activation(out=jq[:], in_=psb[pk][:, 0:512], func=AF.Square, accum_out=q4[b][:, eb:eb + 1]), rd=[psr[pk]], wr=[r_jq, r_q4[b]])
                        op("dve", lambda e, b=b: e.tensor_reduce(out=q4[b][:, 4:5], in_=q4[b][:, 0:4], axis=AX.X, op=ALU.add), rd=[r_q4[b]], wr=[r_q4[b]])
                        op("act", lambda e, b=b: e.activation(out=q4[b][:, 5:6], in_=q4[b][:, 4:5], func=AF.Sqrt, scale=1.0 / D, bias=EPS), rd=[r_q4[b]], wr=[r_q4[b]])
                        op("dve", lambda e, b=b: e.reciprocal(out=q4[b][:, 6:7], in_=q4[b][:, 5:6]), rd=[r_q4[b]], wr=[r_q4[b]])
                        for eb in range(4):
                            pk = base + eb
                            es_ = slice(eb * 512, (eb + 1) * 512)
                            op("dve", lambda e, pk=pk, es_=es_, b=b: e.scalar_tensor_tensor(out=ot[b][:, es_], in0=psb[pk][:, 0:512], scalar=q4[b][:, 6:7], in1=pg[:, es_], op0=ALU.mult, op1=ALU.mult),
                               rd=[psr[pk], r_q4[b], r_pg], wr=[r_ot[b]])
                        op("pool", lambda e, b=b: e.tensor_tensor(out=ot[b][:], in0=ot[b][:], in1=xr[b][:], op=ALU.add), rd=[r_ot[b], r_xr[b]], wr=[r_ot[b]])
                        dma("pool", x_dst[tt * 128:(tt + 1) * 128, :], ot[b][:], rd=[r_ot[b]])
                    tk.barrier()
        tk.barrier()
    return nc


def _const_tables(S):
    f32 = np.float32
    pos = np.arange(S, dtype=f32)
    ret_freq = (1.0 / (f32(10000.0) ** np.linspace(0.0, 1.0, 64, dtype=f32))).astype(f32)
    ang = (pos[:, None] * ret_freq[None, :]).astype(f32)
    c, s = np.cos(ang).astype(f32), np.sin(ang).astype(f32)
    Cq = np.repeat(c, 2, axis=1)
    Sq = np.stack([-s, s], axis=-1).reshape(S, 128)
    ks = f32(128.0 ** -0.5)
    ropeR = np.stack([Cq, Sq, Cq * ks, Sq * ks]).astype(f32)
    inv = (f32(500000.0) ** (-np.arange(0, 16, 2, dtype=f32) / f32(16))).astype(f32)
    angd = (pos[:, None] * inv[None, :]).astype(f32)
    cd, sd = np.cos(angd).astype(f32), np.sin(angd).astype(f32)
    Cf = np.ones((S, 256), f32)
    Sa = np.zeros((S, 256), f32)
    Sb = np.zeros((S, 256), f32)
    for blk in range(4):
        o = blk * 64
        Cf[:, o:o + 8] = cd
        Cf[:, o + 8:o + 16] = cd
        Sa[:, o:o + 8] = -sd
        Sb[:, o + 8:o + 16] = sd
    ropeD = np.stack([Cf, Sa, Sb]).astype(f32)
    H = 8
    log_g = np.log1p(-np.exp2(-5.0 - np.arange(H, dtype=f32))).astype(f32)
    idx = np.arange(128, dtype=f32)
    rel = idx[:, None] - idx[None, :]
    intra = np.where(rel[None] >= 0, np.exp(log_g[:, None, None] * np.maximum(rel, 0.0)[None]), 0.0).astype(f32)
    intraT = np.ascontiguousarray(intra.transpose(2, 0, 1))
    kdec = np.ascontiguousarray(np.exp(log_g[:, None] * (127.0 - idx)[None, :]).astype(f32).T)
    qd = np.exp(log_g[:, None] * (idx + 1.0)[None, :]).astype(f32)
    qdec = np.ascontiguousarray(np.broadcast_to(np.tile(qd, (1, 4))[None], (128, 8, 512))).astype(f32)
    ident = np.eye(128, dtype=f32)
    tri = (idx[None, :] >= idx[:, None]).astype(f32)
    return dict(ropeR=ropeR, ropeD=ropeD, intraT=intraT, kdec=kdec, qdec=qdec, ident=ident, tri=tri)


def _prep_weights(inp, depth):
    f32 = np.float32
    cols = np.zeros((depth, 128, 80), f32)
    lruw = np.zeros((depth, 2, 8, 128, 128), f32)
    for l in range(depth):
        cols[l, :, 0:16] = inp["pre_norm"][l].reshape(16, 128).T
        cols[l, :, 16:48] = inp["conv_w"][l].reshape(4, 8, 128).transpose(2, 0, 1).reshape(128, 32)
        cols[l, :, 48:56] = inp["conv_b"][l].reshape(8, 128).T
        cols[l, :, 56:64] = inp["lru_ba"][l].reshape(8, 128).T
        cols[l, :, 64:72] = inp["lru_bx"][l].reshape(8, 128).T
        cols[l, :, 72:80] = inp["lru_lambda"][l].reshape(8, 128).T
        for wi, name in enumerate(("lru_wa", "lru_wx")):
            w = inp[name][l]
            for cc in range(8):
                for j in range(2):
                    lruw[l, wi, cc, j * 64:(j + 1) * 64, j * 64:(j + 1) * 64] = w[cc * 2 + j]
    postg = np.ascontiguousarray(np.broadcast_to(inp["post_norm"][:depth, None, :], (depth, 128, D))).astype(f32)
    subg = np.ascontiguousarray(np.broadcast_to(inp["diff_subln"][:depth, None, :], (depth, 128, 128))).astype(f32)
    dlam = np.ascontiguousarray(np.broadcast_to(inp["diff_lambda"][:depth].reshape(depth, 1, 256), (depth, 128, 256))).astype(f32)
    w_br = np.ascontiguousarray(np.stack([inp["w_branch_a"][:depth], inp["w_branch_b"][:depth], inp["w_branch_c"][:depth]], axis=1)).astype(f32)
    return dict(cols=cols, lruw=lruw, postg=postg, subg=subg, dlam=dlam, w_br=w_br,
                w_in=np.ascontiguousarray(inp["w_in"][:depth]), w_out=np.ascontiguousarray(inp["w_out"][:depth]))


def kernel(**inputs):
    x = np.asarray(inputs["x"], dtype=np.float32)
    B, S, _ = x.shape
    depth = inputs["w_in"].shape[0]
    nc = build(S, depth)
    shared = _prep_weights({k: np.asarray(v) for k, v in inputs.items()}, depth)
    shared.update(_const_tables(S))
    in_maps = []
    for b in range(B):
        m = dict(shared)
        m["x"] = np.ascontiguousarray(x[b])
        in_maps.append(m)
    res = run_bass_kernel_spmd(nc, in_maps, core_ids=list(range(B)))
    return np.stack([np.asarray(r["y"], dtype=np.float32) for r in res.results], axis=0)
```

```python
import math
from contextlib import ExitStack
import numpy as np
import concourse.bass as bass
import concourse.mybir as mybir
from concourse.bass_utils import run_bass_kernel_spmd

F32 = mybir.dt.float32
BF16 = mybir.dt.bfloat16
AF = mybir.ActivationFunctionType
ALU = mybir.AluOpType
AX = mybir.AxisListType

D = 2048
DIN = 16384
EPS = 1e-6
KC = 16


class Res:
    __slots__ = ("w", "rd")

    def __init__(self):
        self.w = None
        self.rd = {}


class TK:
    SEM_LIMIT = 8000

    def __init__(self, nc, es):
        self.nc = nc
        self.E = {"pe": nc.tensor, "act": nc.scalar, "dve": nc.vector, "pool": nc.gpsimd, "sp": nc.sync}
        self.es = es
        self.csem = {}
        self.ccnt = {}
        self.retired = []
        self.nsem = 0
        for e in ("pe", "act", "dve", "pool"):
            self.csem[e] = es.enter_context(nc.semaphore("c_" + e))
            self.ccnt[e] = 0
        self.dsem = {
            "sp": [es.enter_context(nc.semaphore("dsp%d" % i)) for i in range(32)],
            "pool": [es.enter_context(nc.semaphore("dpl%d" % i)) for i in range(16)],
            "act": [es.enter_context(nc.semaphore("dac%d" % i)) for i in range(2)],
        }
        self.didx = {"sp": 0, "pool": 0, "act": 0}
        self.dtot = {}
        self.waited = {e: {} for e in self.E}

    def _wait(self, eng, ev):
        sem, val, _ = ev
        k = id(sem)
        if self.waited[eng].get(k, 0) >= val:
            return
        self.E[eng].wait_ge(sem, val)
        self.waited[eng][k] = val

    def op(self, eng, fn, rd=(), wr=()):
        for r in rd:
            if r.w is not None and not (r.w[2] == "pe" and eng == "pe"):
                self._wait(eng, r.w)
        for w in wr:
            if w.w is not None and not (w.w[2] == "pe" and eng == "pe"):
                self._wait(eng, w.w)
            for ev in w.rd.values():
                self._wait(eng, ev)
        if self.ccnt[eng] >= self.SEM_LIMIT:
            self.retired.append((self.csem[eng], self.ccnt[eng], eng))
            self.nsem += 1
            self.csem[eng] = self.es.enter_context(self.nc.semaphore("c_%s_%d" % (eng, self.nsem)))
            self.ccnt[eng] = 0
        inst = fn(self.E[eng])
        self.ccnt[eng] += 1
        inst.then_inc(self.csem[eng], 1)
        ev = (self.csem[eng], self.ccnt[eng], eng)
        for r in rd:
            r.rd[eng] = ev
        for w in wr:
            w.w = ev
            w.rd = {}
        return ev

    def dma(self, q, out, in_, rd=(), wr=()):
        for r in rd:
            if r.w is not None:
                self._wait(q, r.w)
        for w in wr:
            if w.w is not None:
                self._wait(q, w.w)
            for ev in w.rd.values():
                self._wait(q, ev)
        pool = self.dsem[q]
        i = self.didx[q]
        self.didx[q] = (i + 1) % len(pool)
        sem = pool[i]
        tot = self.dtot.get(id(sem), 0)
        if tot > 0:
            self._wait(q, (sem, tot, "dma"))
        self.E[q].dma_start(out=out, in_=in_).then_inc(sem, 16)
        tot += 16
        self.dtot[id(sem)] = tot
        ev = (sem, tot, "dma")
        for r in rd:
            r.rd[("d", id(sem))] = ev
        for w in wr:
            w.w = ev
            w.rd = {}
        return ev

    def barrier(self):
        for e in self.E:
            for ev in self.retired:
                self._wait(e, ev)
            for pe, s in self.csem.items():
                if self.ccnt[pe] > 0:
                    self._wait(e, (s, self.ccnt[pe], pe))
            for q, pool in self.dsem.items():
                for s in pool:
                    t = self.dtot.get(id(s), 0)
                    if t > 0:
                        self._wait(e, (s, t, "dma"))


def build(S, depth, dbg=(), stop=99, kinds=None):
    NT = S // 128
    NB = S // 512
    PASS = min(S, 2048)
    NPASS = S // PASS
    PT = PASS // 128
    PB = PASS // 512
    nc = bass.Bass("TRN2", target_bir_lowering=False)

    def din(name, shape, dt=F32):
        return nc.dram_tensor(name, list(shape), dt, kind="ExternalInput").ap()

    def dscr(name, shape, dt=BF16):
        kind = "ExternalOutput" if name in dbg else "Internal"
        return nc.dram_tensor(name, list(shape), dt, kind=kind).ap()

    x_in = din("x", [S, D])
    w_in = din("w_in", [depth, D, DIN])
    w_br = din("w_br", [depth, 3, 1024, D])
    w_out = din("w_out", [depth, D, D])
    cols = din("cols", [depth, 128, 80])
    postg = din("postg", [depth, 128, D])
    subg = din("subg", [depth, 128, 128])
    dlam = din("dlam", [depth, 128, 256])
    lruw = din("lruw", [depth, 2, 8, 128, 128])
    ident = din("ident", [128, 128])
    tri = din("tri", [128, 128])
    ropeR = din("ropeR", [4, S, 256])
    ropeD = din("ropeD", [3, S, 256])
    intraT = din("intraT", [128, 8, 128])
    kdec = din("kdec", [128, 8])
    qdec = din("qdec", [128, 8, 512])
    y_out = nc.dram_tensor("y", [S, D], F32, kind="ExternalOutput").ap()
    xmid = dscr("xmid", [S, D], F32)
    xaT = dscr("xaT", [8, 128, S])
    gaT = dscr("gaT", [8, 128, S])
    gmT = dscr("gmT", [48, 128, S])
    QrT = dscr("QrT", [8, 128, S])
    KrT = dscr("KrT", [8, 128, S])
    Kr = dscr("Kr", [S, 1024])
    Vr = dscr("Vr", [S, 1024])
    Gr = dscr("Gr", [S, 1024])
    QdT = dscr("QdT", [8, 128, S])
    KdT = dscr("KdT", [8, 128, S])
    Vd = dscr("Vd", [S, 1024])
    Gd = dscr("Gd", [S, 1024])
    yT = dscr("yT", [3, 8, 128, S])
    mT = dscr("mT", [16, 128, S])

    with ExitStack() as es:
        tk = TK(nc, es)
        op = tk.op
        dma = tk.dma

        uid = [0]

        def sb(stack, name, shape, dt):
            uid[0] += 1
            return stack.enter_context(nc.sbuf_tensor("%s_u%d" % (name, uid[0]), list(shape), dt))

        psb = [es.enter_context(nc.psum_tensor("ps%d" % i, [128, 512], F32)) for i in range(8)]
        psr = [Res() for _ in range(8)]

        ident_f = sb(es, "ident_f", [128, 128], F32)
        ident_b = sb(es, "ident_b", [128, 128], BF16)
        tri_f = sb(es, "tri_f", [128, 128], F32)
        tri_b = sb(es, "tri_b", [128, 128], BF16)
        r_c = Res()
        dma("sp", ident_f[:], ident, wr=[r_c])
        dma("sp", tri_f[:], tri, wr=[r_c])
        op("dve", lambda e: e.tensor_copy(out=ident_b[:], in_=ident_f[:]), rd=[r_c], wr=[r_c])
        op("dve", lambda e: e.tensor_copy(out=tri_b[:], in_=tri_f[:]), rd=[r_c], wr=[r_c])
        tk.barrier()

        for l in range(depth):
            lam_init = 0.8 - 0.6 * math.exp(-0.3 * l)
            x_src = x_in if l == 0 else xmid
            x_dst = y_out if l == depth - 1 else xmid
            with ExitStack() as ls:
                colst = sb(ls, "colst", [128, 80], F32)
                s8 = sb(ls, "s8", [128, 8], F32)
                s16 = sb(ls, "s16", [128, 8], F32)
                tmp8 = sb(ls, "tmp8", [128, 8], F32)
                dl = sb(ls, "dl", [128, 256], F32)
                dprod = sb(ls, "dprod", [128, 128], F32)
                dsum = sb(ls, "dsum", [128, 4], F32)
                neglam = sb(ls, "neglam", [128, 1], F32)
                subgs = sb(ls, "subgs", [128, 128], F32)
                r_p = Res()
                dma("sp", colst[:], cols[l], wr=[r_p])
                dma("sp", dl[:], dlam[l], wr=[r_p])
                dma("sp", subgs[:], subg[l], wr=[r_p])
                op("act", lambda e: e.activation(out=tmp8[:], in_=colst[:, 72:80], func=AF.Exp, scale=-1.0), rd=[r_p], wr=[r_p])
                op("act", lambda e: e.activation(out=tmp8[:], in_=tmp8[:], func=AF.Ln, bias=1.0), rd=[r_p], wr=[r_p])
                op("dve", lambda e: e.tensor_scalar(out=s8[:], in0=tmp8[:], scalar1=-8.0, scalar2=None, op0=ALU.mult), rd=[r_p], wr=[r_p])
                op("dve", lambda e: e.tensor_scalar(out=s16[:], in0=tmp8[:], scalar1=-16.0, scalar2=None, op0=ALU.mult), rd=[r_p], wr=[r_p])
                op("dve", lambda e: e.tensor_tensor(out=dprod[:, 0:64], in0=dl[:, 0:64], in1=dl[:, 64:128], op=ALU.mult), rd=[r_p], wr=[r_p])
                op("dve", lambda e: e.tensor_tensor(out=dprod[:, 64:128], in0=dl[:, 128:192], in1=dl[:, 192:256], op=ALU.mult), rd=[r_p], wr=[r_p])
                op("dve", lambda e: e.tensor_reduce(out=dsum[:, 0:1], in_=dprod[:, 0:64], axis=AX.X, op=ALU.add), rd=[r_p], wr=[r_p])
                op("dve", lambda e: e.tensor_reduce(out=dsum[:, 1:2], in_=dprod[:, 64:128], axis=AX.X, op=ALU.add), rd=[r_p], wr=[r_p])
                op("act", lambda e: e.activation(out=dsum[:, 2:4], in_=dsum[:, 0:2], func=AF.Exp), rd=[r_p], wr=[r_p])
                op("dve", lambda e: e.tensor_tensor(out=neglam[:], in0=dsum[:, 3:4], in1=dsum[:, 2:3], op=ALU.subtract), rd=[r_p], wr=[r_p])
                op("dve", lambda e: e.tensor_scalar(out=neglam[:], in0=neglam[:], scalar1=-lam_init, scalar2=None, op0=ALU.add), rd=[r_p], wr=[r_p])
                op("dve", lambda e: e.tensor_scalar(out=subgs[:], in0=subgs[:], scalar1=1.0 - lam_init, scalar2=None, op0=ALU.mult), rd=[r_p], wr=[r_p])
                tk.barrier()

                for p in range(NPASS):
                    tok_p = p * PASS
                    with ExitStack() as s1:
                        hT = sb(s1, "hT", [128, KC, PASS], BF16)
                        r_hT = [Res() for _ in range(PT)]
                        with ExitStack() as s0:
                            xt = [sb(s0, "xt%d" % i, [128, D], F32) for i in range(2)]
                            xn = [sb(s0, "xn%d" % i, [128, D], BF16) for i in range(2)]
                            junk = sb(s0, "junk", [128, D], BF16)
                            st = [sb(s0, "st%d" % i, [128, 4], F32) for i in range(2)]
                            r_xt = [Res(), Res()]
                            r_xn = [Res(), Res()]
                            r_junk = Res()
                            r_st = [Res(), Res()]
                            dma("sp", xt[0][:], x_src[tok_p:tok_p + 128, :], wr=[r_xt[0]])
                            for tt in range(PT):
                                b = tt % 2
                                if tt + 1 < PT:
                                    t1 = tok_p + (tt + 1) * 128
                                    dma("sp", xt[1 - b][:], x_src[t1:t1 + 128, :], wr=[r_xt[1 - b]])
                                op("act", lambda e, b=b: e.activation(out=junk[:], in_=xt[b][:], func=AF.Square, accum_out=st[b][:, 0:1]),
                                   rd=[r_xt[b]], wr=[r_junk, r_st[b]])
                                op("act", lambda e, b=b: e.activation(out=st[b][:, 1:2], in_=st[b][:, 0:1], func=AF.Sqrt, scale=1.0 / D, bias=EPS),
                                   rd=[r_st[b]], wr=[r_st[b]])
                                op("dve", lambda e, b=b: e.reciprocal(out=st[b][:, 2:3], in_=st[b][:, 1:2]), rd=[r_st[b]], wr=[r_st[b]])
                                op("dve", lambda e, b=b: e.tensor_scalar(out=xn[b][:], in0=xt[b][:], scalar1=st[b][:, 2:3], scalar2=None, op0=ALU.mult),
                                   rd=[r_xt[b], r_st[b]], wr=[r_xn[b]])
                                for half in range(2):
                                    pbk = 6 + half
                                    ptv = psb[pbk][:].bitcast(BF16)
                                    for j in range(8):
                                        kc = half * 8 + j
                                        op("pe", lambda e, b=b, kc=kc, j=j, ptv=ptv: e.transpose(out=ptv[:, j * 128:(j + 1) * 128], in_=xn[b][:, kc * 128:(kc + 1) * 128], identity=ident_b[:]),
                                           rd=[r_xn[b]], wr=[psr[pbk]])
                                    src = ptv.rearrange("p (j t) -> p j t", j=8)
                                    dst = hT[:, half * 8:(half + 1) * 8, tt * 128:(tt + 1) * 128]
                                    if half == 0:
                                        op("act", lambda e, src=src, dst=dst: e.activation(out=dst, in_=src, func=AF.Copy), rd=[psr[pbk]], wr=[r_hT[tt]])
                                    else:
                                        op("dve", lambda e, src=src, dst=dst: e.tensor_copy(out=dst, in_=src), rd=[psr[pbk]], wr=[r_hT[tt]])
                            tk.barrier()
                            if stop == 1:
                                return nc
                        with ExitStack() as s2:
                            wst = [sb(s2, "wst%d" % i, [128, KC, 256], F32) for i in range(2)]
                            wbf = [sb(s2, "wbf%d" % i, [128, KC, 256], BF16) for i in range(2)]
                            r_wst = [Res(), Res()]
                            r_wbf = [Res(), Res()]
                            fst = [sb(s2, "fst%d" % i, [128, 512], BF16) for i in range(2)]
                            r_fst = [Res(), Res()]
                            tmb = [sb(s2, "tmb%d" % i, [128, 256], BF16) for i in range(2)]
                            r_tmb = [Res(), Res()]
                            ra = sb(s2, "ra", [128, 256], F32)
                            rb = sb(s2, "rb", [128, 256], F32)
                            r_ra = Res()
                            r_rb = Res()
                            tst = [sb(s2, "tst%d" % i, [128, 2, 512], BF16) for i in range(2)]
                            r_tst = [Res(), Res()]
                            rtt = [sb(s2, "rtt%d" % i, [128, 2, 256], F32) for i in range(2)]
                            r_rtt = [Res(), Res()]
                            dtt = [sb(s2, "dtt%d" % i, [128, 3, 256], F32) for i in range(2)]
                            r_dtt = [Res(), Res()]
                            rc = sb(s2, "rc", [128, 256], F32)
                            r_rc = Res()
                            rbd = sb(s2, "rbd", [128, 256], F32)
                            r_rbd = Res()
                            op("pool", lambda e: e.memset(rbd[:], 0.0), wr=[r_rbd])
                            op("pool", lambda e: e.memset(rc[:], 0.0), wr=[r_rc])
                            r_tab = Res()
                            groups = []
                            for g in range(8):
                                groups.append(("xa", g * 128, 128, g))
                            for kind, base in (("qr", 2048), ("kr", 3072), ("vr", 4096), ("qd", 6144), ("kd", 7168), ("vd", 8192)):
                                for g in range(4):
                                    groups.append((kind, base + g * 256, 256, g))
                            for g in range(8):
                                groups.append(("ga", 1024 + g * 128, 128, g))
                            for kind, base in (("gr", 5120), ("gd", 9216)):
                                for g in range(4):
                                    groups.append((kind, base + g * 256, 256, g))
                            for g in range(48):
                                groups.append(("gm", 10240 + g * 128, 128, g))
                            if kinds is not None:
                                groups = [g_ for g_ in groups if g_[0] in kinds]
                            wv = w_in[l].rearrange("(kc p) n -> p kc n", p=128)

                            def wload(gi):
                                _, c0, ncol, _ = groups[gi]
                                b = gi % 2
                                dma("sp", wst[b][:, :, 0:ncol], wv[:, :, c0:c0 + ncol], wr=[r_wst[b]])

                            def wcast(gi):
                                _, c0, ncol, _ = groups[gi]
                                b = gi % 2
                                busy_dve = gi >= 1 and groups[gi - 1][0] in ("qr", "kr", "qd", "kd")
                                for kc in range(KC):
                                    if kc % 2 == 0 and not busy_dve:
                                        op("dve", lambda e, b=b, kc=kc, ncol=ncol: e.tensor_scalar(out=wbf[b][:, kc, 0:ncol], in0=wst[b][:, kc, 0:ncol], scalar1=colst[:, kc:kc + 1], scalar2=None, op0=ALU.mult),
                                           rd=[r_wst[b]], wr=[r_wbf[b]])
                                    else:
                                        op("act", lambda e, b=b, kc=kc, ncol=ncol: e.activation(out=wbf[b][:, kc, 0:ncol], in_=wst[b][:, kc, 0:ncol], func=AF.Copy, scale=colst[:, kc:kc + 1]),
                                           rd=[r_wst[b]], wr=[r_wbf[b]])

                            cnt = {"ps": 0, "fst": 0, "tmb": 0, "tst": 0, "pt": 0, "dtt": 0, "rtt": 0}

                            def compute(gi):
                                kind, c0, ncol, g = groups[gi]
                                b = gi % 2
                                if ncol == 128:
                                    dst = {"xa": xaT, "ga": gaT, "gm": gmT}[kind]
                                    func = {"xa": AF.Copy, "ga": AF.Silu, "gm": AF.Sigmoid}[kind]
                                    for tb in range(PB):
                                        pk = cnt["ps"] % 6
                                        cnt["ps"] += 1
                                        for kc in range(KC):
                                            op("pe", lambda e, pk=pk, kc=kc, tb=tb, b=b: e.matmul(psb[pk][:, 0:512], lhsT=wbf[b][:, kc, 0:128], rhs=hT[:, kc, tb * 512:(tb + 1) * 512], start=(kc == 0), stop=(kc == KC - 1)),
                                               rd=[r_wbf[b]] + r_hT[tb * 4:tb * 4 + 4], wr=[psr[pk]])
                                        fb = cnt["fst"] % 2
                                        cnt["fst"] += 1
                                        op("act", lambda e, pk=pk, fb=fb, func=func: e.activation(out=fst[fb][:], in_=psb[pk][:, 0:512], func=func), rd=[psr[pk]], wr=[r_fst[fb]])
                                        t0 = tok_p + tb * 512
                                        dma("pool", dst[g, :, t0:t0 + 512], fst[fb][:], rd=[r_fst[fb]])
                                    return
                                trq = []
                                for tb in range(PB):
                                    need_t = kind in ("qr", "kr", "qd", "kd")
                                    if need_t:
                                        ptk = 6 + cnt["pt"] % 2
                                        cnt["pt"] += 1
                                        ptv = psb[ptk][:].bitcast(BF16).rearrange("p (h t d) -> p h t d", h=2, t=4)
                                    for t4 in range(4):
                                        tt = tb * 4 + t4
                                        tok0 = tok_p + tt * 128
                                        pk = cnt["ps"] % 6
                                        cnt["ps"] += 1
                                        for kc in range(KC):
                                            op("pe", lambda e, pk=pk, kc=kc, tt=tt, b=b: e.matmul(psb[pk][:, 0:256], lhsT=hT[:, kc, tt * 128:(tt + 1) * 128], rhs=wbf[b][:, kc, 0:256], start=(kc == 0), stop=(kc == KC - 1)),
                                               rd=[r_wbf[b], r_hT[tt]], wr=[psr[pk]])
                                        while trq:
                                            trq.pop(0)()
                                        mb = cnt["tmb"] % 2
                                        cnt["tmb"] += 1
                                        pv = psb[pk][:, 0:256]
                                        if kind in ("vr", "vd", "gr", "gd"):
                                            func = AF.Silu if kind[0] == "g" else AF.Copy
                                            op("act", lambda e, pv=pv, mb=mb, func=func: e.activation(out=tmb[mb][:], in_=pv, func=func), rd=[psr[pk]], wr=[r_tmb[mb]])
                                            dst = {"vr": Vr, "vd": Vd, "gr": Gr, "gd": Gd}[kind]
                                            dma("pool", dst[tok0:tok0 + 128, g * 256:(g + 1) * 256], tmb[mb][:], rd=[r_tmb[mb]])
                                            continue
                                        if kind in ("qr", "kr"):
                                            ti = 0 if kind == "qr" else 2
                                            rbi = cnt["rtt"] % 2
                                            cnt["rtt"] += 1
                                            dma("sp", rtt[rbi][:], ropeR[ti:ti + 2, tok0:tok0 + 128, :].rearrange("i p f -> p i f"), wr=[r_rtt[rbi]])
                                            op("dve", lambda e, pv=pv, rbi=rbi: e.tensor_tensor(out=ra[:], in0=pv, in1=rtt[rbi][:, 0, :], op=ALU.mult),
                                               rd=[psr[pk], r_rtt[rbi]], wr=[r_ra])
                                            pvp = pv.rearrange("p (a two) -> p a two", two=2)
                                            rbp = rb[:].rearrange("p (a two) -> p a two", two=2)
                                            stp = rtt[rbi][:, 1, :].rearrange("p (a two) -> p a two", two=2)
                                            op("dve", lambda e, pvp=pvp, rbp=rbp, stp=stp: e.tensor_tensor(out=rbp[:, :, 0], in0=pvp[:, :, 1], in1=stp[:, :, 0], op=ALU.mult),
                                               rd=[psr[pk], r_rtt[rbi]], wr=[r_rb])
                                            op("dve", lambda e, pvp=pvp, rbp=rbp, stp=stp: e.tensor_tensor(out=rbp[:, :, 1], in0=pvp[:, :, 0], in1=stp[:, :, 1], op=ALU.mult),
                                               rd=[psr[pk], r_rtt[rbi]], wr=[r_rb])
                                            op("pool", lambda e, mb=mb: e.tensor_tensor(out=tmb[mb][:], in0=ra[:], in1=rb[:], op=ALU.add), rd=[r_ra, r_rb], wr=[r_tmb[mb]])
                                            if kind == "kr":
                                                dma("pool", Kr[tok0:tok0 + 128, g * 256:(g + 1) * 256], tmb[mb][:], rd=[r_tmb[mb]])
                                        else:
                                            db = cnt["dtt"] % 2
                                            cnt["dtt"] += 1
                                            dma("sp", dtt[db][:], ropeD[:, tok0:tok0 + 128, :].rearrange("i p f -> p i f"), wr=[r_dtt[db]])
                                            op("dve", lambda e, pv=pv, db=db: e.tensor_tensor(out=ra[:], in0=pv, in1=dtt[db][:, 0, :], op=ALU.mult),
                                               rd=[psr[pk], r_dtt[db]], wr=[r_ra])
                                            op("dve", lambda e, pv=pv, db=db: e.tensor_tensor(out=rbd[:, 0:248], in0=pv[:, 8:256], in1=dtt[db][:, 1, 0:248], op=ALU.mult),
                                               rd=[psr[pk], r_dtt[db]], wr=[r_rbd])
                                            op("dve", lambda e, pv=pv, db=db: e.tensor_tensor(out=rc[:, 8:256], in0=pv[:, 0:248], in1=dtt[db][:, 2, 8:256], op=ALU.mult),
                                               rd=[psr[pk], r_dtt[db]], wr=[r_rc])
                                            op("pool", lambda e: e.tensor_tensor(out=ra[:], in0=ra[:], in1=rbd[:], op=ALU.add), rd=[r_ra, r_rbd], wr=[r_ra])
                                            op("pool", lambda e, mb=mb: e.tensor_tensor(out=tmb[mb][:], in0=ra[:], in1=rc[:], op=ALU.add), rd=[r_ra, r_rc], wr=[r_tmb[mb]])
                                        def _tr(ptv=ptv, ptk=ptk, t4=t4, mb=mb):
                                            for hh in range(2):
                                                op("pe", lambda e, hh=hh: e.transpose(out=ptv[:, hh, t4, :], in_=tmb[mb][:, hh * 128:(hh + 1) * 128], identity=ident_b[:]),
                                                   rd=[r_tmb[mb]], wr=[psr[ptk]])
                                        trq.append(_tr)
                                    if need_t:
                                        def _ev(ptk=ptk, tb=tb, kind=kind, g=g):
                                            sbk = cnt["tst"] % 2
                                            cnt["tst"] += 1
                                            src = psb[ptk][:].bitcast(BF16).rearrange("p (h s) -> p h s", h=2)
                                            op("act", lambda e: e.activation(out=tst[sbk][:], in_=src, func=AF.Copy), rd=[psr[ptk]], wr=[r_tst[sbk]])
                                            dst = {"qr": QrT, "kr": KrT, "qd": QdT, "kd": KdT}[kind]
                                            t0 = tok_p + tb * 512
                                            dma("pool", dst[2 * g:2 * g + 2, :, t0:t0 + 512].rearrange("h d s -> d h s"), tst[sbk][:], rd=[r_tst[sbk]])
                                        trq.append(_ev)
                                while trq:
                                    trq.pop(0)()

                            ng = len(groups)
                            wload(0)
                            wload(1)
                            wcast(0)
                            for gi in range(ng):
                                if gi + 1 < ng:
                                    wcast(gi + 1)
                                compute(gi)
                                if gi + 2 < ng:
                                    wload(gi + 2)
                            tk.barrier()

                if stop == 2:
                    tk.barrier()
                    return nc
                with ExitStack() as s3:
                    HS = min(S, 2048)
                    NH = S // HS
                    HB = HS // 512
                    xpad = [sb(s3, "xpad%d" % i, [128, S + 4], BF16) for i in range(2)]
                    sga = [sb(s3, "sga%d" % i, [128, S], BF16) for i in range(2)]
                    r_in = [Res(), Res()]
                    wlf = sb(s3, "wlf", [128, 2, 128], F32)
                    wlb = [sb(s3, "wlb%d" % i, [128, 2, 128], BF16) for i in range(2)]
                    r_wlf = Res()
                    r_wlb = [Res(), Res()]
                    dg = [sb(s3, "dg%d" % i, [128, 4, 128], BF16) for i in range(2)]
                    r_dg = [Res(), Res()]
                    xc = [sb(s3, "xc%d" % i, [128, HS], BF16) for i in range(2)]
                    rr = [sb(s3, "rr%d" % i, [128, HS], F32) for i in range(2)]
                    ii = [sb(s3, "ii%d" % i, [128, HS], F32) for i in range(2)]
                    aa = [sb(s3, "aa%d" % i, [128, HS], F32) for i in range(2)]
                    a2 = [sb(s3, "a2%d" % i, [128, HS], F32) for i in range(2)]
                    ya = [sb(s3, "ya%d" % i, [128, S], BF16) for i in range(2)]
                    r_xc = [Res(), Res()]
                    r_rr = [Res(), Res()]
                    r_ii = [Res(), Res()]
                    r_aa = [Res(), Res()]
                    r_a2 = [Res(), Res()]
                    r_ya = [Res(), Res()]
                    for i in range(2):
                        op("pool", lambda e, i=i: e.memset(xpad[i][:, 0:4], 0.0), wr=[r_in[i]])

                    def lru_load(cc):
                        b = cc % 2
                        dma("sp", xpad[b][:, 3:3 + S], xaT[cc], wr=[r_in[b]])
                        dma("sp", sga[b][:], gaT[cc], wr=[r_in[b]])

                    lru_load(0)
                    pc = 0
                    u = 0
                    for cc in range(8):
                        b = cc % 2
                        if cc + 1 < 8:
                            lru_load(cc + 1)
                        dma("sp", wlf[:, 0, :], lruw[l, 0, cc], wr=[r_wlf])
                        dma("sp", wlf[:, 1, :], lruw[l, 1, cc], wr=[r_wlf])
                        op("pool", lambda e, b=b: e.tensor_copy(out=wlb[b][:], in_=wlf[:]), rd=[r_wlf], wr=[r_wlb[b]])
                        for k in range(4):
                            op("pool", lambda e, b=b, k=k, cc=cc: e.tensor_scalar(out=dg[b][:, k, :], in0=ident_f[:], scalar1=colst[:, 16 + k * 8 + cc:17 + k * 8 + cc], scalar2=1.0, op0=ALU.mult, op1=ALU.mult),
                               wr=[r_dg[b]])
                        for hf in range(NH):
                            ub = u % 2
                            u += 1
                            h0 = hf * HS
                            for tb in range(HB):
                                pk = pc % 6
                                pc += 1
                                t0 = h0 + tb * 512
                                for k in range(4):
                                    op("pe", lambda e, pk=pk, k=k, t0=t0, b=b: e.matmul(psb[pk][:, 0:512], lhsT=dg[b][:, k, :], rhs=xpad[b][:, t0 + k:t0 + k + 512], start=(k == 0), stop=(k == 3)),
                                       rd=[r_dg[b], r_in[b]], wr=[psr[pk]])
                                op("act", lambda e, pk=pk, tb=tb, cc=cc, ub=ub: e.activation(out=xc[ub][:, tb * 512:(tb + 1) * 512], in_=psb[pk][:, 0:512], func=AF.Identity, bias=colst[:, 48 + cc:49 + cc]),
                                   rd=[psr[pk]], wr=[r_xc[ub]])
                            for tb in range(HB):
                                for gi_, (dstt, r_d, bo) in enumerate(((rr[ub], r_rr[ub], 56), (ii[ub], r_ii[ub], 64))):
                                    pk = pc % 6
                                    pc += 1
                                    op("pe", lambda e, pk=pk, tb=tb, b=b, gi_=gi_, ub=ub: e.matmul(psb[pk][:, 0:512], lhsT=wlb[b][:, gi_, :], rhs=xc[ub][:, tb * 512:(tb + 1) * 512], start=True, stop=True),
                                       rd=[r_wlb[b], r_xc[ub]], wr=[psr[pk]])
                                    op("act", lambda e, pk=pk, tb=tb, dstt=dstt, bo=bo, cc=cc: e.activation(out=dstt[:, tb * 512:(tb + 1) * 512], in_=psb[pk][:, 0:512], func=AF.Sigmoid, bias=colst[:, bo + cc:bo + cc + 1]),
                                       rd=[psr[pk]], wr=[r_d])
                            op("act", lambda e, cc=cc, ub=ub: e.activation(out=aa[ub][:], in_=rr[ub][:], func=AF.Exp, scale=s8[:, cc:cc + 1]), rd=[r_rr[ub]], wr=[r_aa[ub]])
                            op("act", lambda e, cc=cc, ub=ub: e.activation(out=a2[ub][:], in_=rr[ub][:], func=AF.Exp, scale=s16[:, cc:cc + 1]), rd=[r_rr[ub]], wr=[r_a2[ub]])
                            op("act", lambda e, ub=ub: e.activation(out=a2[ub][:], in_=a2[ub][:], func=AF.Sqrt, scale=-1.0, bias=1.0), rd=[r_a2[ub]], wr=[r_a2[ub]])
                            op("dve", lambda e, ub=ub: e.tensor_tensor(out=ii[ub][:], in0=ii[ub][:], in1=xc[ub][:], op=ALU.mult), rd=[r_ii[ub], r_xc[ub]], wr=[r_ii[ub]])
                            op("dve", lambda e, ub=ub: e.tensor_tensor(out=ii[ub][:], in0=ii[ub][:], in1=a2[ub][:], op=ALU.mult), rd=[r_ii[ub], r_a2[ub]], wr=[r_ii[ub]])
                            if hf == 0:
                                op("dve", lambda e, ub=ub: e.tensor_tensor_scan(out=rr[ub][:], data0=aa[ub][:], data1=ii[ub][:], initial=0.0, op0=ALU.mult, op1=ALU.add), rd=[r_aa[ub], r_ii[ub]], wr=[r_rr[ub]])
                            else:
                                op("dve", lambda e, ub=ub: e.tensor_tensor_scan(out=rr[ub][:], data0=aa[ub][:], data1=ii[ub][:], initial=rr[1 - ub][:, HS - 1:HS], op0=ALU.mult, op1=ALU.add), rd=[r_aa[ub], r_ii[ub], r_rr[1 - ub]], wr=[r_rr[ub]])
                            op("dve", lambda e, b=b, ub=ub, h0=h0: e.tensor_tensor(out=ya[b][:, h0:h0 + HS], in0=rr[ub][:], in1=sga[b][:, h0:h0 + HS], op=ALU.mult), rd=[r_rr[ub], r_in[b]], wr=[r_ya[b]])
                        dma("pool", yT[0, cc], ya[b][:], rd=[r_ya[b]])
                    tk.barrier()

                if stop == 3:
                    tk.barrier()
                    return nc
                with ExitStack() as s4:
                    itT = sb(s4, "itT", [128, 8, 128], F32)
                    kdt = sb(s4, "kdt", [128, 8], F32)
                    qdt = sb(s4, "qdt", [128, 8, 512], F32)
                    r_k = Res()
                    dma("sp", itT[:], intraT, wr=[r_k])
                    dma("sp", kdt[:], kdec, wr=[r_k])
                    dma("sp", qdt[:], qdec, wr=[r_k])
                    qT = [sb(s4, "qT%d" % i, [128, 8, 512], BF16) for i in range(2)]
                    kT = [sb(s4, "kT%d" % i, [128, 8, 512], BF16) for i in range(2)]
                    kM = [sb(s4, "kM%d" % i, [128, 4, 1024], BF16) for i in range(2)]
                    vM = [sb(s4, "vM%d" % i, [128, 4, 1024], BF16) for i in range(2)]
                    gM = [sb(s4, "gM%d" % i, [128, 4, 1024], BF16) for i in range(2)]
                    r_q = [Res(), Res()]
                    r_kt = [Res(), Res()]
                    r_km = [Res(), Res()]
                    r_vm = [Res(), Res()]
                    r_gm = [Res(), Res()]
                    qTd = sb(s4, "qTd", [128, 8, 512], BF16)
                    kMd = sb(s4, "kMd", [128, 4, 1024], BF16)
                    r_qTd, r_kMd = Res(), Res()
                    Sf = sb(s4, "Sf", [128, 8, 128], F32)
                    Sb = sb(s4, "Sb", [128, 8, 128], BF16)
                    r_Sf, r_Sb = Res(), Res()
                    sT = [sb(s4, "sT%d" % i, [128, 512], BF16) for i in range(2)]
                    r_sT = [Res(), Res()]
                    bst = sb(s4, "bst", [128, 8, 6], F32)
                    mv = sb(s4, "mv", [128, 8, 2], F32)
                    rs = sb(s4, "rs", [128, 8], F32)
                    nbias = sb(s4, "nbias", [128, 8], F32)
                    r_nb = Res()
                    r_bst, r_mv, r_rs = Res(), Res(), Res()
                    yn = sb(s4, "yn", [128, 1024], F32)
                    ybm = sb(s4, "ybm", [128, 1024], BF16)
                    r_yn, r_ybm = Res(), Res()
                    ybt = [sb(s4, "ybt%d" % i, [128, 8, 512], BF16) for i in range(2)]
                    r_ybt = [Res(), Res()]
                    op("dve", lambda e: e.memset(Sf[:], 0.0), wr=[r_Sf])
                    op("pool", lambda e: e.memset(Sb[:], 0.0), wr=[r_Sb])

                    def ret_load(tb):
                        b = tb % 2
                        t0 = tb * 512
                        dma("sp", qT[b][:], QrT[:, :, t0:t0 + 512].rearrange("h d s -> d h s"), wr=[r_q[b]])
                        dma("sp", kT[b][:], KrT[:, :, t0:t0 + 512].rearrange("h d s -> d h s"), wr=[r_kt[b]])
                        dma("sp", kM[b][:], Kr[t0:t0 + 512, :].rearrange("(n p) f -> p n f", p=128), wr=[r_km[b]])
                        dma("sp", vM[b][:], Vr[t0:t0 + 512, :].rearrange("(n p) f -> p n f", p=128), wr=[r_vm[b]])
                        dma("sp", gM[b][:], Gr[t0:t0 + 512, :].rearrange("(n p) f -> p n f", p=128), wr=[r_gm[b]])

                    ret_load(0)
                    pc = 0
                    for tb in range(NB):
                        b = tb % 2
                        if tb + 1 < NB:
                            ret_load(tb + 1)
                        op("pool", lambda e, b=b: e.tensor_tensor(out=qTd[:], in0=qT[b][:], in1=qdt[:], op=ALU.mult), rd=[r_q[b], r_k], wr=[r_qTd])
                        for h in range(8):
                            op("pool", lambda e, b=b, h=h: e.tensor_scalar(out=kMd[:, :, h * 128:(h + 1) * 128], in0=kM[b][:, :, h * 128:(h + 1) * 128], scalar1=kdt[:, h:h + 1], scalar2=1.0, op0=ALU.mult, op1=ALU.mult),
                               rd=[r_km[b], r_k], wr=[r_kMd])
                        for n in range(4):
                            cs = slice(n * 128, (n + 1) * 128)
                            obank = []
                            for hg in range(2):
                                pk = pc % 6
                                pc += 1
                                for h4 in range(4):
                                    h = hg * 4 + h4
                                    op("pe", lambda e, pk=pk, h4=h4, h=h, b=b, cs=cs: e.matmul(psb[pk][:, h4 * 128:(h4 + 1) * 128], lhsT=kT[b][:, h, cs], rhs=qT[b][:, h, cs], start=True, stop=True),
                                       rd=[r_kt[b], r_q[b]], wr=[psr[pk]])
                                sbk = (2 * n + hg) % 2
                                op("dve", lambda e, pk=pk, sbk=sbk, hg=hg: e.tensor_tensor(out=sT[sbk][:].rearrange("p (h i) -> p h i", h=4), in0=psb[pk][:, 0:512].rearrange("p (h i) -> p h i", h=4), in1=itT[:, hg * 4:(hg + 1) * 4, :], op=ALU.mult),
                                   rd=[psr[pk], r_k], wr=[r_sT[sbk]])
                                po = pc % 6
                                pc += 1
                                obank.append(po)
                                for h4 in range(4):
                                    h = hg * 4 + h4
                                    op("pe", lambda e, po=po, h4=h4, h=h, b=b, n=n, sbk=sbk: e.matmul(psb[po][:, h4 * 128:(h4 + 1) * 128], lhsT=sT[sbk][:, h4 * 128:(h4 + 1) * 128], rhs=vM[b][:, n, h * 128:(h + 1) * 128], start=True, stop=False),
                                       rd=[r_sT[sbk], r_vm[b]], wr=[psr[po]])
                                    op("pe", lambda e, po=po, h4=h4, h=h, cs=cs: e.matmul(psb[po][:, h4 * 128:(h4 + 1) * 128], lhsT=qTd[:, h, cs], rhs=Sb[:, h, :], start=False, stop=True),
                                       rd=[r_qTd, r_Sb], wr=[psr[po]])
                            for hg in range(2):
                                pk = pc % 6
                                pc += 1
                                for h4 in range(4):
                                    h = hg * 4 + h4
                                    op("pe", lambda e, pk=pk, h4=h4, h=h, b=b, n=n: e.matmul(psb[pk][:, h4 * 128:(h4 + 1) * 128], lhsT=kMd[:, n, h * 128:(h + 1) * 128], rhs=vM[b][:, n, h * 128:(h + 1) * 128], start=True, stop=True),
                                       rd=[r_kMd, r_vm[b]], wr=[psr[pk]])
                                for h4 in range(4):
                                    h = hg * 4 + h4
                                    cd = float(np.exp(np.float32(128.0) * np.log1p(-np.exp2(np.float32(-5.0 - h)))))
                                    op("dve", lambda e, pk=pk, h4=h4, h=h, cd=cd: e.scalar_tensor_tensor(out=Sf[:, h, :], in0=Sf[:, h, :], scalar=cd, in1=psb[pk][:, h4 * 128:(h4 + 1) * 128], op0=ALU.mult, op1=ALU.add),
                                       rd=[psr[pk], r_Sf], wr=[r_Sf])
                            op("act", lambda e: e.activation(out=Sb[:], in_=Sf[:], func=AF.Copy), rd=[r_Sf], wr=[r_Sb])
                            for hg in range(2):
                                po = obank[hg]
                                for h4 in range(4):
                                    h = hg * 4 + h4
                                    op("dve", lambda e, po=po, h4=h4, h=h: e.bn_stats(out=bst[:, h, :], in_=psb[po][:, h4 * 128:(h4 + 1) * 128]), rd=[psr[po]], wr=[r_bst])
                                    op("dve", lambda e, h=h: e.bn_aggr(out=mv[:, h, :], in_=bst[:, h, :]), rd=[r_bst], wr=[r_mv])
                            op("act", lambda e: e.activation(out=rs[:], in_=mv[:, :, 1], func=AF.Ln, bias=EPS), rd=[r_mv], wr=[r_rs])
                            op("act", lambda e: e.activation(out=rs[:], in_=rs[:], func=AF.Exp, scale=-0.5), rd=[r_rs], wr=[r_rs])
                            op("dve", lambda e: e.scalar_tensor_tensor(out=nbias[:], in0=mv[:, :, 0], scalar=-1.0, in1=rs[:], op0=ALU.mult, op1=ALU.mult), rd=[r_mv, r_rs], wr=[r_nb])
                            for hg in range(2):
                                po = obank[hg]
                                for h4 in range(4):
                                    h = hg * 4 + h4
                                    op("act", lambda e, po=po, h4=h4, h=h: e.activation(out=yn[:, h * 128:(h + 1) * 128], in_=psb[po][:, h4 * 128:(h4 + 1) * 128], func=AF.Identity, scale=rs[:, h:h + 1], bias=nbias[:, h:h + 1]),
                                       rd=[psr[po], r_nb, r_rs], wr=[r_yn])
                            op("pool", lambda e, b=b, n=n: e.tensor_tensor(out=ybm[:], in0=yn[:], in1=gM[b][:, n, :], op=ALU.mult), rd=[r_yn, r_gm[b]], wr=[r_ybm])
                            ptk = 6 + (tb * 4 + n) % 2
                            ptv = psb[ptk][:].bitcast(BF16).rearrange("p (h t) -> p h t", h=8)
                            for h in range(8):
                                op("pe", lambda e, ptv=ptv, h=h: e.transpose(out=ptv[:, h, :], in_=ybm[:, h * 128:(h + 1) * 128], identity=ident_b[:]), rd=[r_ybm], wr=[psr[ptk]])
                            op("act", lambda e, ptv=ptv, b=b, cs=cs: e.activation(out=ybt[b][:, :, cs], in_=ptv, func=AF.Copy), rd=[psr[ptk]], wr=[r_ybt[b]])
                        t0 = tb * 512
                        dma("pool", yT[1, :, :, t0:t0 + 512].rearrange("h d s -> d h s"), ybt[b][:], rd=[r_ybt[b]])
                    tk.barrier()

                if stop == 4:
                    tk.barrier()
                    return nc
                with ExitStack() as s5:
                    qh = [sb(s5, "qh%d" % i, [128, S], BF16) for i in range(2)]
                    kh = [sb(s5, "kh%d" % i, [128, S], BF16) for i in range(2)]
                    vh = [sb(s5, "vh%d" % i, [128, NT, 132], BF16) for i in range(2)]
                    gh = [sb(s5, "gh%d" % i, [128, NT, 128], BF16) for i in range(2)]
                    r_qh = [Res(), Res()]
                    r_kh = [Res(), Res()]
                    r_vh = [Res(), Res()]
                    r_gh = [Res(), Res()]
                    NE = 4
                    Eb = [[sb(s5, "E%d_%d" % (c, i), [128, 512], BF16) for i in range(NE)] for c in range(2)]
                    r_E = [[Res() for _ in range(NE)] for _ in range(2)]
                    accs = [sb(s5, "accs%d" % i, [128, 3, 512], F32) for i in range(2)]
                    r_accs = [Res(), Res()]
                    ogb = [sb(s5, "ogb%d" % i, [128, 4, 128], F32) for i in range(2)]
                    r_ogb = [Res(), Res()]
                    smb = [sb(s5, "smb%d" % i, [128, 16], F32) for i in range(2)]
                    r_smb = [Res(), Res()]
                    o1 = sb(s5, "o1", [128, 128], F32)
                    o2 = sb(s5, "o2", [128, 128], F32)
                    jk = sb(s5, "jk", [128, 128], F32)
                    r_o1, r_o2, r_jk = Res(), Res(), Res()
                    ycm = [sb(s5, "ycm%d" % i, [128, 4, 128], BF16) for i in range(2)]
                    r_ycm = [Res(), Res()]
                    yct = [sb(s5, "yct%d" % i, [128, 512], BF16) for i in range(2)]
                    r_yct = [Res(), Res()]
                    for i in range(2):
                        op("pool", lambda e, i=i: e.memset(vh[i][:, :, 128:129], 1.0), wr=[r_vh[i]])

                    def da_load(h):
                        b = h % 2
                        dma("sp", qh[b][:], QdT[h], wr=[r_qh[b]])
                        dma("sp", kh[b][:], KdT[h], wr=[r_kh[b]])
                        dma("sp", vh[b][:, :, 0:128], Vd[:, h * 128:(h + 1) * 128].rearrange("(n p) e -> p n e", p=128), wr=[r_vh[b]])
                        dma("sp", gh[b][:], Gd[:, h * 128:(h + 1) * 128].rearrange("(n p) e -> p n e", p=128), wr=[r_gh[b]])

                    def acc_ap(c, qs):
                        if qs < 3:
                            return c, psb[c][:, qs * 129:qs * 129 + 129]
                        return 2, psb[2][:, c * 129:c * 129 + 129]

                    def acc_sb(ab, c, qs):
                        if qs < 3:
                            return accs[ab][:, c, qs * 129:qs * 129 + 129]
                        return accs[ab][:, 2, c * 129:c * 129 + 129]

                    pending = []

                    def defer(n, fn):
                        pending.append([n, fn])

                    def tick():
                        for it in pending:
                            it[0] -= 1
                        while pending and pending[0][0] <= 0:
                            pending.pop(0)[1]()

                    def flush():
                        while pending:
                            pending.pop(0)[1]()

                    LA = 2
                    da_load(0)
                    da_load(1)
                    st_ = {"sc": 0, "ec": [0, 0], "blk": 0}
                    for h in range(8):
                        b = h % 2
                        for qb in range(NB):
                            if qb == min(1, NB - 1) and h >= 1 and h + 1 < 8:
                                da_load(h + 1)
                            nkt = 4 * qb + 4
                            started = [False, False, False]
                            info = {}

                            def qk(kt):
                                r = kt - 4 * qb
                                c0 = r * 128 if r > 0 else 0
                                pks = []
                                for c in range(2):
                                    pk = 3 + st_["sc"] % 4
                                    st_["sc"] += 1
                                    pks.append(pk)
                                    ps_ = slice(c * 64, (c + 1) * 64)
                                    op("pe", lambda e, pk=pk, ps_=ps_: e.matmul(psb[pk][:, c0:512], lhsT=kh[b][ps_, kt * 128:(kt + 1) * 128], rhs=qh[b][ps_, qb * 512 + c0:(qb + 1) * 512], start=True, stop=True),
                                       rd=[r_kh[b], r_qh[b]], wr=[psr[pk]])
                                for c in range(2):
                                    pk = pks[c]
                                    ei = st_["ec"][c] % NE
                                    st_["ec"][c] += 1
                                    Et = Eb[c][ei]
                                    rE = r_E[c][ei]
                                    op("act", lambda e, pk=pk, Et=Et: e.activation(out=Et[:, c0:512], in_=psb[pk][:, c0:512], func=AF.Exp, scale=0.125), rd=[psr[pk]], wr=[rE])
                                    if r >= 0:
                                        op("pool", lambda e, Et=Et: e.tensor_tensor(out=Et[:, c0:c0 + 128], in0=Et[:, c0:c0 + 128], in1=tri_b[:], op=ALU.mult), rd=[rE], wr=[rE])
                                    info[(kt, c)] = (Et, rE)

                            def pv(kt):
                                r = kt - 4 * qb
                                for c in range(2):
                                    Et, rE = info[(kt, c)]
                                    for qs in range(max(r, 0), 4):
                                        bk, aap = acc_ap(c, qs)
                                        first = not started[bk]
                                        started[bk] = True
                                        last = (kt == 4 * qb + qs)
                                        op("pe", lambda e, aap=aap, qs=qs, first=first, last=last, Et=Et: e.matmul(aap, lhsT=Et[:, qs * 128:(qs + 1) * 128], rhs=vh[b][:, kt, 0:129], start=first, stop=last, skip_group_check=True),
                                           rd=[rE, r_vh[b]], wr=[psr[bk]])

                            for i in range(nkt + LA):
                                if i < nkt:
                                    qk(i)
                                if i - LA >= 0:
                                    pv(i - LA)
                                tick()
                                tick()
                            ab = st_["blk"] % 2
                            st_["blk"] += 1
                            for bk, ncol in ((0, 387), (1, 387), (2, 258)):
                                op("dve", lambda e, bk=bk, ncol=ncol: e.tensor_copy(out=accs[ab][:, bk, 0:ncol], in_=psb[bk][:, 0:ncol]), rd=[psr[bk]], wr=[r_accs[ab]])
                            sview0 = accs[ab][:, 0, 0:387].rearrange("p (q e) -> p q e", e=129)[:, :, 128]
                            sview1 = accs[ab][:, 1, 0:387].rearrange("p (q e) -> p q e", e=129)[:, :, 128]
                            op("dve", lambda e: e.reciprocal(out=smb[ab][:, 0:3], in_=sview0), rd=[r_accs[ab]], wr=[r_smb[ab]])
                            op("dve", lambda e: e.reciprocal(out=smb[ab][:, 3:4], in_=accs[ab][:, 2, 128:129]), rd=[r_accs[ab]], wr=[r_smb[ab]])
                            op("dve", lambda e: e.reciprocal(out=smb[ab][:, 4:7], in_=sview1), rd=[r_accs[ab]], wr=[r_smb[ab]])
                            op("dve", lambda e: e.reciprocal(out=smb[ab][:, 7:8], in_=accs[ab][:, 2, 257:258]), rd=[r_accs[ab]], wr=[r_smb[ab]])
                            op("dve", lambda e: e.tensor_scalar(out=smb[ab][:, 4:8], in0=smb[ab][:, 4:8], scalar1=neglam[:, 0:1], scalar2=None, op0=ALU.mult), rd=[r_smb[ab]], wr=[r_smb[ab]])
                            for qs in range(4):
                                a0 = acc_sb(ab, 0, qs)
                                a1 = acc_sb(ab, 1, qs)
                                tile_i = qb * 4 + qs
                                op("dve", lambda e, a0=a0, qs=qs: e.tensor_scalar(out=o1[:], in0=a0[:, 0:128], scalar1=smb[ab][:, qs:qs + 1], scalar2=None, op0=ALU.mult), rd=[r_accs[ab], r_smb[ab]], wr=[r_o1])
                                op("dve", lambda e, a1=a1, qs=qs: e.scalar_tensor_tensor(out=o2[:], in0=a1[:, 0:128], scalar=smb[ab][:, 4 + qs:5 + qs], in1=o1[:], op0=ALU.mult, op1=ALU.add), rd=[r_accs[ab], r_smb[ab], r_o1], wr=[r_o2])
                                op("dve", lambda e, qs=qs: e.scalar_tensor_tensor(out=jk[:], in0=o2[:], scalar=1.0, in1=o2[:], op0=ALU.mult, op1=ALU.mult, accum_out=smb[ab][:, 8 + qs:9 + qs]), rd=[r_o2], wr=[r_jk, r_smb[ab]])
                                op("dve", lambda e, qs=qs, tile_i=tile_i: e.tensor_tensor(out=ogb[ab][:, qs, :], in0=o2[:], in1=gh[b][:, tile_i, :], op=ALU.mult), rd=[r_o2, r_gh[b]], wr=[r_ogb[ab]])
                                op("dve", lambda e, qs=qs: e.tensor_tensor(out=ogb[ab][:, qs, :], in0=ogb[ab][:, qs, :], in1=subgs[:], op=ALU.mult), rd=[r_ogb[ab]], wr=[r_ogb[ab]])

                            def stB(ab=ab):
                                op("act", lambda e: e.activation(out=smb[ab][:, 12:16], in_=smb[ab][:, 8:12], func=AF.Ln, scale=1.0 / 128, bias=EPS), rd=[r_smb[ab]], wr=[r_smb[ab]])
                                op("act", lambda e: e.activation(out=smb[ab][:, 12:16], in_=smb[ab][:, 12:16], func=AF.Exp, scale=-0.5), rd=[r_smb[ab]], wr=[r_smb[ab]])

                            def stC(ab=ab):
                                for qs in range(4):
                                    op("dve", lambda e, qs=qs: e.tensor_scalar(out=ycm[ab][:, qs, :], in0=ogb[ab][:, qs, :], scalar1=smb[ab][:, 12 + qs:13 + qs], scalar2=None, op0=ALU.mult), rd=[r_ogb[ab], r_smb[ab]], wr=[r_ycm[ab]])

                            def stT(ab=ab, h=h, qb=qb):
                                ptv = psb[7][:].bitcast(BF16)
                                for qs in range(4):
                                    op("pe", lambda e, qs=qs: e.transpose(out=ptv[:, qs * 128:(qs + 1) * 128], in_=ycm[ab][:, qs, :], identity=ident_b[:]), rd=[r_ycm[ab]], wr=[psr[7]])
                                op("dve", lambda e: e.tensor_copy(out=yct[ab][:], in_=ptv[:, 0:512]), rd=[psr[7]], wr=[r_yct[ab]])
                                dma("pool", yT[2, h, :, qb * 512:(qb + 1) * 512], yct[ab][:], rd=[r_yct[ab]])

                            defer(4, stB)
                            defer(5, stC)
                            defer(7, stT)
                    flush()
                    tk.barrier()

                if stop == 5:
                    tk.barrier()
                    return nc
                with ExitStack() as s6:
                    WB = sb(s6, "WB", [128, 3, 8, D], BF16)
                    r_WB = Res()
                    wsf = [sb(s6, "wsf%d" % i, [128, D], F32) for i in range(2)]
                    r_wsf = [Res(), Res()]
                    i_ = 0
                    for br in range(3):
                        for kc in range(8):
                            b = i_ % 2
                            dma("sp", wsf[b][:], w_br[l, br, kc * 128:(kc + 1) * 128, :], wr=[r_wsf[b]])
                            if i_ % 2 == 0:
                                op("dve", lambda e, b=b, br=br, kc=kc: e.tensor_copy(out=WB[:, br, kc, :], in_=wsf[b][:]), rd=[r_wsf[b]], wr=[r_WB])
                            else:
                                op("act", lambda e, b=b, br=br, kc=kc: e.activation(out=WB[:, br, kc, :], in_=wsf[b][:], func=AF.Copy), rd=[r_wsf[b]], wr=[r_WB])
                            i_ += 1
                    yTb = [sb(s6, "yTb%d" % i, [128, 3, 8, 512], BF16) for i in range(2)]
                    r_yTb = [Res(), Res()]
                    gmb = [sb(s6, "gmb%d" % i, [128, 3, 512], BF16) for i in range(2)]
                    r_gmb = [Res(), Res()]
                    t0b = sb(s6, "t0b", [128, 512], F32)
                    t1b = sb(s6, "t1b", [128, 512], F32)
                    t2b = sb(s6, "t2b", [128, 512], F32)
                    r_t0, r_t1, r_t2 = Res(), Res(), Res()
                    mst = [sb(s6, "mst%d" % i, [128, 512], BF16) for i in range(2)]
                    r_mst = [Res(), Res()]

                    def y_load(tb):
                        b = tb % 2
                        t0 = tb * 512
                        for br in range(3):
                            dma("sp", yTb[b][:, br, :, :], yT[br, :, :, t0:t0 + 512].rearrange("c p s -> p c s"), wr=[r_yTb[b]])

                    def gm_load(tb, dc, gi):
                        t0 = tb * 512
                        b = gi % 2
                        dma("sp", gmb[b][:], gmT[:, :, t0:t0 + 512].rearrange("(br dc) p s -> dc p br s", br=3)[dc], wr=[r_gmb[b]])

                    y_load(0)
                    gi = 0
                    gm_load(0, 0, 0)
                    pc = 0
                    for tb in range(NB):
                        b = tb % 2
                        if tb + 1 < NB:
                            y_load(tb + 1)
                        for dc in range(16):
                            nxt = tb * 16 + dc + 1
                            if nxt < NB * 16:
                                gm_load(nxt // 16, nxt % 16, gi + 1)
                            gb = gi % 2
                            gi += 1
                            pks = []
                            for br in range(3):
                                pk = pc % 8
                                pc += 1
                                pks.append(pk)
                                for kc in range(8):
                                    op("pe", lambda e, pk=pk, br=br, kc=kc, dc=dc, b=b: e.matmul(psb[pk][:, 0:512], lhsT=WB[:, br, kc, dc * 128:(dc + 1) * 128], rhs=yTb[b][:, br, kc, :], start=(kc == 0), stop=(kc == 7)),
                                       rd=[r_WB, r_yTb[b]], wr=[psr[pk]])
                            op("dve", lambda e, pk=pks[0], gb=gb: e.tensor_tensor(out=t0b[:], in0=psb[pk][:, 0:512], in1=gmb[gb][:, 0, :], op=ALU.mult), rd=[psr[pks[0]], r_gmb[gb]], wr=[r_t0])
                            op("dve", lambda e, pk=pks[1], gb=gb: e.tensor_tensor(out=t1b[:], in0=psb[pk][:, 0:512], in1=gmb[gb][:, 1, :], op=ALU.mult), rd=[psr[pks[1]], r_gmb[gb]], wr=[r_t1])
                            op("dve", lambda e, pk=pks[2], gb=gb: e.tensor_tensor(out=t2b[:], in0=psb[pk][:, 0:512], in1=gmb[gb][:, 2, :], op=ALU.mult), rd=[psr[pks[2]], r_gmb[gb]], wr=[r_t2])
                            op("pool", lambda e: e.tensor_tensor(out=t0b[:], in0=t0b[:], in1=t1b[:], op=ALU.add), rd=[r_t0, r_t1], wr=[r_t0])
                            mb = (tb * 16 + dc) % 2
                            op("pool", lambda e, mb=mb: e.tensor_tensor(out=mst[mb][:], in0=t0b[:], in1=t2b[:], op=ALU.add), rd=[r_t0, r_t2], wr=[r_mst[mb]])
                            dma("pool", mT[dc, :, tb * 512:(tb + 1) * 512], mst[mb][:], rd=[r_mst[mb]])
                    tk.barrier()

                if stop == 6:
                    tk.barrier()
                    return nc
                with ExitStack() as s7:
                    WO = sb(s7, "WO", [128, KC, D], BF16)
                    r_WO = Res()
                    wsf = [sb(s7, "wsf%d" % i, [128, D], F32) for i in range(2)]
                    r_wsf = [Res(), Res()]
                    for kc in range(KC):
                        b = kc % 2
                        dma("sp", wsf[b][:], w_out[l, kc * 128:(kc + 1) * 128, :], wr=[r_wsf[b]])
                        if kc % 2 == 0:
                            op("dve", lambda e, b=b, kc=kc: e.tensor_copy(out=WO[:, kc, :], in_=wsf[b][:]), rd=[r_wsf[b]], wr=[r_WO])
                        else:
                            op("act", lambda e, b=b, kc=kc: e.activation(out=WO[:, kc, :], in_=wsf[b][:], func=AF.Copy), rd=[r_wsf[b]], wr=[r_WO])
                    pg = sb(s7, "pg", [128, D], F32)
                    r_pg = Res()
                    dma("sp", pg[:], postg[l], wr=[r_pg])
                    mtb = [sb(s7, "mtb%d" % i, [128, KC, 128], BF16) for i in range(2)]
                    xr = [sb(s7, "xr%d" % i, [128, D], F32) for i in range(2)]
                    r_mtb = [Res(), Res()]
                    r_xr = [Res(), Res()]
                    ot = [sb(s7, "ot%d" % i, [128, D], F32) for i in range(2)]
                    r_ot = [Res(), Res()]
                    jq = sb(s7, "jq", [128, 512], BF16)
                    r_jq = Res()
                    q4 = [sb(s7, "q4_%d" % i, [128, 8], F32) for i in range(2)]
                    r_q4 = [Res(), Res()]

                    def o_load(tt):
                        b = tt % 2
                        t0 = tt * 128
                        dma("sp", mtb[b][:], mT[:, :, t0:t0 + 128].rearrange("c p s -> p c s"), wr=[r_mtb[b]])
                        dma("sp", xr[b][:], x_src[t0:t0 + 128, :], wr=[r_xr[b]])

                    o_load(0)
                    for tt in range(NT):
                        b = tt % 2
                        if tt + 1 < NT:
                            o_load(tt + 1)
                        base = 4 * (tt % 2)
                        for eb in range(4):
                            pk = base + eb
                            for dc in range(KC):
                                op("pe", lambda e, pk=pk, dc=dc, eb=eb, b=b: e.matmul(psb[pk][:, 0:512], lhsT=mtb[b][:, dc, :], rhs=WO[:, dc, eb * 512:(eb + 1) * 512], start=(dc == 0), stop=(dc == KC - 1)),
                                   rd=[r_mtb[b], r_WO], wr=[psr[pk]])
                            op("act", lambda e, pk=pk, eb=eb, b=b: e.activation(out=jq[:], in_=psb[pk][:, 0:512], func=AF.Square, accum_out=q4[b][:, eb:eb + 1]), rd=[psr[pk]], wr=[r_jq, r_q4[b]])
                        op("dve", lambda e, b=b: e.tensor_reduce(out=q4[b][:, 4:5], in_=q4[b][:, 0:4], axis=AX.X, op=ALU.add), rd=[r_q4[b]], wr=[r_q4[b]])
                        op("act", lambda e, b=b: e.activation(out=q4[b][:, 5:6], in_=q4[b][:, 4:5], func=AF.Sqrt, scale=1.0 / D, bias=EPS), rd=[r_q4[b]], wr=[r_q4[b]])
                        op("dve", lambda e, b=b: e.reciprocal(out=q4[b][:, 6:7], in_=q4[b][:, 5:6]), rd=[r_q4[b]], wr=[r_q4[b]])
                        for eb in range(4):
                            pk = base + eb
                            es_ = slice(eb * 512, (eb + 1) * 512)
                            op("dve", lambda e, pk=pk, es_=es_, b=b: e.scalar_tensor_tensor(out=ot[b][:, es_], in0=psb[pk][:, 0:512], scalar=q4[b][:, 6:7], in1=pg[:, es_], op0=ALU.mult, op1=ALU.mult),
                               rd=[psr[pk], r_q4[b], r_pg], wr=[r_ot[b]])
                        op("pool", lambda e, b=b: e.tensor_tensor(out=ot[b][:], in0=ot[b][:], in1=xr[b][:], op=ALU.add), rd=[r_ot[b], r_xr[b]], wr=[r_ot[b]])
                        dma("pool", x_dst[tt * 128:(tt + 1) * 128, :], ot[b][:], rd=[r_ot[b]])
                    tk.barrier()
        tk.barrier()
    return nc


def _const_tables(S):
    f32 = np.float32
    pos = np.arange(S, dtype=f32)
    ret_freq = (1.0 / (f32(10000.0) ** np.linspace(0.0, 1.0, 64, dtype=f32))).astype(f32)
    ang = (pos[:, None] * ret_freq[None, :]).astype(f32)
    c, s = np.cos(ang).astype(f32), np.sin(ang).astype(f32)
    Cq = np.repeat(c, 2, axis=1)
    Sq = np.stack([-s, s], axis=-1).reshape(S, 128)
    ks = f32(128.0 ** -0.5)
    ropeR = np.tile(np.stack([Cq, Sq, Cq * ks, Sq * ks]).astype(f32), (1, 1, 2))
    inv = (f32(500000.0) ** (-np.arange(0, 16, 2, dtype=f32) / f32(16))).astype(f32)
    angd = (pos[:, None] * inv[None, :]).astype(f32)
    cd, sd = np.cos(angd).astype(f32), np.sin(angd).astype(f32)
    Cf = np.ones((S, 256), f32)
    Sa = np.zeros((S, 256), f32)
    Sb = np.zeros((S, 256), f32)
    for blk in range(4):
        o = blk * 64
        Cf[:, o:o + 8] = cd
        Cf[:, o + 8:o + 16] = cd
        Sa[:, o:o + 8] = -sd
        Sb[:, o + 8:o + 16] = sd
    ropeD = np.stack([Cf, Sa, Sb]).astype(f32)
    H = 8
    log_g = np.log1p(-np.exp2(-5.0 - np.arange(H, dtype=f32))).astype(f32)
    idx = np.arange(128, dtype=f32)
    rel = idx[:, None] - idx[None, :]
    intra = np.where(rel[None] >= 0, np.exp(log_g[:, None, None] * np.maximum(rel, 0.0)[None]), 0.0).astype(f32)
    intraT = np.ascontiguousarray(intra.transpose(2, 0, 1))
    kdec = np.ascontiguousarray(np.exp(log_g[:, None] * (127.0 - idx)[None, :]).astype(f32).T)
    qd = np.exp(log_g[:, None] * (idx + 1.0)[None, :]).astype(f32)
    qdec = np.ascontiguousarray(np.broadcast_to(np.tile(qd, (1, 4))[None], (128, 8, 512))).astype(f32)
    ident = np.eye(128, dtype=f32)
    tri = (idx[None, :] >= idx[:, None]).astype(f32)
    return dict(ropeR=ropeR, ropeD=ropeD, intraT=intraT, kdec=kdec, qdec=qdec, ident=ident, tri=tri)


def _prep_weights(inp, depth):
    f32 = np.float32
    cols = np.zeros((depth, 128, 80), f32)
    lruw = np.zeros((depth, 2, 8, 128, 128), f32)
    for l in range(depth):
        cols[l, :, 0:16] = inp["pre_norm"][l].reshape(16, 128).T
        cols[l, :, 16:48] = inp["conv_w"][l].reshape(4, 8, 128).transpose(2, 0, 1).reshape(128, 32)
        cols[l, :, 48:56] = inp["conv_b"][l].reshape(8, 128).T
        cols[l, :, 56:64] = inp["lru_ba"][l].reshape(8, 128).T
        cols[l, :, 64:72] = inp["lru_bx"][l].reshape(8, 128).T
        cols[l, :, 72:80] = inp["lru_lambda"][l].reshape(8, 128).T
        for wi, name in enumerate(("lru_wa", "lru_wx")):
            w = inp[name][l]
            for cc in range(8):
                for j in range(2):
                    lruw[l, wi, cc, j * 64:(j + 1) * 64, j * 64:(j + 1) * 64] = w[cc * 2 + j]
    postg = np.ascontiguousarray(np.broadcast_to(inp["post_norm"][:depth, None, :], (depth, 128, D))).astype(f32)
    subg = np.ascontiguousarray(np.broadcast_to(inp["diff_subln"][:depth, None, :], (depth, 128, 128))).astype(f32)
    dlam = np.ascontiguousarray(np.broadcast_to(inp["diff_lambda"][:depth].reshape(depth, 1, 256), (depth, 128, 256))).astype(f32)
    w_br = np.ascontiguousarray(np.stack([inp["w_branch_a"][:depth], inp["w_branch_b"][:depth], inp["w_branch_c"][:depth]], axis=1)).astype(f32)
    return dict(cols=cols, lruw=lruw, postg=postg, subg=subg, dlam=dlam, w_br=w_br,
                w_in=np.ascontiguousarray(inp["w_in"][:depth]), w_out=np.ascontiguousarray(inp["w_out"][:depth]))


def kernel(**inputs):
    x = np.asarray(inputs["x"], dtype=np.float32)
    B, S, _ = x.shape
    depth = inputs["w_in"].shape[0]
    nc = build(S, depth)
    shared = _prep_weights({k: np.asarray(v) for k, v in inputs.items()}, depth)
    shared.update(_const_tables(S))
    in_maps = []
    for b in range(B):
        m = dict(shared)
        m["x"] = np.ascontiguousarray(x[b])
        in_maps.append(m)
    res = run_bass_kernel_spmd(nc, in_maps, core_ids=list(range(B)))
    return np.stack([np.asarray(r["y"], dtype=np.float32) for r in res.results], axis=0)
```

```python
import math
from contextlib import ExitStack
import numpy as np
import concourse.bass as bass
import concourse.mybir as mybir
from concourse.bass_utils import run_bass_kernel_spmd

F32 = mybir.dt.float32
BF16 = mybir.dt.bfloat16
AF = mybir.ActivationFunctionType
ALU = mybir.AluOpType
AX = mybir.AxisListType

D = 2048
DIN = 16384
EPS = 1e-6
KC = 16


class Res:
    __slots__ = ("w", "rd")

    def __init__(self):
        self.w = None
        self.rd = {}


class TK:
    SEM_LIMIT = 8000

    def __init__(self, nc, es):
        self.nc = nc
        self.E = {"pe": nc.tensor, "act": nc.scalar, "dve": nc.vector, "pool": nc.gpsimd, "sp": nc.sync}
        self.es = es
        self.csem = {}
        self.ccnt = {}
        self.retired = []
        self.nsem = 0
        for e in ("pe", "act", "dve", "pool"):
            self.csem[e] = es.enter_context(nc.semaphore("c_" + e))
            self.ccnt[e] = 0
        self.dsem = {
            "sp": [es.enter_context(nc.semaphore("dsp%d" % i)) for i in range(32)],
            "pool": [es.enter_context(nc.semaphore("dpl%d" % i)) for i in range(16)],
            "act": [es.enter_context(nc.semaphore("dac%d" % i)) for i in range(2)],
        }
        self.didx = {"sp": 0, "pool": 0, "act": 0}
        self.dtot = {}
        self.waited = {e: {} for e in self.E}

    def _wait(self, eng, ev):
        sem, val, _ = ev
        k = id(sem)
        if self.waited[eng].get(k, 0) >= val:
            return
        self.E[eng].wait_ge(sem, val)
        self.waited[eng][k] = val

    def op(self, eng, fn, rd=(), wr=()):
        for r in rd:
            if r.w is not None and not (r.w[2] == "pe" and eng == "pe"):
                self._wait(eng, r.w)
        for w in wr:
            if w.w is not None and not (w.w[2] == "pe" and eng == "pe"):
                self._wait(eng, w.w)
            for ev in w.rd.values():
                self._wait(eng, ev)
        if self.ccnt[eng] >= self.SEM_LIMIT:
            self.retired.append((self.csem[eng], self.ccnt[eng], eng))
            self.nsem += 1
            self.csem[eng] = self.es.enter_context(self.nc.semaphore("c_%s_%d" % (eng, self.nsem)))
            self.ccnt[eng] = 0
        inst = fn(self.E[eng])
        self.ccnt[eng] += 1
        inst.then_inc(self.csem[eng], 1)
        ev = (self.csem[eng], self.ccnt[eng], eng)
        for r in rd:
            r.rd[eng] = ev
        for w in wr:
            w.w = ev
            w.rd = {}
        return ev

    def dma(self, q, out, in_, rd=(), wr=()):
        for r in rd:
            if r.w is not None:
                self._wait(q, r.w)
        for w in wr:
            if w.w is not None:
                self._wait(q, w.w)
            for ev in w.rd.values():
                self._wait(q, ev)
        pool = self.dsem[q]
        i = self.didx[q]
        self.didx[q] = (i + 1) % len(pool)
        sem = pool[i]
        tot = self.dtot.get(id(sem), 0)
        if tot > 0:
            self._wait(q, (sem, tot, "dma"))
        self.E[q].dma_start(out=out, in_=in_).then_inc(sem, 16)
        tot += 16
        self.dtot[id(sem)] = tot
        ev = (sem, tot, "dma")
        for r in rd:
            r.rd[("d", id(sem))] = ev
        for w in wr:
            w.w = ev
            w.rd = {}
        return ev

    def barrier(self):
        for e in self.E:
            for ev in self.retired:
                self._wait(e, ev)
            for pe, s in self.csem.items():
                if self.ccnt[pe] > 0:
                    self._wait(e, (s, self.ccnt[pe], pe))
            for q, pool in self.dsem.items():
                for s in pool:
                    t = self.dtot.get(id(s), 0)
                    if t > 0:
                        self._wait(e, (s, t, "dma"))


def build(S, depth, dbg=(), stop=99, kinds=None):
    NT = S // 128
    NB = S // 512
    PASS = min(S, 2048)
    NPASS = S // PASS
    PT = PASS // 128
    PB = PASS // 512
    nc = bass.Bass("TRN2", target_bir_lowering=False)

    def din(name, shape, dt=F32):
        return nc.dram_tensor(name, list(shape), dt, kind="ExternalInput").ap()

    def dscr(name, shape, dt=BF16):
        kind = "ExternalOutput" if name in dbg else "Internal"
        return nc.dram_tensor(name, list(shape), dt, kind=kind).ap()

    x_in = din("x", [S, D])
    w_in = din("w_in", [depth, D, DIN])
    w_br = din("w_br", [depth, 3, 1024, D])
    w_out = din("w_out", [depth, D, D])
    cols = din("cols", [depth, 128, 80])
    postg = din("postg", [depth, 128, D])
    subg = din("subg", [depth, 128, 128])
    dlam = din("dlam", [depth, 128, 256])
    lruw = din("lruw", [depth, 2, 8, 128, 128])
    ident = din("ident", [128, 128])
    tri = din("tri", [128, 128])
    ropeR = din("ropeR", [4, S, 256])
    ropeD = din("ropeD", [3, S, 256])
    intraT = din("intraT", [128, 8, 128])
    kdec = din("kdec", [128, 8])
    qdec = din("qdec", [128, 8, 512])
    y_out = nc.dram_tensor("y", [S, D], F32, kind="ExternalOutput").ap()
    xmid = dscr("xmid", [S, D], F32)
    xaT = dscr("xaT", [8, 128, S])
    gaT = dscr("gaT", [8, 128, S])
    gmT = dscr("gmT", [48, 128, S])
    QrT = dscr("QrT", [8, 128, S])
    KrT = dscr("KrT", [8, 128, S])
    Kr = dscr("Kr", [S, 1024])
    Vr = dscr("Vr", [S, 1024])
    Gr = dscr("Gr", [S, 1024])
    QdT = dscr("QdT", [8, 128, S])
    KdT = dscr("KdT", [8, 128, S])
    Vd = dscr("Vd", [S, 1024])
    Gd = dscr("Gd", [S, 1024])
    yT = dscr("yT", [3, 8, 128, S])
    mT = dscr("mT", [16, 128, S])

    with ExitStack() as es:
        tk = TK(nc, es)
        op = tk.op
        dma = tk.dma

        uid = [0]

        def sb(stack, name, shape, dt):
            uid[0] += 1
            return stack.enter_context(nc.sbuf_tensor("%s_u%d" % (name, uid[0]), list(shape), dt))

        psb = [es.enter_context(nc.psum_tensor("ps%d" % i, [128, 512], F32)) for i in range(8)]
        psr = [Res() for _ in range(8)]

        ident_f = sb(es, "ident_f", [128, 128], F32)
        ident_b = sb(es, "ident_b", [128, 128], BF16)
        tri_f = sb(es, "tri_f", [128, 128], F32)
        tri_b = sb(es, "tri_b", [128, 128], BF16)
        r_c = Res()
        dma("sp", ident_f[:], ident, wr=[r_c])
        dma("sp", tri_f[:], tri, wr=[r_c])
        op("dve", lambda e: e.tensor_copy(out=ident_b[:], in_=ident_f[:]), rd=[r_c], wr=[r_c])
        op("dve", lambda e: e.tensor_copy(out=tri_b[:], in_=tri_f[:]), rd=[r_c], wr=[r_c])
        tk.barrier()

        for l in range(depth):
            lam_init = 0.8 - 0.6 * math.exp(-0.3 * l)
            x_src = x_in if l == 0 else xmid
            x_dst = y_out if l == depth - 1 else xmid
            with ExitStack() as ls:
                colst = sb(ls, "colst", [128, 80], F32)
                s8 = sb(ls, "s8", [128, 8], F32)
                s16 = sb(ls, "s16", [128, 8], F32)
                tmp8 = sb(ls, "tmp8", [128, 8], F32)
                dl = sb(ls, "dl", [128, 256], F32)
                dprod = sb(ls, "dprod", [128, 128], F32)
                dsum = sb(ls, "dsum", [128, 4], F32)
                neglam = sb(ls, "neglam", [128, 1], F32)
                subgs = sb(ls, "subgs", [128, 128], F32)
                r_p = Res()
                dma("sp", colst[:], cols[l], wr=[r_p])
                dma("sp", dl[:], dlam[l], wr=[r_p])
                dma("sp", subgs[:], subg[l], wr=[r_p])
                op("act", lambda e: e.activation(out=tmp8[:], in_=colst[:, 72:80], func=AF.Exp, scale=-1.0), rd=[r_p], wr=[r_p])
                op("act", lambda e: e.activation(out=tmp8[:], in_=tmp8[:], func=AF.Ln, bias=1.0), rd=[r_p], wr=[r_p])
                op("dve", lambda e: e.tensor_scalar(out=s8[:], in0=tmp8[:], scalar1=-8.0, scalar2=None, op0=ALU.mult), rd=[r_p], wr=[r_p])
                op("dve", lambda e: e.tensor_scalar(out=s16[:], in0=tmp8[:], scalar1=-16.0, scalar2=None, op0=ALU.mult), rd=[r_p], wr=[r_p])
                op("dve", lambda e: e.tensor_tensor(out=dprod[:, 0:64], in0=dl[:, 0:64], in1=dl[:, 64:128], op=ALU.mult), rd=[r_p], wr=[r_p])
                op("dve", lambda e: e.tensor_tensor(out=dprod[:, 64:128], in0=dl[:, 128:192], in1=dl[:, 192:256], op=ALU.mult), rd=[r_p], wr=[r_p])
                op("dve", lambda e: e.tensor_reduce(out=dsum[:, 0:1], in_=dprod[:, 0:64], axis=AX.X, op=ALU.add), rd=[r_p], wr=[r_p])
                op("dve", lambda e: e.tensor_reduce(out=dsum[:, 1:2], in_=dprod[:, 64:128], axis=AX.X, op=ALU.add), rd=[r_p], wr=[r_p])
                op("act", lambda e: e.activation(out=dsum[:, 2:4], in_=dsum[:, 0:2], func=AF.Exp), rd=[r_p], wr=[r_p])
                op("dve", lambda e: e.tensor_tensor(out=neglam[:], in0=dsum[:, 3:4], in1=dsum[:, 2:3], op=ALU.subtract), rd=[r_p], wr=[r_p])
                op("dve", lambda e: e.tensor_scalar(out=neglam[:], in0=neglam[:], scalar1=-lam_init, scalar2=None, op0=ALU.add), rd=[r_p], wr=[r_p])
                op("dve", lambda e: e.tensor_scalar(out=subgs[:], in0=subgs[:], scalar1=1.0 - lam_init, scalar2=None, op0=ALU.mult), rd=[r_p], wr=[r_p])
                tk.barrier()

                for p in range(NPASS):
                    tok_p = p * PASS
                    with ExitStack() as s1:
                        hT = sb(s1, "hT", [128, KC, PASS], BF16)
                        r_hT = [Res() for _ in range(PT)]
                        with ExitStack() as s0:
                            xt = [sb(s0, "xt%d" % i, [128, D], F32) for i in range(2)]
                            xn = [sb(s0, "xn%d" % i, [128, D], BF16) for i in range(2)]
                            junk = sb(s0, "junk", [128, D], BF16)
                            st = [sb(s0, "st%d" % i, [128, 4], F32) for i in range(2)]
                            r_xt = [Res(), Res()]
                            r_xn = [Res(), Res()]
                            r_junk = Res()
                            r_st = [Res(), Res()]
                            dma("sp", xt[0][:], x_src[tok_p:tok_p + 128, :], wr=[r_xt[0]])
                            for tt in range(PT):
                                b = tt % 2
                                if tt + 1 < PT:
                                    t1 = tok_p + (tt + 1) * 128
                                    dma("sp", xt[1 - b][:], x_src[t1:t1 + 128, :], wr=[r_xt[1 - b]])
                                op("act", lambda e, b=b: e.activation(out=junk[:], in_=xt[b][:], func=AF.Square, accum_out=st[b][:, 0:1]),
                                   rd=[r_xt[b]], wr=[r_junk, r_st[b]])
                                op("act", lambda e, b=b: e.activation(out=st[b][:, 1:2], in_=st[b][:, 0:1], func=AF.Sqrt, scale=1.0 / D, bias=EPS),
                                   rd=[r_st[b]], wr=[r_st[b]])
                                op("dve", lambda e, b=b: e.reciprocal(out=st[b][:, 2:3], in_=st[b][:, 1:2]), rd=[r_st[b]], wr=[r_st[b]])
                                op("dve", lambda e, b=b: e.tensor_scalar(out=xn[b][:], in0=xt[b][:], scalar1=st[b][:, 2:3], scalar2=None, op0=ALU.mult),
                                   rd=[r_xt[b], r_st[b]], wr=[r_xn[b]])
                                for half in range(2):
                                    pbk = 6 + half
                                    ptv = psb[pbk][:].bitcast(BF16)
                                    for j in range(8):
                                        kc = half * 8 + j
                                        op("pe", lambda e, b=b, kc=kc, j=j, ptv=ptv: e.transpose(out=ptv[:, j * 128:(j + 1) * 128], in_=xn[b][:, kc * 128:(kc + 1) * 128], identity=ident_b[:]),
                                           rd=[r_xn[b]], wr=[psr[pbk]])
                                    src = ptv.rearrange("p (j t) -> p j t", j=8)
                                    dst = hT[:, half * 8:(half + 1) * 8, tt * 128:(tt + 1) * 128]
                                    if half == 0:
                                        op("act", lambda e, src=src, dst=dst: e.activation(out=dst, in_=src, func=AF.Copy), rd=[psr[pbk]], wr=[r_hT[tt]])
                                    else:
                                        op("dve", lambda e, src=src, dst=dst: e.tensor_copy(out=dst, in_=src), rd=[psr[pbk]], wr=[r_hT[tt]])
                            tk.barrier()
                            if stop == 1:
                                return nc
                        with ExitStack() as s2:
                            wst = [sb(s2, "wst%d" % i, [128, KC, 256], F32) for i in range(2)]
                            wbf = [sb(s2, "wbf%d" % i, [128, KC, 256], BF16) for i in range(2)]
                            r_wst = [Res(), Res()]
                            r_wbf = [Res(), Res()]
                            fst = [sb(s2, "fst%d" % i, [128, 512], BF16) for i in range(2)]
                            r_fst = [Res(), Res()]
                            tmb = [sb(s2, "tmb%d" % i, [128, 256], BF16) for i in range(2)]
                            r_tmb = [Res(), Res()]
                            ra = sb(s2, "ra", [128, 256], F32)
                            rb = sb(s2, "rb", [128, 256], F32)
                            r_ra = Res()
                            r_rb = Res()
                            tst = [sb(s2, "tst%d" % i, [128, 2, 512], BF16) for i in range(2)]
                            r_tst = [Res(), Res()]
                            rtt = [sb(s2, "rtt%d" % i, [128, 2, 256], F32) for i in range(2)]
                            r_rtt = [Res(), Res()]
                            dtt = [sb(s2, "dtt%d" % i, [128, 3, 256], F32) for i in range(2)]
                            r_dtt = [Res(), Res()]
                            rc = sb(s2, "rc", [128, 256], F32)
                            r_rc = Res()
                            rbd = sb(s2, "rbd", [128, 256], F32)
                            r_rbd = Res()
                            op("pool", lambda e: e.memset(rbd[:], 0.0), wr=[r_rbd])
                            op("pool", lambda e: e.memset(rc[:], 0.0), wr=[r_rc])
                            r_tab = Res()
                            groups = []
                            for g in range(8):
                                groups.append(("xa", g * 128, 128, g))
                            for kind, base in (("qr", 2048), ("kr", 3072), ("vr", 4096), ("qd", 6144), ("kd", 7168), ("vd", 8192)):
                                for g in range(4):
                                    groups.append((kind, base + g * 256, 256, g))
                            for g in range(8):
                                groups.append(("ga", 1024 + g * 128, 128, g))
                            for kind, base in (("gr", 5120), ("gd", 9216)):
                                for g in range(4):
                                    groups.append((kind, base + g * 256, 256, g))
                            for g in range(48):
                                groups.append(("gm", 10240 + g * 128, 128, g))
                            if kinds is not None:
                                groups = [g_ for g_ in groups if g_[0] in kinds]
                            wv = w_in[l].rearrange("(kc p) n -> p kc n", p=128)

                            def wload(gi):
                                _, c0, ncol, _ = groups[gi]
                                b = gi % 2
                                dma("sp", wst[b][:, :, 0:ncol], wv[:, :, c0:c0 + ncol], wr=[r_wst[b]])

                            def wcast(gi):
                                _, c0, ncol, _ = groups[gi]
                                b = gi % 2
                                busy_dve = gi >= 1 and groups[gi - 1][0] in ("qr", "kr", "qd", "kd")
                                for kc in range(KC):
                                    if kc % 2 == 0 and not busy_dve:
                                        op("dve", lambda e, b=b, kc=kc, ncol=ncol: e.tensor_scalar(out=wbf[b][:, kc, 0:ncol], in0=wst[b][:, kc, 0:ncol], scalar1=colst[:, kc:kc + 1], scalar2=None, op0=ALU.mult),
                                           rd=[r_wst[b]], wr=[r_wbf[b]])
                                    else:
                                        op("act", lambda e, b=b, kc=kc, ncol=ncol: e.activation(out=wbf[b][:, kc, 0:ncol], in_=wst[b][:, kc, 0:ncol], func=AF.Copy, scale=colst[:, kc:kc + 1]),
                                           rd=[r_wst[b]], wr=[r_wbf[b]])

                            cnt = {"ps": 0, "fst": 0, "tmb": 0, "tst": 0, "pt": 0, "dtt": 0, "rtt": 0}

                            def compute(gi):
                                kind, c0, ncol, g = groups[gi]
                                b = gi % 2
                                if ncol == 128:
                                    dst = {"xa": xaT, "ga": gaT, "gm": gmT}[kind]
                                    func = {"xa": AF.Copy, "ga": AF.Silu, "gm": AF.Sigmoid}[kind]
                                    for tb in range(PB):
                                        pk = cnt["ps"] % 6
                                        cnt["ps"] += 1
                                        for kc in range(KC):
                                            op("pe", lambda e, pk=pk, kc=kc, tb=tb, b=b: e.matmul(psb[pk][:, 0:512], lhsT=wbf[b][:, kc, 0:128], rhs=hT[:, kc, tb * 512:(tb + 1) * 512], start=(kc == 0), stop=(kc == KC - 1)),
                                               rd=[r_wbf[b]] + r_hT[tb * 4:tb * 4 + 4], wr=[psr[pk]])
                                        fb = cnt["fst"] % 2
                                        cnt["fst"] += 1
                                        op("act", lambda e, pk=pk, fb=fb, func=func: e.activation(out=fst[fb][:], in_=psb[pk][:, 0:512], func=func), rd=[psr[pk]], wr=[r_fst[fb]])
                                        t0 = tok_p + tb * 512
                                        dma("pool", dst[g, :, t0:t0 + 512], fst[fb][:], rd=[r_fst[fb]])
                                    return
                                trq = []
                                for tb in range(PB):
                                    need_t = kind in ("qr", "kr", "qd", "kd")
                                    if need_t:
                                        ptk = 6 + cnt["pt"] % 2
                                        cnt["pt"] += 1
                                        ptv = psb[ptk][:].bitcast(BF16).rearrange("p (h t d) -> p h t d", h=2, t=4)
                                    for t4 in range(4):
                                        tt = tb * 4 + t4
                                        tok0 = tok_p + tt * 128
                                        pk = cnt["ps"] % 6
                                        cnt["ps"] += 1
                                        for kc in range(KC):
                                            op("pe", lambda e, pk=pk, kc=kc, tt=tt, b=b: e.matmul(psb[pk][:, 0:256], lhsT=hT[:, kc, tt * 128:(tt + 1) * 128], rhs=wbf[b][:, kc, 0:256], start=(kc == 0), stop=(kc == KC - 1)),
                                               rd=[r_wbf[b], r_hT[tt]], wr=[psr[pk]])
                                        while trq:
                                            trq.pop(0)()
                                        mb = cnt["tmb"] % 2
                                        cnt["tmb"] += 1
                                        pv = psb[pk][:, 0:256]
                                        if kind in ("vr", "vd", "gr", "gd"):
                                            func = AF.Silu if kind[0] == "g" else AF.Copy
                                            op("act", lambda e, pv=pv, mb=mb, func=func: e.activation(out=tmb[mb][:], in_=pv, func=func), rd=[psr[pk]], wr=[r_tmb[mb]])
                                            dst = {"vr": Vr, "vd": Vd, "gr": Gr, "gd": Gd}[kind]
                                            dma("pool", dst[tok0:tok0 + 128, g * 256:(g + 1) * 256], tmb[mb][:], rd=[r_tmb[mb]])
                                            continue
                                        if kind in ("qr", "kr"):
                                            ti = 0 if kind == "qr" else 2
                                            rbi = cnt["rtt"] % 2
                                            cnt["rtt"] += 1
                                            dma("sp", rtt[rbi][:], ropeR[ti:ti + 2, tok0:tok0 + 128, :].rearrange("i p f -> p i f"), wr=[r_rtt[rbi]])
                                            op("dve", lambda e, pv=pv, rbi=rbi: e.tensor_tensor(out=ra[:], in0=pv, in1=rtt[rbi][:, 0, :], op=ALU.mult),
                                               rd=[psr[pk], r_rtt[rbi]], wr=[r_ra])
                                            pvp = pv.rearrange("p (a two) -> p a two", two=2)
                                            rbp = rb[:].rearrange("p (a two) -> p a two", two=2)
                                            stp = rtt[rbi][:, 1, :].rearrange("p (a two) -> p a two", two=2)
                                            op("dve", lambda e, pvp=pvp, rbp=rbp, stp=stp: e.tensor_tensor(out=rbp[:, :, 0], in0=pvp[:, :, 1], in1=stp[:, :, 0], op=ALU.mult),
                                               rd=[psr[pk], r_rtt[rbi]], wr=[r_rb])
                                            op("dve", lambda e, pvp=pvp, rbp=rbp, stp=stp: e.tensor_tensor(out=rbp[:, :, 1], in0=pvp[:, :, 0], in1=stp[:, :, 1], op=ALU.mult),
                                               rd=[psr[pk], r_rtt[rbi]], wr=[r_rb])
                                            op("dve", lambda e, mb=mb: e.tensor_tensor(out=tmb[mb][:], in0=ra[:], in1=rb[:], op=ALU.add), rd=[r_ra, r_rb], wr=[r_tmb[mb]])
                                            if kind == "kr":
                                                dma("pool", Kr[tok0:tok0 + 128, g * 256:(g + 1) * 256], tmb[mb][:], rd=[r_tmb[mb]])
                                        else:
                                            db = cnt["dtt"] % 2
                                            cnt["dtt"] += 1
                                            dma("sp", dtt[db][:], ropeD[:, tok0:tok0 + 128, :].rearrange("i p f -> p i f"), wr=[r_dtt[db]])
                                            op("dve", lambda e, pv=pv, db=db: e.tensor_tensor(out=ra[:], in0=pv, in1=dtt[db][:, 0, :], op=ALU.mult),
                                               rd=[psr[pk], r_dtt[db]], wr=[r_ra])
                                            op("dve", lambda e, pv=pv, db=db: e.tensor_tensor(out=rbd[:, 0:248], in0=pv[:, 8:256], in1=dtt[db][:, 1, 0:248], op=ALU.mult),
                                               rd=[psr[pk], r_dtt[db]], wr=[r_rbd])
                                            op("dve", lambda e, pv=pv, db=db: e.tensor_tensor(out=rc[:, 8:256], in0=pv[:, 0:248], in1=dtt[db][:, 2, 8:256], op=ALU.mult),
                                               rd=[psr[pk], r_dtt[db]], wr=[r_rc])
                                            op("dve", lambda e: e.tensor_tensor(out=ra[:], in0=ra[:], in1=rbd[:], op=ALU.add), rd=[r_ra, r_rbd], wr=[r_ra])
                                            op("dve", lambda e, mb=mb: e.tensor_tensor(out=tmb[mb][:], in0=ra[:], in1=rc[:], op=ALU.add), rd=[r_ra, r_rc], wr=[r_tmb[mb]])
                                        def _tr(ptv=ptv, ptk=ptk, t4=t4, mb=mb):
                                            for hh in range(2):
                                                op("pe", lambda e, hh=hh: e.transpose(out=ptv[:, hh, t4, :], in_=tmb[mb][:, hh * 128:(hh + 1) * 128], identity=ident_b[:]),
                                                   rd=[r_tmb[mb]], wr=[psr[ptk]])
                                        trq.append(_tr)
                                    if need_t:
                                        def _ev(ptk=ptk, tb=tb, kind=kind, g=g):
                                            sbk = cnt["tst"] % 2
                                            cnt["tst"] += 1
                                            src = psb[ptk][:].bitcast(BF16).rearrange("p (h s) -> p h s", h=2)
                                            op("act", lambda e: e.activation(out=tst[sbk][:], in_=src, func=AF.Copy), rd=[psr[ptk]], wr=[r_tst[sbk]])
                                            dst = {"qr": QrT, "kr": KrT, "qd": QdT, "kd": KdT}[kind]
                                            t0 = tok_p + tb * 512
                                            dma("pool", dst[2 * g:2 * g + 2, :, t0:t0 + 512].rearrange("h d s -> d h s"), tst[sbk][:], rd=[r_tst[sbk]])
                                        trq.append(_ev)
                                while trq:
                                    trq.pop(0)()

                            ng = len(groups)
                            wload(0)
                            wload(1)
                            wcast(0)
                            for gi in range(ng):
                                if gi + 1 < ng:
                                    wcast(gi + 1)
                                compute(gi)
                                if gi + 2 < ng:
                                    wload(gi + 2)
                            tk.barrier()

                if stop == 2:
                    tk.barrier()
                    return nc
                with ExitStack() as s3:
                    HS = min(S, 2048)
                    NH = S // HS
                    HB = HS // 512
                    xpad = [sb(s3, "xpad%d" % i, [128, S + 4], BF16) for i in range(2)]
                    sga = [sb(s3, "sga%d" % i, [128, S], BF16) for i in range(2)]
                    r_in = [Res(), Res()]
                    wlf = sb(s3, "wlf", [128, 2, 128], F32)
                    wlb = [sb(s3, "wlb%d" % i, [128, 2, 128], BF16) for i in range(2)]
                    r_wlf = Res()
                    r_wlb = [Res(), Res()]
                    dg = [sb(s3, "dg%d" % i, [128, 4, 128], BF16) for i in range(2)]
                    r_dg = [Res(), Res()]
                    xc = [sb(s3, "xc%d" % i, [128, HS], BF16) for i in range(2)]
                    rr = [sb(s3, "rr%d" % i, [128, HS], F32) for i in range(2)]
                    ii = [sb(s3, "ii%d" % i, [128, HS], F32) for i in range(2)]
                    aa = [sb(s3, "aa%d" % i, [128, HS], F32) for i in range(2)]
                    a2 = [sb(s3, "a2%d" % i, [128, HS], F32) for i in range(2)]
                    ya = [sb(s3, "ya%d" % i, [128, S], BF16) for i in range(2)]
                    r_xc = [Res(), Res()]
                    r_rr = [Res(), Res()]
                    r_ii = [Res(), Res()]
                    r_aa = [Res(), Res()]
                    r_a2 = [Res(), Res()]
                    r_ya = [Res(), Res()]
                    for i in range(2):
                        op("pool", lambda e, i=i: e.memset(xpad[i][:, 0:4], 0.0), wr=[r_in[i]])

                    def lru_load(cc):
                        b = cc % 2
                        dma("sp", xpad[b][:, 3:3 + S], xaT[cc], wr=[r_in[b]])
                        dma("sp", sga[b][:], gaT[cc], wr=[r_in[b]])

                    lru_load(0)
                    pc = 0
                    u = 0
                    for cc in range(8):
                        b = cc % 2
                        if cc + 1 < 8:
                            lru_load(cc + 1)
                        dma("sp", wlf[:, 0, :], lruw[l, 0, cc], wr=[r_wlf])
                        dma("sp", wlf[:, 1, :], lruw[l, 1, cc], wr=[r_wlf])
                        op("pool", lambda e, b=b: e.tensor_copy(out=wlb[b][:], in_=wlf[:]), rd=[r_wlf], wr=[r_wlb[b]])
                        for k in range(4):
                            op("pool", lambda e, b=b, k=k, cc=cc: e.tensor_scalar(out=dg[b][:, k, :], in0=ident_f[:], scalar1=colst[:, 16 + k * 8 + cc:17 + k * 8 + cc], scalar2=1.0, op0=ALU.mult, op1=ALU.mult),
                               wr=[r_dg[b]])
                        for hf in range(NH):
                            ub = u % 2
                            u += 1
                            h0 = hf * HS
                            for tb in range(HB):
                                pk = pc % 6
                                pc += 1
                                t0 = h0 + tb * 512
                                for k in range(4):
                                    op("pe", lambda e, pk=pk, k=k, t0=t0, b=b: e.matmul(psb[pk][:, 0:512], lhsT=dg[b][:, k, :], rhs=xpad[b][:, t0 + k:t0 + k + 512], start=(k == 0), stop=(k == 3)),
                                       rd=[r_dg[b], r_in[b]], wr=[psr[pk]])
                                op("act", lambda e, pk=pk, tb=tb, cc=cc, ub=ub: e.activation(out=xc[ub][:, tb * 512:(tb + 1) * 512], in_=psb[pk][:, 0:512], func=AF.Identity, bias=colst[:, 48 + cc:49 + cc]),
                                   rd=[psr[pk]], wr=[r_xc[ub]])
                            for tb in range(HB):
                                for gi_, (dstt, r_d, bo) in enumerate(((rr[ub], r_rr[ub], 56), (ii[ub], r_ii[ub], 64))):
                                    pk = pc % 6
                                    pc += 1
                                    op("pe", lambda e, pk=pk, tb=tb, b=b, gi_=gi_, ub=ub: e.matmul(psb[pk][:, 0:512], lhsT=wlb[b][:, gi_, :], rhs=xc[ub][:, tb * 512:(tb + 1) * 512], start=True, stop=True),
                                       rd=[r_wlb[b], r_xc[ub]], wr=[psr[pk]])
                                    op("act", lambda e, pk=pk, tb=tb, dstt=dstt, bo=bo, cc=cc: e.activation(out=dstt[:, tb * 512:(tb + 1) * 512], in_=psb[pk][:, 0:512], func=AF.Sigmoid, bias=colst[:, bo + cc:bo + cc + 1]),
                                       rd=[psr[pk]], wr=[r_d])
                            op("act", lambda e, cc=cc, ub=ub: e.activation(out=aa[ub][:], in_=rr[ub][:], func=AF.Exp, scale=s8[:, cc:cc + 1]), rd=[r_rr[ub]], wr=[r_aa[ub]])
                            op("act", lambda e, cc=cc, ub=ub: e.activation(out=a2[ub][:], in_=rr[ub][:], func=AF.Exp, scale=s16[:, cc:cc + 1]), rd=[r_rr[ub]], wr=[r_a2[ub]])
                            op("act", lambda e, ub=ub: e.activation(out=a2[ub][:], in_=a2[ub][:], func=AF.Sqrt, scale=-1.0, bias=1.0), rd=[r_a2[ub]], wr=[r_a2[ub]])
                            op("dve", lambda e, ub=ub: e.tensor_tensor(out=ii[ub][:], in0=ii[ub][:], in1=xc[ub][:], op=ALU.mult), rd=[r_ii[ub], r_xc[ub]], wr=[r_ii[ub]])
                            op("dve", lambda e, ub=ub: e.tensor_tensor(out=ii[ub][:], in0=ii[ub][:], in1=a2[ub][:], op=ALU.mult), rd=[r_ii[ub], r_a2[ub]], wr=[r_ii[ub]])
                            if hf == 0:
                                op("dve", lambda e, ub=ub: e.tensor_tensor_scan(out=rr[ub][:], data0=aa[ub][:], data1=ii[ub][:], initial=0.0, op0=ALU.mult, op1=ALU.add), rd=[r_aa[ub], r_ii[ub]], wr=[r_rr[ub]])
                            else:
                                op("dve", lambda e, ub=ub: e.tensor_tensor_scan(out=rr[ub][:], data0=aa[ub][:], data1=ii[ub][:], initial=rr[1 - ub][:, HS - 1:HS], op0=ALU.mult, op1=ALU.add), rd=[r_aa[ub], r_ii[ub], r_rr[1 - ub]], wr=[r_rr[ub]])
                            op("dve", lambda e, b=b, ub=ub, h0=h0: e.tensor_tensor(out=ya[b][:, h0:h0 + HS], in0=rr[ub][:], in1=sga[b][:, h0:h0 + HS], op=ALU.mult), rd=[r_rr[ub], r_in[b]], wr=[r_ya[b]])
                        dma("pool", yT[0, cc], ya[b][:], rd=[r_ya[b]])
                    tk.barrier()

                if stop == 3:
                    tk.barrier()
                    return nc
                with ExitStack() as s4:
                    itT = sb(s4, "itT", [128, 8, 128], F32)
                    kdt = sb(s4, "kdt", [128, 8], F32)
                    qdt = sb(s4, "qdt", [128, 8, 512], F32)
                    r_k = Res()
                    dma("sp", itT[:], intraT, wr=[r_k])
                    dma("sp", kdt[:], kdec, wr=[r_k])
                    dma("sp", qdt[:], qdec, wr=[r_k])
                    qT = [sb(s4, "qT%d" % i, [128, 8, 512], BF16) for i in range(2)]
                    kT = [sb(s4, "kT%d" % i, [128, 8, 512], BF16) for i in range(2)]
                    kM = [sb(s4, "kM%d" % i, [128, 4, 1024], BF16) for i in range(2)]
                    vM = [sb(s4, "vM%d" % i, [128, 4, 1024], BF16) for i in range(2)]
                    gM = [sb(s4, "gM%d" % i, [128, 4, 1024], BF16) for i in range(2)]
                    r_q = [Res(), Res()]
                    r_kt = [Res(), Res()]
                    r_km = [Res(), Res()]
                    r_vm = [Res(), Res()]
                    r_gm = [Res(), Res()]
                    qTd = sb(s4, "qTd", [128, 8, 512], BF16)
                    kMd = sb(s4, "kMd", [128, 4, 1024], BF16)
                    r_qTd, r_kMd = Res(), Res()
                    Sf = sb(s4, "Sf", [128, 8, 128], F32)
                    Sb = sb(s4, "Sb", [128, 8, 128], BF16)
                    r_Sf, r_Sb = Res(), Res()
                    sT = [sb(s4, "sT%d" % i, [128, 512], BF16) for i in range(2)]
                    r_sT = [Res(), Res()]
                    bst = sb(s4, "bst", [128, 8, 6], F32)
                    mv = sb(s4, "mv", [128, 8, 2], F32)
                    rs = sb(s4, "rs", [128, 8], F32)
                    nbias = sb(s4, "nbias", [128, 8], F32)
                    r_nb = Res()
                    r_bst, r_mv, r_rs = Res(), Res(), Res()
                    yn = sb(s4, "yn", [128, 1024], F32)
                    ybm = [sb(s4, "ybm%d" % i, [128, 1024], BF16) for i in range(2)]
                    r_yn = Res()
                    r_ybm = [Res(), Res()]
                    rq = []
                    ybt = [sb(s4, "ybt%d" % i, [128, 8, 512], BF16) for i in range(2)]
                    r_ybt = [Res(), Res()]
                    op("dve", lambda e: e.memset(Sf[:], 0.0), wr=[r_Sf])
                    op("pool", lambda e: e.memset(Sb[:], 0.0), wr=[r_Sb])

                    def ret_load(tb):
                        b = tb % 2
                        t0 = tb * 512
                        dma("sp", qT[b][:], QrT[:, :, t0:t0 + 512].rearrange("h d s -> d h s"), wr=[r_q[b]])
                        dma("sp", kT[b][:], KrT[:, :, t0:t0 + 512].rearrange("h d s -> d h s"), wr=[r_kt[b]])
                        dma("sp", kM[b][:], Kr[t0:t0 + 512, :].rearrange("(n p) f -> p n f", p=128), wr=[r_km[b]])
                        dma("sp", vM[b][:], Vr[t0:t0 + 512, :].rearrange("(n p) f -> p n f", p=128), wr=[r_vm[b]])
                        dma("sp", gM[b][:], Gr[t0:t0 + 512, :].rearrange("(n p) f -> p n f", p=128), wr=[r_gm[b]])

                    ret_load(0)
                    pc = 0
                    for tb in range(NB):
                        b = tb % 2
                        if tb + 1 < NB:
                            ret_load(tb + 1)
                        op("pool", lambda e, b=b: e.tensor_tensor(out=qTd[:], in0=qT[b][:], in1=qdt[:], op=ALU.mult), rd=[r_q[b], r_k], wr=[r_qTd])
                        for h in range(8):
                            op("pool", lambda e, b=b, h=h: e.tensor_scalar(out=kMd[:, :, h * 128:(h + 1) * 128], in0=kM[b][:, :, h * 128:(h + 1) * 128], scalar1=kdt[:, h:h + 1], scalar2=1.0, op0=ALU.mult, op1=ALU.mult),
                               rd=[r_km[b], r_k], wr=[r_kMd])
                        for n in range(4):
                            cs = slice(n * 128, (n + 1) * 128)
                            obank = []
                            for hg in range(2):
                                pk = pc % 6
                                pc += 1
                                for h4 in range(4):
                                    h = hg * 4 + h4
                                    op("pe", lambda e, pk=pk, h4=h4, h=h, b=b, cs=cs: e.matmul(psb[pk][:, h4 * 128:(h4 + 1) * 128], lhsT=kT[b][:, h, cs], rhs=qT[b][:, h, cs], start=True, stop=True),
                                       rd=[r_kt[b], r_q[b]], wr=[psr[pk]])
                                sbk = (2 * n + hg) % 2
                                op("dve", lambda e, pk=pk, sbk=sbk, hg=hg: e.tensor_tensor(out=sT[sbk][:].rearrange("p (h i) -> p h i", h=4), in0=psb[pk][:, 0:512].rearrange("p (h i) -> p h i", h=4), in1=itT[:, hg * 4:(hg + 1) * 4, :], op=ALU.mult),
                                   rd=[psr[pk], r_k], wr=[r_sT[sbk]])
                                po = pc % 6
                                pc += 1
                                obank.append(po)
                                for h4 in range(4):
                                    h = hg * 4 + h4
                                    op("pe", lambda e, po=po, h4=h4, h=h, b=b, n=n, sbk=sbk: e.matmul(psb[po][:, h4 * 128:(h4 + 1) * 128], lhsT=sT[sbk][:, h4 * 128:(h4 + 1) * 128], rhs=vM[b][:, n, h * 128:(h + 1) * 128], start=True, stop=False),
                                       rd=[r_sT[sbk], r_vm[b]], wr=[psr[po]])
                                    op("pe", lambda e, po=po, h4=h4, h=h, cs=cs: e.matmul(psb[po][:, h4 * 128:(h4 + 1) * 128], lhsT=qTd[:, h, cs], rhs=Sb[:, h, :], start=False, stop=True),
                                       rd=[r_qTd, r_Sb], wr=[psr[po]])
                            for hg in range(2):
                                pk = pc % 6
                                pc += 1
                                for h4 in range(4):
                                    h = hg * 4 + h4
                                    op("pe", lambda e, pk=pk, h4=h4, h=h, b=b, n=n: e.matmul(psb[pk][:, h4 * 128:(h4 + 1) * 128], lhsT=kMd[:, n, h * 128:(h + 1) * 128], rhs=vM[b][:, n, h * 128:(h + 1) * 128], start=True, stop=True),
                                       rd=[r_kMd, r_vm[b]], wr=[psr[pk]])
                                for h4 in range(4):
                                    h = hg * 4 + h4
                                    cd = float(np.exp(np.float32(128.0) * np.log1p(-np.exp2(np.float32(-5.0 - h)))))
                                    op("dve", lambda e, pk=pk, h4=h4, h=h, cd=cd: e.scalar_tensor_tensor(out=Sf[:, h, :], in0=Sf[:, h, :], scalar=cd, in1=psb[pk][:, h4 * 128:(h4 + 1) * 128], op0=ALU.mult, op1=ALU.add),
                                       rd=[psr[pk], r_Sf], wr=[r_Sf])
                            op("act", lambda e: e.activation(out=Sb[:], in_=Sf[:], func=AF.Copy), rd=[r_Sf], wr=[r_Sb])
                            while rq:
                                rq.pop(0)()
                            for hg in range(2):
                                po = obank[hg]
                                for h4 in range(4):
                                    h = hg * 4 + h4
                                    op("dve", lambda e, po=po, h4=h4, h=h: e.bn_stats(out=bst[:, h, :], in_=psb[po][:, h4 * 128:(h4 + 1) * 128]), rd=[psr[po]], wr=[r_bst])
                                    op("dve", lambda e, h=h: e.bn_aggr(out=mv[:, h, :], in_=bst[:, h, :]), rd=[r_bst], wr=[r_mv])
                            op("act", lambda e: e.activation(out=rs[:], in_=mv[:, :, 1], func=AF.Ln, bias=EPS), rd=[r_mv], wr=[r_rs])
                            op("act", lambda e: e.activation(out=rs[:], in_=rs[:], func=AF.Exp, scale=-0.5), rd=[r_rs], wr=[r_rs])
                            op("dve", lambda e: e.scalar_tensor_tensor(out=nbias[:], in0=mv[:, :, 0], scalar=-1.0, in1=rs[:], op0=ALU.mult, op1=ALU.mult), rd=[r_mv, r_rs], wr=[r_nb])
                            for hg in range(2):
                                po = obank[hg]
                                for h4 in range(4):
                                    h = hg * 4 + h4
                                    op("act", lambda e, po=po, h4=h4, h=h: e.activation(out=yn[:, h * 128:(h + 1) * 128], in_=psb[po][:, h4 * 128:(h4 + 1) * 128], func=AF.Identity, scale=rs[:, h:h + 1], bias=nbias[:, h:h + 1]),
                                       rd=[psr[po], r_nb, r_rs], wr=[r_yn])
                            yi = (tb * 4 + n) % 2
                            op("pool", lambda e, b=b, n=n, yi=yi: e.tensor_tensor(out=ybm[yi][:], in0=yn[:], in1=gM[b][:, n, :], op=ALU.mult), rd=[r_yn, r_gm[b]], wr=[r_ybm[yi]])

                            def _tr(yi=yi, b=b, cs=cs, n=n, tb=tb):
                                ptk = 6 + yi
                                ptv = psb[ptk][:].bitcast(BF16).rearrange("p (h t) -> p h t", h=8)
                                for h in range(8):
                                    op("pe", lambda e, h=h: e.transpose(out=ptv[:, h, :], in_=ybm[yi][:, h * 128:(h + 1) * 128], identity=ident_b[:]), rd=[r_ybm[yi]], wr=[psr[ptk]])
                                op("act", lambda e: e.activation(out=ybt[b][:, :, cs], in_=ptv, func=AF.Copy), rd=[psr[ptk]], wr=[r_ybt[b]])
                                if n == 3:
                                    t0 = tb * 512
                                    dma("pool", yT[1, :, :, t0:t0 + 512].rearrange("h d s -> d h s"), ybt[b][:], rd=[r_ybt[b]])
                            rq.append(_tr)
                    while rq:
                        rq.pop(0)()
                    tk.barrier()

                if stop == 4:
                    tk.barrier()
                    return nc
                with ExitStack() as s5:
                    qh = [sb(s5, "qh%d" % i, [128, S], BF16) for i in range(2)]
                    kh = [sb(s5, "kh%d" % i, [128, S], BF16) for i in range(2)]
                    vh = [sb(s5, "vh%d" % i, [128, NT, 132], BF16) for i in range(2)]
                    gh = [sb(s5, "gh%d" % i, [128, NT, 128], BF16) for i in range(2)]
                    r_qh = [Res(), Res()]
                    r_kh = [Res(), Res()]
                    r_vh = [Res(), Res()]
                    r_gh = [Res(), Res()]
                    NE = 4
                    Eb = [[sb(s5, "E%d_%d" % (c, i), [128, 512], BF16) for i in range(NE)] for c in range(2)]
                    r_E = [[Res() for _ in range(NE)] for _ in range(2)]
                    accs = [sb(s5, "accs%d" % i, [128, 3, 512], F32) for i in range(2)]
                    r_accs = [Res(), Res()]
                    ogb = [sb(s5, "ogb%d" % i, [128, 4, 128], F32) for i in range(2)]
                    r_ogb = [Res(), Res()]
                    smb = [sb(s5, "smb%d" % i, [128, 16], F32) for i in range(2)]
                    r_smb = [Res(), Res()]
                    o1 = sb(s5, "o1", [128, 128], F32)
                    o2 = sb(s5, "o2", [128, 128], F32)
                    jk = sb(s5, "jk", [128, 128], F32)
                    r_o1, r_o2, r_jk = Res(), Res(), Res()
                    ycm = [sb(s5, "ycm%d" % i, [128, 4, 128], BF16) for i in range(2)]
                    r_ycm = [Res(), Res()]
                    yct = [sb(s5, "yct%d" % i, [128, 512], BF16) for i in range(2)]
                    r_yct = [Res(), Res()]
                    for i in range(2):
                        op("pool", lambda e, i=i: e.memset(vh[i][:, :, 128:129], 1.0), wr=[r_vh[i]])

                    def da_load(h):
                        b = h % 2
                        dma("sp", qh[b][:], QdT[h], wr=[r_qh[b]])
                        dma("sp", kh[b][:], KdT[h], wr=[r_kh[b]])
                        dma("sp", vh[b][:, :, 0:128], Vd[:, h * 128:(h + 1) * 128].rearrange("(n p) e -> p n e", p=128), wr=[r_vh[b]])
                        dma("sp", gh[b][:], Gd[:, h * 128:(h + 1) * 128].rearrange("(n p) e -> p n e", p=128), wr=[r_gh[b]])

                    def acc_ap(c, qs):
                        if qs < 3:
                            return c, psb[c][:, qs * 129:qs * 129 + 129]
                        return 2, psb[2][:, c * 129:c * 129 + 129]

                    def acc_sb(ab, c, qs):
                        if qs < 3:
                            return accs[ab][:, c, qs * 129:qs * 129 + 129]
                        return accs[ab][:, 2, c * 129:c * 129 + 129]

                    pending = []

                    def defer(n, fn):
                        pending.append([n, fn])

                    def tick():
                        for it in pending:
                            it[0] -= 1
                        while pending and pending[0][0] <= 0:
                            pending.pop(0)[1]()

                    def flush():
                        while pending:
                            pending.pop(0)[1]()

                    LA = 2
                    da_load(0)
                    da_load(1)
                    st_ = {"sc": 0, "ec": [0, 0], "blk": 0}
                    for h in range(8):
                        b = h % 2
                        for qb in range(NB):
                            if qb == min(1, NB - 1) and h >= 1 and h + 1 < 8:
                                da_load(h + 1)
                            nkt = 4 * qb + 4
                            started = [False, False, False]
                            info = {}

                            def qk(kt):
                                r = kt - 4 * qb
                                c0 = r * 128 if r > 0 else 0
                                pks = []
                                for c in range(2):
                                    pk = 3 + st_["sc"] % 4
                                    st_["sc"] += 1
                                    pks.append(pk)
                                    ps_ = slice(c * 64, (c + 1) * 64)
                                    op("pe", lambda e, pk=pk, ps_=ps_: e.matmul(psb[pk][:, c0:512], lhsT=kh[b][ps_, kt * 128:(kt + 1) * 128], rhs=qh[b][ps_, qb * 512 + c0:(qb + 1) * 512], start=True, stop=True),
                                       rd=[r_kh[b], r_qh[b]], wr=[psr[pk]])
                                for c in range(2):
                                    pk = pks[c]
                                    ei = st_["ec"][c] % NE
                                    st_["ec"][c] += 1
                                    Et = Eb[c][ei]
                                    rE = r_E[c][ei]
                                    op("act", lambda e, pk=pk, Et=Et: e.activation(out=Et[:, c0:512], in_=psb[pk][:, c0:512], func=AF.Exp, scale=0.125), rd=[psr[pk]], wr=[rE])
                                    if r >= 0:
                                        op("pool", lambda e, Et=Et: e.tensor_tensor(out=Et[:, c0:c0 + 128], in0=Et[:, c0:c0 + 128], in1=tri_b[:], op=ALU.mult), rd=[rE], wr=[rE])
                                    info[(kt, c)] = (Et, rE)

                            def pv(kt):
                                r = kt - 4 * qb
                                for c in range(2):
                                    Et, rE = info[(kt, c)]
                                    for qs in range(max(r, 0), 4):
                                        bk, aap = acc_ap(c, qs)
                                        first = not started[bk]
                                        started[bk] = True
                                        last = (kt == 4 * qb + qs)
                                        op("pe", lambda e, aap=aap, qs=qs, first=first, last=last, Et=Et: e.matmul(aap, lhsT=Et[:, qs * 128:(qs + 1) * 128], rhs=vh[b][:, kt, 0:129], start=first, stop=last, skip_group_check=True),
                                           rd=[rE, r_vh[b]], wr=[psr[bk]])

                            for i in range(nkt + LA):
                                if i < nkt:
                                    qk(i)
                                if i - LA >= 0:
                                    pv(i - LA)
                                tick()
                                tick()
                            ab = st_["blk"] % 2
                            st_["blk"] += 1
                            for bk, ncol in ((0, 387), (1, 387), (2, 258)):
                                op("dve", lambda e, bk=bk, ncol=ncol: e.tensor_copy(out=accs[ab][:, bk, 0:ncol], in_=psb[bk][:, 0:ncol]), rd=[psr[bk]], wr=[r_accs[ab]])
                            sview0 = accs[ab][:, 0, 0:387].rearrange("p (q e) -> p q e", e=129)[:, :, 128]
                            sview1 = accs[ab][:, 1, 0:387].rearrange("p (q e) -> p q e", e=129)[:, :, 128]
                            op("dve", lambda e: e.reciprocal(out=smb[ab][:, 0:3], in_=sview0), rd=[r_accs[ab]], wr=[r_smb[ab]])
                            op("dve", lambda e: e.reciprocal(out=smb[ab][:, 3:4], in_=accs[ab][:, 2, 128:129]), rd=[r_accs[ab]], wr=[r_smb[ab]])
                            op("dve", lambda e: e.reciprocal(out=smb[ab][:, 4:7], in_=sview1), rd=[r_accs[ab]], wr=[r_smb[ab]])
                            op("dve", lambda e: e.reciprocal(out=smb[ab][:, 7:8], in_=accs[ab][:, 2, 257:258]), rd=[r_accs[ab]], wr=[r_smb[ab]])
                            op("dve", lambda e: e.tensor_scalar(out=smb[ab][:, 4:8], in0=smb[ab][:, 4:8], scalar1=neglam[:, 0:1], scalar2=None, op0=ALU.mult), rd=[r_smb[ab]], wr=[r_smb[ab]])
                            for qs in range(4):
                                a0 = acc_sb(ab, 0, qs)
                                a1 = acc_sb(ab, 1, qs)
                                tile_i = qb * 4 + qs
                                op("dve", lambda e, a0=a0, qs=qs: e.tensor_scalar(out=o1[:], in0=a0[:, 0:128], scalar1=smb[ab][:, qs:qs + 1], scalar2=None, op0=ALU.mult), rd=[r_accs[ab], r_smb[ab]], wr=[r_o1])
                                op("dve", lambda e, a1=a1, qs=qs: e.scalar_tensor_tensor(out=o2[:], in0=a1[:, 0:128], scalar=smb[ab][:, 4 + qs:5 + qs], in1=o1[:], op0=ALU.mult, op1=ALU.add), rd=[r_accs[ab], r_smb[ab], r_o1], wr=[r_o2])
                                op("dve", lambda e, qs=qs: e.scalar_tensor_tensor(out=jk[:], in0=o2[:], scalar=1.0, in1=o2[:], op0=ALU.mult, op1=ALU.mult, accum_out=smb[ab][:, 8 + qs:9 + qs]), rd=[r_o2], wr=[r_jk, r_smb[ab]])
                                op("dve", lambda e, qs=qs, tile_i=tile_i: e.tensor_tensor(out=ogb[ab][:, qs, :], in0=o2[:], in1=gh[b][:, tile_i, :], op=ALU.mult), rd=[r_o2, r_gh[b]], wr=[r_ogb[ab]])
                                op("dve", lambda e, qs=qs: e.tensor_tensor(out=ogb[ab][:, qs, :], in0=ogb[ab][:, qs, :], in1=subgs[:], op=ALU.mult), rd=[r_ogb[ab]], wr=[r_ogb[ab]])

                            def stB(ab=ab):
                                op("act", lambda e: e.activation(out=smb[ab][:, 12:16], in_=smb[ab][:, 8:12], func=AF.Ln, scale=1.0 / 128, bias=EPS), rd=[r_smb[ab]], wr=[r_smb[ab]])
                                op("act", lambda e: e.activation(out=smb[ab][:, 12:16], in_=smb[ab][:, 12:16], func=AF.Exp, scale=-0.5), rd=[r_smb[ab]], wr=[r_smb[ab]])

                            def stC(ab=ab):
                                for qs in range(4):
                                    op("dve", lambda e, qs=qs: e.tensor_scalar(out=ycm[ab][:, qs, :], in0=ogb[ab][:, qs, :], scalar1=smb[ab][:, 12 + qs:13 + qs], scalar2=None, op0=ALU.mult), rd=[r_ogb[ab], r_smb[ab]], wr=[r_ycm[ab]])

                            def stT(ab=ab, h=h, qb=qb):
                                ptv = psb[7][:].bitcast(BF16)
                                for qs in range(4):
                                    op("pe", lambda e, qs=qs: e.transpose(out=ptv[:, qs * 128:(qs + 1) * 128], in_=ycm[ab][:, qs, :], identity=ident_b[:]), rd=[r_ycm[ab]], wr=[psr[7]])
                                op("dve", lambda e: e.tensor_copy(out=yct[ab][:], in_=ptv[:, 0:512]), rd=[psr[7]], wr=[r_yct[ab]])
                                dma("pool", yT[2, h, :, qb * 512:(qb + 1) * 512], yct[ab][:], rd=[r_yct[ab]])

                            defer(4, stB)
                            defer(5, stC)
                            defer(7, stT)
                    flush()
                    tk.barrier()

                if stop == 5:
                    tk.barrier()
                    return nc
                with ExitStack() as s6:
                    WB = sb(s6, "WB", [128, 3, 8, D], BF16)
                    r_WB = Res()
                    wsf = [sb(s6, "wsf%d" % i, [128, D], F32) for i in range(2)]
                    r_wsf = [Res(), Res()]
                    i_ = 0
                    for br in range(3):
                        for kc in range(8):
                            b = i_ % 2
                            dma("sp", wsf[b][:], w_br[l, br, kc * 128:(kc + 1) * 128, :], wr=[r_wsf[b]])
                            if i_ % 2 == 0:
                                op("dve", lambda e, b=b, br=br, kc=kc: e.tensor_copy(out=WB[:, br, kc, :], in_=wsf[b][:]), rd=[r_wsf[b]], wr=[r_WB])
                            else:
                                op("act", lambda e, b=b, br=br, kc=kc: e.activation(out=WB[:, br, kc, :], in_=wsf[b][:], func=AF.Copy), rd=[r_wsf[b]], wr=[r_WB])
                            i_ += 1
                    yTb = [sb(s6, "yTb%d" % i, [128, 3, 8, 512], BF16) for i in range(2)]
                    r_yTb = [Res(), Res()]
                    gmb = [sb(s6, "gmb%d" % i, [128, 3, 512], BF16) for i in range(2)]
                    r_gmb = [Res(), Res()]
                    t0b = sb(s6, "t0b", [128, 512], F32)
                    t1b = sb(s6, "t1b", [128, 512], F32)
                    t2b = sb(s6, "t2b", [128, 512], F32)
                    r_t0, r_t1, r_t2 = Res(), Res(), Res()
                    mst = [sb(s6, "mst%d" % i, [128, 512], BF16) for i in range(2)]
                    r_mst = [Res(), Res()]

                    def y_load(tb):
                        b = tb % 2
                        t0 = tb * 512
                        for br in range(3):
                            dma("sp", yTb[b][:, br, :, :], yT[br, :, :, t0:t0 + 512].rearrange("c p s -> p c s"), wr=[r_yTb[b]])

                    def gm_load(tb, dc, gi):
                        t0 = tb * 512
                        b = gi % 2
                        dma("sp", gmb[b][:], gmT[:, :, t0:t0 + 512].rearrange("(br dc) p s -> dc p br s", br=3)[dc], wr=[r_gmb[b]])

                    y_load(0)
                    gi = 0
                    gm_load(0, 0, 0)
                    pc = 0
                    for tb in range(NB):
                        b = tb % 2
                        if tb + 1 < NB:
                            y_load(tb + 1)
                        for dc in range(16):
                            nxt = tb * 16 + dc + 1
                            if nxt < NB * 16:
                                gm_load(nxt // 16, nxt % 16, gi + 1)
                            gb = gi % 2
                            gi += 1
                            pks = []
                            for br in range(3):
                                pk = pc % 8
                                pc += 1
                                pks.append(pk)
                                for kc in range(8):
                                    op("pe", lambda e, pk=pk, br=br, kc=kc, dc=dc, b=b: e.matmul(psb[pk][:, 0:512], lhsT=WB[:, br, kc, dc * 128:(dc + 1) * 128], rhs=yTb[b][:, br, kc, :], start=(kc == 0), stop=(kc == 7)),
                                       rd=[r_WB, r_yTb[b]], wr=[psr[pk]])
                            op("dve", lambda e, pk=pks[0], gb=gb: e.tensor_tensor(out=t0b[:], in0=psb[pk][:, 0:512], in1=gmb[gb][:, 0, :], op=ALU.mult), rd=[psr[pks[0]], r_gmb[gb]], wr=[r_t0])
                            op("dve", lambda e, pk=pks[1], gb=gb: e.tensor_tensor(out=t1b[:], in0=psb[pk][:, 0:512], in1=gmb[gb][:, 1, :], op=ALU.mult), rd=[psr[pks[1]], r_gmb[gb]], wr=[r_t1])
                            op("dve", lambda e, pk=pks[2], gb=gb: e.tensor_tensor(out=t2b[:], in0=psb[pk][:, 0:512], in1=gmb[gb][:, 2, :], op=ALU.mult), rd=[psr[pks[2]], r_gmb[gb]], wr=[r_t2])
                            op("pool", lambda e: e.tensor_tensor(out=t0b[:], in0=t0b[:], in1=t1b[:], op=ALU.add), rd=[r_t0, r_t1], wr=[r_t0])
                            mb = (tb * 16 + dc) % 2
                            op("pool", lambda e, mb=mb: e.tensor_tensor(out=mst[mb][:], in0=t0b[:], in1=t2b[:], op=ALU.add), rd=[r_t0, r_t2], wr=[r_mst[mb]])
                            dma("pool", mT[dc, :, tb * 512:(tb + 1) * 512], mst[mb][:], rd=[r_mst[mb]])
                    tk.barrier()

                if stop == 6:
                    tk.barrier()
                    return nc
                with ExitStack() as s7:
                    WO = sb(s7, "WO", [128, KC, D], BF16)
                    r_WO = Res()
                    wsf = [sb(s7, "wsf%d" % i, [128, D], F32) for i in range(2)]
                    r_wsf = [Res(), Res()]
                    for kc in range(KC):
                        b = kc % 2
                        dma("sp", wsf[b][:], w_out[l, kc * 128:(kc + 1) * 128, :], wr=[r_wsf[b]])
                        if kc % 2 == 0:
                            op("dve", lambda e, b=b, kc=kc: e.tensor_copy(out=WO[:, kc, :], in_=wsf[b][:]), rd=[r_wsf[b]], wr=[r_WO])
                        else:
                            op("act", lambda e, b=b, kc=kc: e.activation(out=WO[:, kc, :], in_=wsf[b][:], func=AF.Copy), rd=[r_wsf[b]], wr=[r_WO])
                    pg = sb(s7, "pg", [128, D], F32)
                    r_pg = Res()
                    dma("sp", pg[:], postg[l], wr=[r_pg])
                    mtb = [sb(s7, "mtb%d" % i, [128, KC, 128], BF16) for i in range(2)]
                    xr = [sb(s7, "xr%d" % i, [128, D], F32) for i in range(2)]
                    r_mtb = [Res(), Res()]
                    r_xr = [Res(), Res()]
                    ot = [sb(s7, "ot%d" % i, [128, D], F32) for i in range(2)]
                    r_ot = [Res(), Res()]
                    jq = sb(s7, "jq", [128, 512], BF16)
                    r_jq = Res()
                    q4 = [sb(s7, "q4_%d" % i, [128, 8], F32) for i in range(2)]
                    r_q4 = [Res(), Res()]

                    def o_load(tt):
                        b = tt % 2
                        t0 = tt * 128
                        dma("sp", mtb[b][:], mT[:, :, t0:t0 + 128].rearrange("c p s -> p c s"), wr=[r_mtb[b]])
                        dma("sp", xr[b][:], x_src[t0:t0 + 128, :], wr=[r_xr[b]])

                    o_load(0)
                    for tt in range(NT):
                        b = tt % 2
                        if tt + 1 < NT:
                            o_load(tt + 1)
                        base = 4 * (tt % 2)
                        for eb in range(4):
                            pk = base + eb
                            for dc in range(KC):
                                op("pe", lambda e, pk=pk, dc=dc, eb=eb, b=b: e.matmul(psb[pk][:, 0:512], lhsT=mtb[b][:, dc, :], rhs=WO[:, dc, eb * 512:(eb + 1) * 512], start=(dc == 0), stop=(dc == KC - 1)),
                                   rd=[r_mtb[b], r_WO], wr=[psr[pk]])
                            op("act", lambda e, pk=pk, eb=eb, b=b: e.activation(out=jq[:], in_=psb[pk][:, 0:512], func=AF.Square, accum_out=q4[b][:, eb:eb + 1]), rd=[psr[pk]], wr=[r_jq, r_q4[b]])
                        op("dve", lambda e, b=b: e.tensor_reduce(out=q4[b][:, 4:5], in_=q4[b][:, 0:4], axis=AX.X, op=ALU.add), rd=[r_q4[b]], wr=[r_q4[b]])
                        op("act", lambda e, b=b: e.activation(out=q4[b][:, 5:6], in_=q4[b][:, 4:5], func=AF.Sqrt, scale=1.0 / D, bias=EPS), rd=[r_q4[b]], wr=[r_q4[b]])
                        op("dve", lambda e, b=b: e.reciprocal(out=q4[b][:, 6:7], in_=q4[b][:, 5:6]), rd=[r_q4[b]], wr=[r_q4[b]])
                        for eb in range(4):
                            pk = base + eb
                            es_ = slice(eb * 512, (eb + 1) * 512)
                            op("dve", lambda e, pk=pk, es_=es_, b=b: e.scalar_tensor_tensor(out=ot[b][:, es_], in0=psb[pk][:, 0:512], scalar=q4[b][:, 6:7], in1=pg[:, es_], op0=ALU.mult, op1=ALU.mult),
                               rd=[psr[pk], r_q4[b], r_pg], wr=[r_ot[b]])
                        op("pool", lambda e, b=b: e.tensor_tensor(out=ot[b][:], in0=ot[b][:], in1=xr[b][:], op=ALU.add), rd=[r_ot[b], r_xr[b]], wr=[r_ot[b]])
                        dma("pool", x_dst[tt * 128:(tt + 1) * 128, :], ot[b][:], rd=[r_ot[b]])
                    tk.barrier()
        tk.barrier()
    return nc


def _const_tables(S):
    f32 = np.float32
    pos = np.arange(S, dtype=f32)
    ret_freq = (1.0 / (f32(10000.0) ** np.linspace(0.0, 1.0, 64, dtype=f32))).astype(f32)
    ang = (pos[:, None] * ret_freq[None, :]).astype(f32)
    c, s = np.cos(ang).astype(f32), np.sin(ang).astype(f32)
    Cq = np.repeat(c, 2, axis=1)
    Sq = np.stack([-s, s], axis=-1).reshape(S, 128)
    ks = f32(128.0 ** -0.5)
    ropeR = np.tile(np.stack([Cq, Sq, Cq * ks, Sq * ks]).astype(f32), (1, 1, 2))
    inv = (f32(500000.0) ** (-np.arange(0, 16, 2, dtype=f32) / f32(16))).astype(f32)
    angd = (pos[:, None] * inv[None, :]).astype(f32)
    cd, sd = np.cos(angd).astype(f32), np.sin(angd).astype(f32)
    Cf = np.ones((S, 256), f32)
    Sa = np.zeros((S, 256), f32)
    Sb = np.zeros((S, 256), f32)
    for blk in range(4):
        o = blk * 64
        Cf[:, o:o + 8] = cd
        Cf[:, o + 8:o + 16] = cd
        Sa[:, o:o + 8] = -sd
        Sb[:, o + 8:o + 16] = sd
    ropeD = np.stack([Cf, Sa, Sb]).astype(f32)
    H = 8
    log_g = np.log1p(-np.exp2(-5.0 - np.arange(H, dtype=f32))).astype(f32)
    idx = np.arange(128, dtype=f32)
    rel = idx[:, None] - idx[None, :]
    intra = np.where(rel[None] >= 0, np.exp(log_g[:, None, None] * np.maximum(rel, 0.0)[None]), 0.0).astype(f32)
    intraT = np.ascontiguousarray(intra.transpose(2, 0, 1))
    kdec = np.ascontiguousarray(np.exp(log_g[:, None] * (127.0 - idx)[None, :]).astype(f32).T)
    qd = np.exp(log_g[:, None] * (idx + 1.0)[None, :]).astype(f32)
    qdec = np.ascontiguousarray(np.broadcast_to(np.tile(qd, (1, 4))[None], (128, 8, 512))).astype(f32)
    ident = np.eye(128, dtype=f32)
    tri = (idx[None, :] >= idx[:, None]).astype(f32)
    return dict(ropeR=ropeR, ropeD=ropeD, intraT=intraT, kdec=kdec, qdec=qdec, ident=ident, tri=tri)


def _prep_weights(inp, depth):
    f32 = np.float32
    cols = np.zeros((depth, 128, 80), f32)
    lruw = np.zeros((depth, 2, 8, 128, 128), f32)
    for l in range(depth):
        cols[l, :, 0:16] = inp["pre_norm"][l].reshape(16, 128).T
        cols[l, :, 16:48] = inp["conv_w"][l].reshape(4, 8, 128).transpose(2, 0, 1).reshape(128, 32)
        cols[l, :, 48:56] = inp["conv_b"][l].reshape(8, 128).T
        cols[l, :, 56:64] = inp["lru_ba"][l].reshape(8, 128).T
        cols[l, :, 64:72] = inp["lru_bx"][l].reshape(8, 128).T
        cols[l, :, 72:80] = inp["lru_lambda"][l].reshape(8, 128).T
        for wi, name in enumerate(("lru_wa", "lru_wx")):
            w = inp[name][l]
            for cc in range(8):
                for j in range(2):
                    lruw[l, wi, cc, j * 64:(j + 1) * 64, j * 64:(j + 1) * 64] = w[cc * 2 + j]
    postg = np.ascontiguousarray(np.broadcast_to(inp["post_norm"][:depth, None, :], (depth, 128, D))).astype(f32)
    subg = np.ascontiguousarray(np.broadcast_to(inp["diff_subln"][:depth, None, :], (depth, 128, 128))).astype(f32)
    dlam = np.ascontiguousarray(np.broadcast_to(inp["diff_lambda"][:depth].reshape(depth, 1, 256), (depth, 128, 256))).astype(f32)
    w_br = np.ascontiguousarray(np.stack([inp["w_branch_a"][:depth], inp["w_branch_b"][:depth], inp["w_branch_c"][:depth]], axis=1)).astype(f32)
    return dict(cols=cols, lruw=lruw, postg=postg, subg=subg, dlam=dlam, w_br=w_br,
                w_in=np.ascontiguousarray(inp["w_in"][:depth]), w_out=np.ascontiguousarray(inp["w_out"][:depth]))


def kernel(**inputs):
    x = np.asarray(inputs["x"], dtype=np.float32)
    B, S, _ = x.shape
    depth = inputs["w_in"].shape[0]
    nc = build(S, depth)
    shared = _prep_weights({k: np.asarray(v) for k, v in inputs.items()}, depth)
    shared.update(_const_tables(S))
    in_maps = []
    for b in range(B):
        m = dict(shared)
        m["x"] = np.ascontiguousarray(x[b])
        in_maps.append(m)
    res = run_bass_kernel_spmd(nc, in_maps, core_ids=list(range(B)))
    return np.stack([np.asarray(r["y"], dtype=np.float32) for r in res.results], axis=0)
```

```python
import math
from contextlib import ExitStack
import numpy as np
import concourse.bass as bass
import concourse.mybir as mybir
from concourse.bass_utils import run_bass_kernel_spmd

F32 = mybir.dt.float32
BF16 = mybir.dt.bfloat16
AF = mybir.ActivationFunctionType
ALU = mybir.AluOpType
AX = mybir.AxisListType

D = 2048
DIN = 16384
EPS = 1e-6
KC = 16


class Res:
    __slots__ = ("w", "rd")

    def __init__(self):
        self.w = None
        self.rd = {}


class TK:
    SEM_LIMIT = 8000

    def __init__(self, nc, es):
        self.nc = nc
        self.E = {"pe": nc.tensor, "act": nc.scalar, "dve": nc.vector, "pool": nc.gpsimd, "sp": nc.sync}
        self.es = es
        self.csem = {}
        self.ccnt = {}
        self.retired = []
        self.nsem = 0
        for e in ("pe", "act", "dve", "pool"):
            self.csem[e] = es.enter_context(nc.semaphore("c_" + e))
            self.ccnt[e] = 0
        self.dsem = {
            "sp": [es.enter_context(nc.semaphore("dsp%d" % i)) for i in range(32)],
            "pool": [es.enter_context(nc.semaphore("dpl%d" % i)) for i in range(16)],
            "act": [es.enter_context(nc.semaphore("dac%d" % i)) for i in range(2)],
        }
        self.didx = {"sp": 0, "pool": 0, "act": 0}
        self.dtot = {}
        self.waited = {e: {} for e in self.E}

    def _wait(self, eng, ev):
        sem, val, _ = ev
        k = id(sem)
        if self.waited[eng].get(k, 0) >= val:
            return
        self.E[eng].wait_ge(sem, val)
        self.waited[eng][k] = val

    def op(self, eng, fn, rd=(), wr=()):
        for r in rd:
            if r.w is not None and not (r.w[2] == "pe" and eng == "pe"):
                self._wait(eng, r.w)
        for w in wr:
            if w.w is not None and not (w.w[2] == "pe" and eng == "pe"):
                self._wait(eng, w.w)
            for ev in w.rd.values():
                self._wait(eng, ev)
        if self.ccnt[eng] >= self.SEM_LIMIT:
            self.retired.append((self.csem[eng], self.ccnt[eng], eng))
            self.nsem += 1
            self.csem[eng] = self.es.enter_context(self.nc.semaphore("c_%s_%d" % (eng, self.nsem)))
            self.ccnt[eng] = 0
        inst = fn(self.E[eng])
        self.ccnt[eng] += 1
        inst.then_inc(self.csem[eng], 1)
        ev = (self.csem[eng], self.ccnt[eng], eng)
        for r in rd:
            r.rd[eng] = ev
        for w in wr:
            w.w = ev
            w.rd = {}
        return ev

    def dma(self, q, out, in_, rd=(), wr=()):
        for r in rd:
            if r.w is not None:
                self._wait(q, r.w)
        for w in wr:
            if w.w is not None:
                self._wait(q, w.w)
            for ev in w.rd.values():
                self._wait(q, ev)
        pool = self.dsem[q]
        i = self.didx[q]
        self.didx[q] = (i + 1) % len(pool)
        sem = pool[i]
        tot = self.dtot.get(id(sem), 0)
        if tot > 0:
            self._wait(q, (sem, tot, "dma"))
        self.E[q].dma_start(out=out, in_=in_).then_inc(sem, 16)
        tot += 16
        self.dtot[id(sem)] = tot
        ev = (sem, tot, "dma")
        for r in rd:
            r.rd[("d", id(sem))] = ev
        for w in wr:
            w.w = ev
            w.rd = {}
        return ev

    def barrier(self):
        for e in self.E:
            for ev in self.retired:
                self._wait(e, ev)
            for pe, s in self.csem.items():
                if self.ccnt[pe] > 0:
                    self._wait(e, (s, self.ccnt[pe], pe))
            for q, pool in self.dsem.items():
                for s in pool:
                    t = self.dtot.get(id(s), 0)
                    if t > 0:
                        self._wait(e, (s, t, "dma"))


def build(S, depth, dbg=(), stop=99, kinds=None):
    NT = S // 128
    NB = S // 512
    PASS = min(S, 2048)
    NPASS = S // PASS
    PT = PASS // 128
    PB = PASS // 512
    nc = bass.Bass("TRN2", target_bir_lowering=False)

    def din(name, shape, dt=F32):
        return nc.dram_tensor(name, list(shape), dt, kind="ExternalInput").ap()

    def dscr(name, shape, dt=BF16):
        kind = "ExternalOutput" if name in dbg else "Internal"
        return nc.dram_tensor(name, list(shape), dt, kind=kind).ap()

    x_in = din("x", [S, D])
    w_in = din("w_in", [depth, D, DIN])
    w_br = din("w_br", [depth, 3, 1024, D])
    w_out = din("w_out", [depth, D, D])
    cols = din("cols", [depth, 128, 80])
    postg = din("postg", [depth, 128, D])
    subg = din("subg", [depth, 128, 128])
    dlam = din("dlam", [depth, 128, 256])
    lruw = din("lruw", [depth, 2, 8, 128, 128])
    ident = din("ident", [128, 128])
    tri = din("tri", [128, 128])
    ropeR = din("ropeR", [4, S, 256])
    ropeD = din("ropeD", [3, S, 256])
    intraT = din("intraT", [128, 8, 128])
    kdec = din("kdec", [128, 8])
    qdec = din("qdec", [128, 8, 512])
    y_out = nc.dram_tensor("y", [S, D], F32, kind="ExternalOutput").ap()
    xmid = dscr("xmid", [S, D], F32)
    xaT = dscr("xaT", [8, 128, S])
    gaT = dscr("gaT", [8, 128, S])
    gmT = dscr("gmT", [48, 128, S])
    QrT = dscr("QrT", [8, 128, S])
    KrT = dscr("KrT", [8, 128, S])
    Kr = dscr("Kr", [S, 1024])
    Vr = dscr("Vr", [S, 1024])
    Gr = dscr("Gr", [S, 1024])
    QdT = dscr("QdT", [8, 128, S])
    KdT = dscr("KdT", [8, 128, S])
    Vd = dscr("Vd", [S, 1024])
    Gd = dscr("Gd", [S, 1024])
    yT = dscr("yT", [3, 8, 128, S])
    mT = dscr("mT", [16, 128, S])

    with ExitStack() as es:
        tk = TK(nc, es)
        op = tk.op
        dma = tk.dma

        uid = [0]

        def sb(stack, name, shape, dt):
            uid[0] += 1
            return stack.enter_context(nc.sbuf_tensor("%s_u%d" % (name, uid[0]), list(shape), dt))

        psb = [es.enter_context(nc.psum_tensor("ps%d" % i, [128, 512], F32)) for i in range(8)]
        psr = [Res() for _ in range(8)]

        ident_f = sb(es, "ident_f", [128, 128], F32)
        ident_b = sb(es, "ident_b", [128, 128], BF16)
        tri_f = sb(es, "tri_f", [128, 128], F32)
        tri_b = sb(es, "tri_b", [128, 128], BF16)
        r_c = Res()
        dma("sp", ident_f[:], ident, wr=[r_c])
        dma("sp", tri_f[:], tri, wr=[r_c])
        op("dve", lambda e: e.tensor_copy(out=ident_b[:], in_=ident_f[:]), rd=[r_c], wr=[r_c])
        op("dve", lambda e: e.tensor_copy(out=tri_b[:], in_=tri_f[:]), rd=[r_c], wr=[r_c])
        tk.barrier()

        for l in range(depth):
            lam_init = 0.8 - 0.6 * math.exp(-0.3 * l)
            x_src = x_in if l == 0 else xmid
            x_dst = y_out if l == depth - 1 else xmid
            with ExitStack() as ls:
                colst = sb(ls, "colst", [128, 80], F32)
                s8 = sb(ls, "s8", [128, 8], F32)
                s16 = sb(ls, "s16", [128, 8], F32)
                tmp8 = sb(ls, "tmp8", [128, 8], F32)
                dl = sb(ls, "dl", [128, 256], F32)
                dprod = sb(ls, "dprod", [128, 128], F32)
                dsum = sb(ls, "dsum", [128, 4], F32)
                neglam = sb(ls, "neglam", [128, 1], F32)
                subgs = sb(ls, "subgs", [128, 128], F32)
                r_p = Res()
                dma("sp", colst[:], cols[l], wr=[r_p])
                dma("sp", dl[:], dlam[l], wr=[r_p])
                dma("sp", subgs[:], subg[l], wr=[r_p])
                op("act", lambda e: e.activation(out=tmp8[:], in_=colst[:, 72:80], func=AF.Exp, scale=-1.0), rd=[r_p], wr=[r_p])
                op("act", lambda e: e.activation(out=tmp8[:], in_=tmp8[:], func=AF.Ln, bias=1.0), rd=[r_p], wr=[r_p])
                op("dve", lambda e: e.tensor_scalar(out=s8[:], in0=tmp8[:], scalar1=-8.0, scalar2=None, op0=ALU.mult), rd=[r_p], wr=[r_p])
                op("dve", lambda e: e.tensor_scalar(out=s16[:], in0=tmp8[:], scalar1=-16.0, scalar2=None, op0=ALU.mult), rd=[r_p], wr=[r_p])
                op("dve", lambda e: e.tensor_tensor(out=dprod[:, 0:64], in0=dl[:, 0:64], in1=dl[:, 64:128], op=ALU.mult), rd=[r_p], wr=[r_p])
                op("dve", lambda e: e.tensor_tensor(out=dprod[:, 64:128], in0=dl[:, 128:192], in1=dl[:, 192:256], op=ALU.mult), rd=[r_p], wr=[r_p])
                op("dve", lambda e: e.tensor_reduce(out=dsum[:, 0:1], in_=dprod[:, 0:64], axis=AX.X, op=ALU.add), rd=[r_p], wr=[r_p])
                op("dve", lambda e: e.tensor_reduce(out=dsum[:, 1:2], in_=dprod[:, 64:128], axis=AX.X, op=ALU.add), rd=[r_p], wr=[r_p])
                op("act", lambda e: e.activation(out=dsum[:, 2:4], in_=dsum[:, 0:2], func=AF.Exp), rd=[r_p], wr=[r_p])
                op("dve", lambda e: e.tensor_tensor(out=neglam[:], in0=dsum[:, 3:4], in1=dsum[:, 2:3], op=ALU.subtract), rd=[r_p], wr=[r_p])
                op("dve", lambda e: e.tensor_scalar(out=neglam[:], in0=neglam[:], scalar1=-lam_init, scalar2=None, op0=ALU.add), rd=[r_p], wr=[r_p])
                op("dve", lambda e: e.tensor_scalar(out=subgs[:], in0=subgs[:], scalar1=1.0 - lam_init, scalar2=None, op0=ALU.mult), rd=[r_p], wr=[r_p])
                tk.barrier()

                for p in range(NPASS):
                    tok_p = p * PASS
                    with ExitStack() as s1:
                        hT = sb(s1, "hT", [128, KC, PASS], BF16)
                        r_hT = [Res() for _ in range(PT)]
                        with ExitStack() as s0:
                            xt = [sb(s0, "xt%d" % i, [128, D], F32) for i in range(2)]
                            xn = [sb(s0, "xn%d" % i, [128, D], BF16) for i in range(2)]
                            junk = sb(s0, "junk", [128, D], BF16)
                            st = [sb(s0, "st%d" % i, [128, 4], F32) for i in range(2)]
                            r_xt = [Res(), Res()]
                            r_xn = [Res(), Res()]
                            r_junk = Res()
                            r_st = [Res(), Res()]
                            dma("sp", xt[0][:], x_src[tok_p:tok_p + 128, :], wr=[r_xt[0]])
                            for tt in range(PT):
                                b = tt % 2
                                if tt + 1 < PT:
                                    t1 = tok_p + (tt + 1) * 128
                                    dma("sp", xt[1 - b][:], x_src[t1:t1 + 128, :], wr=[r_xt[1 - b]])
                                op("act", lambda e, b=b: e.activation(out=junk[:], in_=xt[b][:], func=AF.Square, accum_out=st[b][:, 0:1]),
                                   rd=[r_xt[b]], wr=[r_junk, r_st[b]])
                                op("act", lambda e, b=b: e.activation(out=st[b][:, 1:2], in_=st[b][:, 0:1], func=AF.Sqrt, scale=1.0 / D, bias=EPS),
                                   rd=[r_st[b]], wr=[r_st[b]])
                                op("dve", lambda e, b=b: e.reciprocal(out=st[b][:, 2:3], in_=st[b][:, 1:2]), rd=[r_st[b]], wr=[r_st[b]])
                                op("dve", lambda e, b=b: e.tensor_scalar(out=xn[b][:], in0=xt[b][:], scalar1=st[b][:, 2:3], scalar2=None, op0=ALU.mult),
                                   rd=[r_xt[b], r_st[b]], wr=[r_xn[b]])
                                for half in range(2):
                                    pbk = 6 + half
                                    ptv = psb[pbk][:].bitcast(BF16)
                                    for j in range(8):
                                        kc = half * 8 + j
                                        op("pe", lambda e, b=b, kc=kc, j=j, ptv=ptv: e.transpose(out=ptv[:, j * 128:(j + 1) * 128], in_=xn[b][:, kc * 128:(kc + 1) * 128], identity=ident_b[:]),
                                           rd=[r_xn[b]], wr=[psr[pbk]])
                                    src = ptv.rearrange("p (j t) -> p j t", j=8)
                                    dst = hT[:, half * 8:(half + 1) * 8, tt * 128:(tt + 1) * 128]
                                    if half == 0:
                                        op("act", lambda e, src=src, dst=dst: e.activation(out=dst, in_=src, func=AF.Copy), rd=[psr[pbk]], wr=[r_hT[tt]])
                                    else:
                                        op("dve", lambda e, src=src, dst=dst: e.tensor_copy(out=dst, in_=src), rd=[psr[pbk]], wr=[r_hT[tt]])
                            tk.barrier()
                            if stop == 1:
                                return nc
                        with ExitStack() as s2:
                            wst = [sb(s2, "wst%d" % i, [128, KC, 256], F32) for i in range(2)]
                            wbf = [sb(s2, "wbf%d" % i, [128, KC, 256], BF16) for i in range(2)]
                            r_wst = [Res(), Res()]
                            r_wbf = [Res(), Res()]
                            fst = [sb(s2, "fst%d" % i, [128, 512], BF16) for i in range(2)]
                            r_fst = [Res(), Res()]
                            tmb = [sb(s2, "tmb%d" % i, [128, 256], BF16) for i in range(2)]
                            r_tmb = [Res(), Res()]
                            ra = sb(s2, "ra", [128, 256], F32)
                            rb = sb(s2, "rb", [128, 256], F32)
                            r_ra = Res()
                            r_rb = Res()
                            tst = [sb(s2, "tst%d" % i, [128, 2, 512], BF16) for i in range(2)]
                            r_tst = [Res(), Res()]
                            rtt = [sb(s2, "rtt%d" % i, [128, 2, 256], F32) for i in range(2)]
                            r_rtt = [Res(), Res()]
                            dtt = [sb(s2, "dtt%d" % i, [128, 3, 256], F32) for i in range(2)]
                            r_dtt = [Res(), Res()]
                            rc = sb(s2, "rc", [128, 256], F32)
                            r_rc = Res()
                            rbd = sb(s2, "rbd", [128, 256], F32)
                            r_rbd = Res()
                            op("pool", lambda e: e.memset(rbd[:], 0.0), wr=[r_rbd])
                            op("pool", lambda e: e.memset(rc[:], 0.0), wr=[r_rc])
                            r_tab = Res()
                            groups = []
                            for g in range(8):
                                groups.append(("xa", g * 128, 128, g))
                            for kind, base in (("qr", 2048), ("kr", 3072), ("vr", 4096), ("qd", 6144), ("kd", 7168), ("vd", 8192)):
                                for g in range(4):
                                    groups.append((kind, base + g * 256, 256, g))
                            for g in range(8):
                                groups.append(("ga", 1024 + g * 128, 128, g))
                            for kind, base in (("gr", 5120), ("gd", 9216)):
                                for g in range(4):
                                    groups.append((kind, base + g * 256, 256, g))
                            for g in range(48):
                                groups.append(("gm", 10240 + g * 128, 128, g))
                            if kinds is not None:
                                groups = [g_ for g_ in groups if g_[0] in kinds]
                            wv = w_in[l].rearrange("(kc p) n -> p kc n", p=128)

                            def wload(gi):
                                _, c0, ncol, _ = groups[gi]
                                b = gi % 2
                                dma("sp", wst[b][:, :, 0:ncol], wv[:, :, c0:c0 + ncol], wr=[r_wst[b]])

                            def wcast(gi):
                                _, c0, ncol, _ = groups[gi]
                                b = gi % 2
                                busy_dve = gi >= 1 and groups[gi - 1][0] in ("qr", "kr", "qd", "kd")
                                for kc in range(KC):
                                    if kc % 2 == 0 and not busy_dve:
                                        op("dve", lambda e, b=b, kc=kc, ncol=ncol: e.tensor_scalar(out=wbf[b][:, kc, 0:ncol], in0=wst[b][:, kc, 0:ncol], scalar1=colst[:, kc:kc + 1], scalar2=None, op0=ALU.mult),
                                           rd=[r_wst[b]], wr=[r_wbf[b]])
                                    else:
                                        op("act", lambda e, b=b, kc=kc, ncol=ncol: e.activation(out=wbf[b][:, kc, 0:ncol], in_=wst[b][:, kc, 0:ncol], func=AF.Copy, scale=colst[:, kc:kc + 1]),
                                           rd=[r_wst[b]], wr=[r_wbf[b]])

                            cnt = {"ps": 0, "fst": 0, "tmb": 0, "tst": 0, "pt": 0, "dtt": 0, "rtt": 0}

                            def compute(gi):
                                kind, c0, ncol, g = groups[gi]
                                b = gi % 2
                                if ncol == 128:
                                    dst = {"xa": xaT, "ga": gaT, "gm": gmT}[kind]
                                    func = {"xa": AF.Copy, "ga": AF.Silu, "gm": AF.Sigmoid}[kind]
                                    for tb in range(PB):
                                        pk = cnt["ps"] % 6
                                        cnt["ps"] += 1
                                        for kc in range(KC):
                                            op("pe", lambda e, pk=pk, kc=kc, tb=tb, b=b: e.matmul(psb[pk][:, 0:512], lhsT=wbf[b][:, kc, 0:128], rhs=hT[:, kc, tb * 512:(tb + 1) * 512], start=(kc == 0), stop=(kc == KC - 1)),
                                               rd=[r_wbf[b]] + r_hT[tb * 4:tb * 4 + 4], wr=[psr[pk]])
                                        fb = cnt["fst"] % 2
                                        cnt["fst"] += 1
                                        op("act", lambda e, pk=pk, fb=fb, func=func: e.activation(out=fst[fb][:], in_=psb[pk][:, 0:512], func=func), rd=[psr[pk]], wr=[r_fst[fb]])
                                        t0 = tok_p + tb * 512
                                        dma("pool", dst[g, :, t0:t0 + 512], fst[fb][:], rd=[r_fst[fb]])
                                    return
                                trq = []
                                for tb in range(PB):
                                    need_t = kind in ("qr", "kr", "qd", "kd")
                                    if need_t:
                                        ptk = 6 + cnt["pt"] % 2
                                        cnt["pt"] += 1
                                        ptv = psb[ptk][:].bitcast(BF16).rearrange("p (h t d) -> p h t d", h=2, t=4)
                                    for t4 in range(4):
                                        tt = tb * 4 + t4
                                        tok0 = tok_p + tt * 128
                                        pk = cnt["ps"] % 6
                                        cnt["ps"] += 1
                                        for kc in range(KC):
                                            op("pe", lambda e, pk=pk, kc=kc, tt=tt, b=b: e.matmul(psb[pk][:, 0:256], lhsT=hT[:, kc, tt * 128:(tt + 1) * 128], rhs=wbf[b][:, kc, 0:256], start=(kc == 0), stop=(kc == KC - 1)),
                                               rd=[r_wbf[b], r_hT[tt]], wr=[psr[pk]])
                                        while trq:
                                            trq.pop(0)()
                                        mb = cnt["tmb"] % 2
                                        cnt["tmb"] += 1
                                        pv = psb[pk][:, 0:256]
                                        if kind in ("vr", "vd", "gr", "gd"):
                                            func = AF.Silu if kind[0] == "g" else AF.Copy
                                            op("act", lambda e, pv=pv, mb=mb, func=func: e.activation(out=tmb[mb][:], in_=pv, func=func), rd=[psr[pk]], wr=[r_tmb[mb]])
                                            dst = {"vr": Vr, "vd": Vd, "gr": Gr, "gd": Gd}[kind]
                                            dma("pool", dst[tok0:tok0 + 128, g * 256:(g + 1) * 256], tmb[mb][:], rd=[r_tmb[mb]])
                                            continue
                                        if kind in ("qr", "kr"):
                                            ti = 0 if kind == "qr" else 2
                                            rbi = cnt["rtt"] % 2
                                            cnt["rtt"] += 1
                                            dma("sp", rtt[rbi][:], ropeR[ti:ti + 2, tok0:tok0 + 128, :].rearrange("i p f -> p i f"), wr=[r_rtt[rbi]])
                                            op("dve", lambda e, pv=pv, rbi=rbi: e.tensor_tensor(out=ra[:], in0=pv, in1=rtt[rbi][:, 0, :], op=ALU.mult),
                                               rd=[psr[pk], r_rtt[rbi]], wr=[r_ra])
                                            pvp = pv.rearrange("p (a two) -> p a two", two=2)
                                            rbp = rb[:].rearrange("p (a two) -> p a two", two=2)
                                            stp = rtt[rbi][:, 1, :].rearrange("p (a two) -> p a two", two=2)
                                            op("dve", lambda e, pvp=pvp, rbp=rbp, stp=stp: e.tensor_tensor(out=rbp[:, :, 0], in0=pvp[:, :, 1], in1=stp[:, :, 0], op=ALU.mult),
                                               rd=[psr[pk], r_rtt[rbi]], wr=[r_rb])
                                            op("dve", lambda e, pvp=pvp, rbp=rbp, stp=stp: e.tensor_tensor(out=rbp[:, :, 1], in0=pvp[:, :, 0], in1=stp[:, :, 1], op=ALU.mult),
                                               rd=[psr[pk], r_rtt[rbi]], wr=[r_rb])
                                            op("dve", lambda e, mb=mb: e.tensor_tensor(out=tmb[mb][:], in0=ra[:], in1=rb[:], op=ALU.add), rd=[r_ra, r_rb], wr=[r_tmb[mb]])
                                            if kind == "kr":
                                                dma("pool", Kr[tok0:tok0 + 128, g * 256:(g + 1) * 256], tmb[mb][:], rd=[r_tmb[mb]])
                                        else:
                                            db = cnt["dtt"] % 2
                                            cnt["dtt"] += 1
                                            dma("sp", dtt[db][:], ropeD[:, tok0:tok0 + 128, :].rearrange("i p f -> p i f"), wr=[r_dtt[db]])
                                            op("dve", lambda e, pv=pv, db=db: e.tensor_tensor(out=ra[:], in0=pv, in1=dtt[db][:, 0, :], op=ALU.mult),
                                               rd=[psr[pk], r_dtt[db]], wr=[r_ra])
                                            op("dve", lambda e, pv=pv, db=db: e.tensor_tensor(out=rbd[:, 0:248], in0=pv[:, 8:256], in1=dtt[db][:, 1, 0:248], op=ALU.mult),
                                               rd=[psr[pk], r_dtt[db]], wr=[r_rbd])
                                            op("dve", lambda e, pv=pv, db=db: e.tensor_tensor(out=rc[:, 8:256], in0=pv[:, 0:248], in1=dtt[db][:, 2, 8:256], op=ALU.mult),
                                               rd=[psr[pk], r_dtt[db]], wr=[r_rc])
                                            op("dve", lambda e: e.tensor_tensor(out=ra[:], in0=ra[:], in1=rbd[:], op=ALU.add), rd=[r_ra, r_rbd], wr=[r_ra])
                                            op("dve", lambda e, mb=mb: e.tensor_tensor(out=tmb[mb][:], in0=ra[:], in1=rc[:], op=ALU.add), rd=[r_ra, r_rc], wr=[r_tmb[mb]])
                                        def _tr(ptv=ptv, ptk=ptk, t4=t4, mb=mb):
                                            for hh in range(2):
                                                op("pe", lambda e, hh=hh: e.transpose(out=ptv[:, hh, t4, :], in_=tmb[mb][:, hh * 128:(hh + 1) * 128], identity=ident_b[:]),
                                                   rd=[r_tmb[mb]], wr=[psr[ptk]])
                                        trq.append(_tr)
                                    if need_t:
                                        def _ev(ptk=ptk, tb=tb, kind=kind, g=g):
                                            sbk = cnt["tst"] % 2
                                            cnt["tst"] += 1
                                            src = psb[ptk][:].bitcast(BF16).rearrange("p (h s) -> p h s", h=2)
                                            op("act", lambda e: e.activation(out=tst[sbk][:], in_=src, func=AF.Copy), rd=[psr[ptk]], wr=[r_tst[sbk]])
                                            dst = {"qr": QrT, "kr": KrT, "qd": QdT, "kd": KdT}[kind]
                                            t0 = tok_p + tb * 512
                                            dma("pool", dst[2 * g:2 * g + 2, :, t0:t0 + 512].rearrange("h d s -> d h s"), tst[sbk][:], rd=[r_tst[sbk]])
                                        trq.append(_ev)
                                while trq:
                                    trq.pop(0)()

                            ng = len(groups)
                            wload(0)
                            wload(1)
                            wcast(0)
                            for gi in range(ng):
                                if gi + 1 < ng:
                                    wcast(gi + 1)
                                compute(gi)
                                if gi + 2 < ng:
                                    wload(gi + 2)
                            tk.barrier()

                if stop == 2:
                    tk.barrier()
                    return nc
                def lru_gen(st):
                    xpad = [sb(st, "xpad%d" % i, [128, S + 4], BF16) for i in range(2)]
                    sga = [sb(st, "sga%d" % i, [128, S], BF16) for i in range(2)]
                    r_in = [Res(), Res()]
                    wlf = sb(st, "wlf", [128, 2, 128], F32)
                    wlb = [sb(st, "wlb%d" % i, [128, 2, 128], BF16) for i in range(2)]
                    r_wlf = Res()
                    r_wlb = [Res(), Res()]
                    dg = [sb(st, "dg%d" % i, [128, 4, 128], BF16) for i in range(2)]
                    r_dg = [Res(), Res()]
                    nbc = sb(st, "nbc", [128, 16], F32)
                    r_nbc = Res()
                    xcb = [sb(st, "xcb%d" % i, [128, 512], BF16) for i in range(2)]
                    rr = [sb(st, "rr%d" % i, [128, 512], F32) for i in range(2)]
                    ii = [sb(st, "ii%d" % i, [128, 512], F32) for i in range(2)]
                    aa = [sb(st, "aa%d" % i, [128, 512], F32) for i in range(2)]
                    a2 = [sb(st, "a2%d" % i, [128, 512], F32) for i in range(2)]
                    ya = [sb(st, "ya%d" % i, [128, S], BF16) for i in range(2)]
                    r_xc = [Res(), Res()]
                    r_rr = [Res(), Res()]
                    r_ii = [Res(), Res()]
                    r_aa = [Res(), Res()]
                    r_a2 = [Res(), Res()]
                    r_ya = [Res(), Res()]
                    LB = 7
                    for i in range(2):
                        op("pool", lambda e, i=i: e.memset(xpad[i][:, 0:4], 0.0), wr=[r_in[i]])
                    op("dve", lambda e: e.tensor_scalar(out=nbc[:], in0=colst[:, 56:72], scalar1=-1.0, scalar2=None, op0=ALU.mult), wr=[r_nbc])
                    yield

                    def lru_load(cc):
                        b = cc % 2
                        dma("sp", xpad[b][:, 3:3 + S], xaT[cc], wr=[r_in[b]])
                        dma("sp", sga[b][:], gaT[cc], wr=[r_in[b]])

                    lru_load(0)
                    yield
                    u = 0
                    for cc in range(8):
                        b = cc % 2
                        if cc + 1 < 8:
                            lru_load(cc + 1)
                            yield
                        dma("sp", wlf[:, 0, :], lruw[l, 0, cc], wr=[r_wlf])
                        dma("sp", wlf[:, 1, :], lruw[l, 1, cc], wr=[r_wlf])
                        op("pool", lambda e: e.tensor_copy(out=wlb[b][:], in_=wlf[:]), rd=[r_wlf], wr=[r_wlb[b]])
                        yield
                        for k in range(4):
                            op("pool", lambda e, k=k: e.tensor_scalar(out=dg[b][:, k, :], in0=ident_f[:], scalar1=colst[:, 16 + k * 8 + cc:17 + k * 8 + cc], scalar2=1.0, op0=ALU.mult, op1=ALU.mult),
                               wr=[r_dg[b]])
                            yield
                        for tb in range(NB):
                            ub = u % 2
                            u += 1
                            t0 = tb * 512
                            for k in range(4):
                                op("pe", lambda e, k=k: e.matmul(psb[LB][:, 0:512], lhsT=dg[b][:, k, :], rhs=xpad[b][:, t0 + k:t0 + k + 512], start=(k == 0), stop=(k == 3)),
                                   rd=[r_dg[b], r_in[b]], wr=[psr[LB]])
                            op("dve", lambda e: e.tensor_scalar(out=xcb[ub][:], in0=psb[LB][:, 0:512], scalar1=colst[:, 48 + cc:49 + cc], scalar2=None, op0=ALU.add), rd=[psr[LB]], wr=[r_xc[ub]])
                            yield
                            for gi_, (dstt, r_d) in enumerate(((rr[ub], r_rr[ub]), (ii[ub], r_ii[ub]))):
                                op("pe", lambda e, gi_=gi_: e.matmul(psb[LB][:, 0:512], lhsT=wlb[b][:, gi_, :], rhs=xcb[ub][:], start=True, stop=True),
                                   rd=[r_wlb[b], r_xc[ub]], wr=[psr[LB]])
                                op("act", lambda e, gi_=gi_, dstt=dstt: e.activation(out=dstt[:], in_=psb[LB][:, 0:512], func=AF.Exp, scale=-1.0, bias=nbc[:, gi_ * 8 + cc:gi_ * 8 + cc + 1]),
                                   rd=[psr[LB], r_nbc], wr=[r_d])
                                yield
                                op("dve", lambda e, dstt=dstt: e.tensor_scalar(out=dstt[:], in0=dstt[:], scalar1=1.0, scalar2=None, op0=ALU.add), rd=[r_d], wr=[r_d])
                                yield
                                op("dve", lambda e, dstt=dstt: e.reciprocal(out=dstt[:], in_=dstt[:]), rd=[r_d], wr=[r_d])
                                yield
                            op("act", lambda e: e.activation(out=aa[ub][:], in_=rr[ub][:], func=AF.Exp, scale=s8[:, cc:cc + 1]), rd=[r_rr[ub]], wr=[r_aa[ub]])
                            yield
                            op("act", lambda e: e.activation(out=a2[ub][:], in_=rr[ub][:], func=AF.Exp, scale=s16[:, cc:cc + 1]), rd=[r_rr[ub]], wr=[r_a2[ub]])
                            yield
                            op("act", lambda e: e.activation(out=a2[ub][:], in_=a2[ub][:], func=AF.Ln, scale=-1.0, bias=1.0), rd=[r_a2[ub]], wr=[r_a2[ub]])
                            yield
                            op("act", lambda e: e.activation(out=a2[ub][:], in_=a2[ub][:], func=AF.Exp, scale=0.5), rd=[r_a2[ub]], wr=[r_a2[ub]])
                            yield
                            op("dve", lambda e: e.tensor_tensor(out=ii[ub][:], in0=ii[ub][:], in1=xcb[ub][:], op=ALU.mult), rd=[r_ii[ub], r_xc[ub]], wr=[r_ii[ub]])
                            yield
                            op("dve", lambda e: e.tensor_tensor(out=ii[ub][:], in0=ii[ub][:], in1=a2[ub][:], op=ALU.mult), rd=[r_ii[ub], r_a2[ub]], wr=[r_ii[ub]])
                            yield
                            if tb == 0:
                                op("dve", lambda e: e.tensor_tensor_scan(out=rr[ub][:], data0=aa[ub][:], data1=ii[ub][:], initial=0.0, op0=ALU.mult, op1=ALU.add), rd=[r_aa[ub], r_ii[ub]], wr=[r_rr[ub]])
                            else:
                                op("dve", lambda e: e.tensor_tensor_scan(out=rr[ub][:], data0=aa[ub][:], data1=ii[ub][:], initial=rr[1 - ub][:, 511:512], op0=ALU.mult, op1=ALU.add), rd=[r_aa[ub], r_ii[ub], r_rr[1 - ub]], wr=[r_rr[ub]])
                            yield
                            op("dve", lambda e: e.tensor_tensor(out=ya[b][:, t0:t0 + 512], in0=rr[ub][:], in1=sga[b][:, t0:t0 + 512], op=ALU.mult), rd=[r_rr[ub], r_in[b]], wr=[r_ya[b]])
                            yield
                        dma("pool", yT[0, cc], ya[b][:], rd=[r_ya[b]])
                        yield

                if stop == 3:
                    tk.barrier()
                    return nc
                with ExitStack() as s4:
                    itT = sb(s4, "itT", [128, 8, 128], F32)
                    kdt = sb(s4, "kdt", [128, 8], F32)
                    qdt = sb(s4, "qdt", [128, 8, 512], F32)
                    r_k = Res()
                    dma("sp", itT[:], intraT, wr=[r_k])
                    dma("sp", kdt[:], kdec, wr=[r_k])
                    dma("sp", qdt[:], qdec, wr=[r_k])
                    qT = [sb(s4, "qT%d" % i, [128, 8, 512], BF16) for i in range(2)]
                    kT = [sb(s4, "kT%d" % i, [128, 8, 512], BF16) for i in range(2)]
                    kM = [sb(s4, "kM%d" % i, [128, 4, 1024], BF16) for i in range(2)]
                    vM = [sb(s4, "vM%d" % i, [128, 4, 1024], BF16) for i in range(2)]
                    gM = [sb(s4, "gM%d" % i, [128, 4, 1024], BF16) for i in range(2)]
                    r_q = [Res(), Res()]
                    r_kt = [Res(), Res()]
                    r_km = [Res(), Res()]
                    r_vm = [Res(), Res()]
                    r_gm = [Res(), Res()]
                    qTd = sb(s4, "qTd", [128, 8, 512], BF16)
                    kMd = sb(s4, "kMd", [128, 4, 1024], BF16)
                    r_qTd, r_kMd = Res(), Res()
                    Sf = sb(s4, "Sf", [128, 8, 128], F32)
                    Sb = sb(s4, "Sb", [128, 8, 128], BF16)
                    r_Sf, r_Sb = Res(), Res()
                    sT = [sb(s4, "sT%d" % i, [128, 512], BF16) for i in range(2)]
                    r_sT = [Res(), Res()]
                    bst = sb(s4, "bst", [128, 8, 6], F32)
                    mv = sb(s4, "mv", [128, 8, 2], F32)
                    rs = sb(s4, "rs", [128, 8], F32)
                    nbias = sb(s4, "nbias", [128, 8], F32)
                    r_nb = Res()
                    r_bst, r_mv, r_rs = Res(), Res(), Res()
                    yn = sb(s4, "yn", [128, 1024], F32)
                    ybm = [sb(s4, "ybm%d" % i, [128, 1024], BF16) for i in range(2)]
                    r_yn = Res()
                    r_ybm = [Res(), Res()]
                    rq = []
                    ybt = [sb(s4, "ybt%d" % i, [128, 8, 512], BF16) for i in range(2)]
                    r_ybt = [Res(), Res()]
                    op("dve", lambda e: e.memset(Sf[:], 0.0), wr=[r_Sf])
                    op("pool", lambda e: e.memset(Sb[:], 0.0), wr=[r_Sb])

                    def ret_load(tb):
                        b = tb % 2
                        t0 = tb * 512
                        dma("sp", qT[b][:], QrT[:, :, t0:t0 + 512].rearrange("h d s -> d h s"), wr=[r_q[b]])
                        dma("sp", kT[b][:], KrT[:, :, t0:t0 + 512].rearrange("h d s -> d h s"), wr=[r_kt[b]])
                        dma("sp", kM[b][:], Kr[t0:t0 + 512, :].rearrange("(n p) f -> p n f", p=128), wr=[r_km[b]])
                        dma("sp", vM[b][:], Vr[t0:t0 + 512, :].rearrange("(n p) f -> p n f", p=128), wr=[r_vm[b]])
                        dma("sp", gM[b][:], Gr[t0:t0 + 512, :].rearrange("(n p) f -> p n f", p=128), wr=[r_gm[b]])

                    ret_load(0)
                    pc = 0
                    for tb in range(NB):
                        b = tb % 2
                        if tb + 1 < NB:
                            ret_load(tb + 1)
                        op("pool", lambda e, b=b: e.tensor_tensor(out=qTd[:], in0=qT[b][:], in1=qdt[:], op=ALU.mult), rd=[r_q[b], r_k], wr=[r_qTd])
                        for h in range(8):
                            op("pool", lambda e, b=b, h=h: e.tensor_scalar(out=kMd[:, :, h * 128:(h + 1) * 128], in0=kM[b][:, :, h * 128:(h + 1) * 128], scalar1=kdt[:, h:h + 1], scalar2=1.0, op0=ALU.mult, op1=ALU.mult),
                               rd=[r_km[b], r_k], wr=[r_kMd])
                        for n in range(4):
                            cs = slice(n * 128, (n + 1) * 128)
                            obank = []
                            for hg in range(2):
                                pk = pc % 6
                                pc += 1
                                for h4 in range(4):
                                    h = hg * 4 + h4
                                    op("pe", lambda e, pk=pk, h4=h4, h=h, b=b, cs=cs: e.matmul(psb[pk][:, h4 * 128:(h4 + 1) * 128], lhsT=kT[b][:, h, cs], rhs=qT[b][:, h, cs], start=True, stop=True),
                                       rd=[r_kt[b], r_q[b]], wr=[psr[pk]])
                                sbk = (2 * n + hg) % 2
                                op("dve", lambda e, pk=pk, sbk=sbk, hg=hg: e.tensor_tensor(out=sT[sbk][:].rearrange("p (h i) -> p h i", h=4), in0=psb[pk][:, 0:512].rearrange("p (h i) -> p h i", h=4), in1=itT[:, hg * 4:(hg + 1) * 4, :], op=ALU.mult),
                                   rd=[psr[pk], r_k], wr=[r_sT[sbk]])
                                po = pc % 6
                                pc += 1
                                obank.append(po)
                                for h4 in range(4):
                                    h = hg * 4 + h4
                                    op("pe", lambda e, po=po, h4=h4, h=h, b=b, n=n, sbk=sbk: e.matmul(psb[po][:, h4 * 128:(h4 + 1) * 128], lhsT=sT[sbk][:, h4 * 128:(h4 + 1) * 128], rhs=vM[b][:, n, h * 128:(h + 1) * 128], start=True, stop=False),
                                       rd=[r_sT[sbk], r_vm[b]], wr=[psr[po]])
                                    op("pe", lambda e, po=po, h4=h4, h=h, cs=cs: e.matmul(psb[po][:, h4 * 128:(h4 + 1) * 128], lhsT=qTd[:, h, cs], rhs=Sb[:, h, :], start=False, stop=True),
                                       rd=[r_qTd, r_Sb], wr=[psr[po]])
                            for hg in range(2):
                                pk = pc % 6
                                pc += 1
                                for h4 in range(4):
                                    h = hg * 4 + h4
                                    op("pe", lambda e, pk=pk, h4=h4, h=h, b=b, n=n: e.matmul(psb[pk][:, h4 * 128:(h4 + 1) * 128], lhsT=kMd[:, n, h * 128:(h + 1) * 128], rhs=vM[b][:, n, h * 128:(h + 1) * 128], start=True, stop=True),
                                       rd=[r_kMd, r_vm[b]], wr=[psr[pk]])
                                for h4 in range(4):
                                    h = hg * 4 + h4
                                    cd = float(np.exp(np.float32(128.0) * np.log1p(-np.exp2(np.float32(-5.0 - h)))))
                                    op("dve", lambda e, pk=pk, h4=h4, h=h, cd=cd: e.scalar_tensor_tensor(out=Sf[:, h, :], in0=Sf[:, h, :], scalar=cd, in1=psb[pk][:, h4 * 128:(h4 + 1) * 128], op0=ALU.mult, op1=ALU.add),
                                       rd=[psr[pk], r_Sf], wr=[r_Sf])
                            op("act", lambda e: e.activation(out=Sb[:], in_=Sf[:], func=AF.Copy), rd=[r_Sf], wr=[r_Sb])
                            while rq:
                                rq.pop(0)()
                            for hg in range(2):
                                po = obank[hg]
                                for h4 in range(4):
                                    h = hg * 4 + h4
                                    op("dve", lambda e, po=po, h4=h4, h=h: e.bn_stats(out=bst[:, h, :], in_=psb[po][:, h4 * 128:(h4 + 1) * 128]), rd=[psr[po]], wr=[r_bst])
                                    op("dve", lambda e, h=h: e.bn_aggr(out=mv[:, h, :], in_=bst[:, h, :]), rd=[r_bst], wr=[r_mv])
                            op("act", lambda e: e.activation(out=rs[:], in_=mv[:, :, 1], func=AF.Ln, bias=EPS), rd=[r_mv], wr=[r_rs])
                            op("act", lambda e: e.activation(out=rs[:], in_=rs[:], func=AF.Exp, scale=-0.5), rd=[r_rs], wr=[r_rs])
                            op("dve", lambda e: e.scalar_tensor_tensor(out=nbias[:], in0=mv[:, :, 0], scalar=-1.0, in1=rs[:], op0=ALU.mult, op1=ALU.mult), rd=[r_mv, r_rs], wr=[r_nb])
                            for hg in range(2):
                                po = obank[hg]
                                for h4 in range(4):
                                    h = hg * 4 + h4
                                    op("act", lambda e, po=po, h4=h4, h=h: e.activation(out=yn[:, h * 128:(h + 1) * 128], in_=psb[po][:, h4 * 128:(h4 + 1) * 128], func=AF.Identity, scale=rs[:, h:h + 1], bias=nbias[:, h:h + 1]),
                                       rd=[psr[po], r_nb, r_rs], wr=[r_yn])
                            yi = (tb * 4 + n) % 2
                            op("pool", lambda e, b=b, n=n, yi=yi: e.tensor_tensor(out=ybm[yi][:], in0=yn[:], in1=gM[b][:, n, :], op=ALU.mult), rd=[r_yn, r_gm[b]], wr=[r_ybm[yi]])

                            def _tr(yi=yi, b=b, cs=cs, n=n, tb=tb):
                                ptk = 6 + yi
                                ptv = psb[ptk][:].bitcast(BF16).rearrange("p (h t) -> p h t", h=8)
                                for h in range(8):
                                    op("pe", lambda e, h=h: e.transpose(out=ptv[:, h, :], in_=ybm[yi][:, h * 128:(h + 1) * 128], identity=ident_b[:]), rd=[r_ybm[yi]], wr=[psr[ptk]])
                                op("act", lambda e: e.activation(out=ybt[b][:, :, cs], in_=ptv, func=AF.Copy), rd=[psr[ptk]], wr=[r_ybt[b]])
                                if n == 3:
                                    t0 = tb * 512
                                    dma("pool", yT[1, :, :, t0:t0 + 512].rearrange("h d s -> d h s"), ybt[b][:], rd=[r_ybt[b]])
                            rq.append(_tr)
                    while rq:
                        rq.pop(0)()
                    tk.barrier()

                if stop == 4:
                    tk.barrier()
                    return nc
                with ExitStack() as s5:
                    qh = [sb(s5, "qh%d" % i, [128, S], BF16) for i in range(2)]
                    kh = [sb(s5, "kh%d" % i, [128, S], BF16) for i in range(2)]
                    vh = [sb(s5, "vh%d" % i, [128, NT, 132], BF16) for i in range(2)]
                    gh = [sb(s5, "gh%d" % i, [128, NT, 128], BF16) for i in range(2)]
                    r_qh = [Res(), Res()]
                    r_kh = [Res(), Res()]
                    r_vh = [Res(), Res()]
                    r_gh = [Res(), Res()]
                    NE = 4
                    Eb = [[sb(s5, "E%d_%d" % (c, i), [128, 512], BF16) for i in range(NE)] for c in range(2)]
                    r_E = [[Res() for _ in range(NE)] for _ in range(2)]
                    accs = [sb(s5, "accs%d" % i, [128, 3, 512], F32) for i in range(2)]
                    r_accs = [Res(), Res()]
                    ogb = [sb(s5, "ogb%d" % i, [128, 4, 128], F32) for i in range(2)]
                    r_ogb = [Res(), Res()]
                    smb = [sb(s5, "smb%d" % i, [128, 16], F32) for i in range(2)]
                    r_smb = [Res(), Res()]
                    o1 = sb(s5, "o1", [128, 128], F32)
                    o2 = sb(s5, "o2", [128, 128], F32)
                    jk = sb(s5, "jk", [128, 128], F32)
                    r_o1, r_o2, r_jk = Res(), Res(), Res()
                    ycm = [sb(s5, "ycm%d" % i, [128, 4, 128], BF16) for i in range(2)]
                    r_ycm = [Res(), Res()]
                    yct = [sb(s5, "yct%d" % i, [128, 512], BF16) for i in range(2)]
                    r_yct = [Res(), Res()]
                    for i in range(2):
                        op("pool", lambda e, i=i: e.memset(vh[i][:, :, 128:129], 1.0), wr=[r_vh[i]])

                    def da_load(h):
                        b = h % 2
                        dma("sp", qh[b][:], QdT[h], wr=[r_qh[b]])
                        dma("sp", kh[b][:], KdT[h], wr=[r_kh[b]])
                        dma("sp", vh[b][:, :, 0:128], Vd[:, h * 128:(h + 1) * 128].rearrange("(n p) e -> p n e", p=128), wr=[r_vh[b]])
                        dma("sp", gh[b][:], Gd[:, h * 128:(h + 1) * 128].rearrange("(n p) e -> p n e", p=128), wr=[r_gh[b]])

                    def acc_ap(c, qs):
                        if qs < 3:
                            return c, psb[c][:, qs * 129:qs * 129 + 129]
                        return 2, psb[2][:, c * 129:c * 129 + 129]

                    def acc_sb(ab, c, qs):
                        if qs < 3:
                            return accs[ab][:, c, qs * 129:qs * 129 + 129]
                        return accs[ab][:, 2, c * 129:c * 129 + 129]

                    pending = []
                    lg = lru_gen(s5)

                    def defer(n, fn):
                        pending.append([n, fn])

                    def tick():
                        for it in pending:
                            it[0] -= 1
                        while pending and pending[0][0] <= 0:
                            pending.pop(0)[1]()
                        next(lg, None)

                    def flush():
                        while pending:
                            pending.pop(0)[1]()

                    LA = 2
                    da_load(0)
                    da_load(1)
                    st_ = {"sc": 0, "ec": [0, 0], "blk": 0}
                    for h in range(8):
                        b = h % 2
                        for qb in range(NB):
                            if qb == min(1, NB - 1) and h >= 1 and h + 1 < 8:
                                da_load(h + 1)
                            nkt = 4 * qb + 4
                            started = [False, False, False]
                            info = {}

                            def qk(kt):
                                r = kt - 4 * qb
                                c0 = r * 128 if r > 0 else 0
                                pks = []
                                for c in range(2):
                                    pk = 3 + st_["sc"] % 4
                                    st_["sc"] += 1
                                    pks.append(pk)
                                    ps_ = slice(c * 64, (c + 1) * 64)
                                    op("pe", lambda e, pk=pk, ps_=ps_: e.matmul(psb[pk][:, c0:512], lhsT=kh[b][ps_, kt * 128:(kt + 1) * 128], rhs=qh[b][ps_, qb * 512 + c0:(qb + 1) * 512], start=True, stop=True),
                                       rd=[r_kh[b], r_qh[b]], wr=[psr[pk]])
                                for c in range(2):
                                    pk = pks[c]
                                    ei = st_["ec"][c] % NE
                                    st_["ec"][c] += 1
                                    Et = Eb[c][ei]
                                    rE = r_E[c][ei]
                                    op("act", lambda e, pk=pk, Et=Et: e.activation(out=Et[:, c0:512], in_=psb[pk][:, c0:512], func=AF.Exp, scale=0.125), rd=[psr[pk]], wr=[rE])
                                    if r >= 0:
                                        op("pool", lambda e, Et=Et: e.tensor_tensor(out=Et[:, c0:c0 + 128], in0=Et[:, c0:c0 + 128], in1=tri_b[:], op=ALU.mult), rd=[rE], wr=[rE])
                                    info[(kt, c)] = (Et, rE)

                            def pv(kt):
                                r = kt - 4 * qb
                                for c in range(2):
                                    Et, rE = info[(kt, c)]
                                    for qs in range(max(r, 0), 4):
                                        bk, aap = acc_ap(c, qs)
                                        first = not started[bk]
                                        started[bk] = True
                                        last = (kt == 4 * qb + qs)
                                        op("pe", lambda e, aap=aap, qs=qs, first=first, last=last, Et=Et: e.matmul(aap, lhsT=Et[:, qs * 128:(qs + 1) * 128], rhs=vh[b][:, kt, 0:129], start=first, stop=last, skip_group_check=True),
                                           rd=[rE, r_vh[b]], wr=[psr[bk]])

                            for i in range(nkt + LA):
                                if i < nkt:
                                    qk(i)
                                if i - LA >= 0:
                                    pv(i - LA)
                                tick()
                                tick()
                            ab = st_["blk"] % 2
                            st_["blk"] += 1
                            for bk, ncol in ((0, 387), (1, 387), (2, 258)):
                                op("dve", lambda e, bk=bk, ncol=ncol: e.tensor_copy(out=accs[ab][:, bk, 0:ncol], in_=psb[bk][:, 0:ncol]), rd=[psr[bk]], wr=[r_accs[ab]])
                            sview0 = accs[ab][:, 0, 0:387].rearrange("p (q e) -> p q e", e=129)[:, :, 128]
                            sview1 = accs[ab][:, 1, 0:387].rearrange("p (q e) -> p q e", e=129)[:, :, 128]
                            op("dve", lambda e: e.reciprocal(out=smb[ab][:, 0:3], in_=sview0), rd=[r_accs[ab]], wr=[r_smb[ab]])
                            op("dve", lambda e: e.reciprocal(out=smb[ab][:, 3:4], in_=accs[ab][:, 2, 128:129]), rd=[r_accs[ab]], wr=[r_smb[ab]])
                            op("dve", lambda e: e.reciprocal(out=smb[ab][:, 4:7], in_=sview1), rd=[r_accs[ab]], wr=[r_smb[ab]])
                            op("dve", lambda e: e.reciprocal(out=smb[ab][:, 7:8], in_=accs[ab][:, 2, 257:258]), rd=[r_accs[ab]], wr=[r_smb[ab]])
                            op("dve", lambda e: e.tensor_scalar(out=smb[ab][:, 4:8], in0=smb[ab][:, 4:8], scalar1=neglam[:, 0:1], scalar2=None, op0=ALU.mult), rd=[r_smb[ab]], wr=[r_smb[ab]])
                            for qs in range(4):
                                a0 = acc_sb(ab, 0, qs)
                                a1 = acc_sb(ab, 1, qs)
                                tile_i = qb * 4 + qs
                                op("dve", lambda e, a0=a0, qs=qs: e.tensor_scalar(out=o1[:], in0=a0[:, 0:128], scalar1=smb[ab][:, qs:qs + 1], scalar2=None, op0=ALU.mult), rd=[r_accs[ab], r_smb[ab]], wr=[r_o1])
                                op("dve", lambda e, a1=a1, qs=qs: e.scalar_tensor_tensor(out=o2[:], in0=a1[:, 0:128], scalar=smb[ab][:, 4 + qs:5 + qs], in1=o1[:], op0=ALU.mult, op1=ALU.add), rd=[r_accs[ab], r_smb[ab], r_o1], wr=[r_o2])
                                op("dve", lambda e, qs=qs: e.scalar_tensor_tensor(out=jk[:], in0=o2[:], scalar=1.0, in1=o2[:], op0=ALU.mult, op1=ALU.mult, accum_out=smb[ab][:, 8 + qs:9 + qs]), rd=[r_o2], wr=[r_jk, r_smb[ab]])
                                op("dve", lambda e, qs=qs, tile_i=tile_i: e.tensor_tensor(out=ogb[ab][:, qs, :], in0=o2[:], in1=gh[b][:, tile_i, :], op=ALU.mult), rd=[r_o2, r_gh[b]], wr=[r_ogb[ab]])
                                op("dve", lambda e, qs=qs: e.tensor_tensor(out=ogb[ab][:, qs, :], in0=ogb[ab][:, qs, :], in1=subgs[:], op=ALU.mult), rd=[r_ogb[ab]], wr=[r_ogb[ab]])

                            def stB(ab=ab):
                                op("act", lambda e: e.activation(out=smb[ab][:, 12:16], in_=smb[ab][:, 8:12], func=AF.Ln, scale=1.0 / 128, bias=EPS), rd=[r_smb[ab]], wr=[r_smb[ab]])
                                op("act", lambda e: e.activation(out=smb[ab][:, 12:16], in_=smb[ab][:, 12:16], func=AF.Exp, scale=-0.5), rd=[r_smb[ab]], wr=[r_smb[ab]])

                            def stC(ab=ab):
                                for qs in range(4):
                                    op("dve", lambda e, qs=qs: e.tensor_scalar(out=ycm[ab][:, qs, :], in0=ogb[ab][:, qs, :], scalar1=smb[ab][:, 12 + qs:13 + qs], scalar2=None, op0=ALU.mult), rd=[r_ogb[ab], r_smb[ab]], wr=[r_ycm[ab]])

                            def stT(ab=ab, h=h, qb=qb):
                                ptv = psb[7][:].bitcast(BF16)
                                for qs in range(4):
                                    op("pe", lambda e, qs=qs: e.transpose(out=ptv[:, qs * 128:(qs + 1) * 128], in_=ycm[ab][:, qs, :], identity=ident_b[:]), rd=[r_ycm[ab]], wr=[psr[7]])
                                op("dve", lambda e: e.tensor_copy(out=yct[ab][:], in_=ptv[:, 0:512]), rd=[psr[7]], wr=[r_yct[ab]])
                                dma("pool", yT[2, h, :, qb * 512:(qb + 1) * 512], yct[ab][:], rd=[r_yct[ab]])

                            defer(4, stB)
                            defer(5, stC)
                            defer(7, stT)
                    flush()
                    for _ in lg:
                        pass
                    tk.barrier()

                if stop == 5:
                    tk.barrier()
                    return nc
                with ExitStack() as s6:
                    WB = sb(s6, "WB", [128, 3, 8, D], BF16)
                    r_WB = {(br_, kc_): Res() for br_ in range(3) for kc_ in range(8)}
                    wsf = [sb(s6, "wsf%d" % i, [128, D], F32) for i in range(2)]
                    r_wsf = [Res(), Res()]
                    i_ = 0
                    for br in range(3):
                        for kc in range(8):
                            b = i_ % 2
                            dma("sp", wsf[b][:], w_br[l, br, kc * 128:(kc + 1) * 128, :], wr=[r_wsf[b]])
                            if i_ % 2 == 0:
                                op("dve", lambda e, b=b, br=br, kc=kc: e.tensor_copy(out=WB[:, br, kc, :], in_=wsf[b][:]), rd=[r_wsf[b]], wr=[r_WB[(br, kc)]])
                            else:
                                op("act", lambda e, b=b, br=br, kc=kc: e.activation(out=WB[:, br, kc, :], in_=wsf[b][:], func=AF.Copy), rd=[r_wsf[b]], wr=[r_WB[(br, kc)]])
                            i_ += 1
                    yTb = [sb(s6, "yTb%d" % i, [128, 3, 8, 512], BF16) for i in range(2)]
                    r_yTb = [Res(), Res()]
                    gmb = [sb(s6, "gmb%d" % i, [128, 3, 512], BF16) for i in range(2)]
                    r_gmb = [Res(), Res()]
                    t0b = sb(s6, "t0b", [128, 512], F32)
                    t1b = sb(s6, "t1b", [128, 512], F32)
                    t2b = sb(s6, "t2b", [128, 512], F32)
                    r_t0, r_t1, r_t2 = Res(), Res(), Res()
                    mst = [sb(s6, "mst%d" % i, [128, 512], BF16) for i in range(2)]
                    r_mst = [Res(), Res()]

                    def y_load(tb):
                        b = tb % 2
                        t0 = tb * 512
                        for br in range(3):
                            dma("sp", yTb[b][:, br, :, :], yT[br, :, :, t0:t0 + 512].rearrange("c p s -> p c s"), wr=[r_yTb[b]])

                    def gm_load(tb, dc, gi):
                        t0 = tb * 512
                        b = gi % 2
                        dma("sp", gmb[b][:], gmT[:, :, t0:t0 + 512].rearrange("(br dc) p s -> dc p br s", br=3)[dc], wr=[r_gmb[b]])

                    y_load(0)
                    gi = 0
                    gm_load(0, 0, 0)
                    pc = 0
                    for tb in range(NB):
                        b = tb % 2
                        if tb + 1 < NB:
                            y_load(tb + 1)
                        for dc in range(16):
                            nxt = tb * 16 + dc + 1
                            if nxt < NB * 16:
                                gm_load(nxt // 16, nxt % 16, gi + 1)
                            gb = gi % 2
                            gi += 1
                            pks = []
                            for br in range(3):
                                pk = pc % 8
                                pc += 1
                                pks.append(pk)
                                for kc in range(8):
                                    op("pe", lambda e, pk=pk, br=br, kc=kc, dc=dc, b=b: e.matmul(psb[pk][:, 0:512], lhsT=WB[:, br, kc, dc * 128:(dc + 1) * 128], rhs=yTb[b][:, br, kc, :], start=(kc == 0), stop=(kc == 7)),
                                       rd=[r_WB[(br, kc)], r_yTb[b]], wr=[psr[pk]])
                            op("dve", lambda e, pk=pks[0], gb=gb: e.tensor_tensor(out=t0b[:], in0=psb[pk][:, 0:512], in1=gmb[gb][:, 0, :], op=ALU.mult), rd=[psr[pks[0]], r_gmb[gb]], wr=[r_t0])
                            op("dve", lambda e, pk=pks[1], gb=gb: e.tensor_tensor(out=t1b[:], in0=psb[pk][:, 0:512], in1=gmb[gb][:, 1, :], op=ALU.mult), rd=[psr[pks[1]], r_gmb[gb]], wr=[r_t1])
                            op("dve", lambda e, pk=pks[2], gb=gb: e.tensor_tensor(out=t2b[:], in0=psb[pk][:, 0:512], in1=gmb[gb][:, 2, :], op=ALU.mult), rd=[psr[pks[2]], r_gmb[gb]], wr=[r_t2])
                            op("pool", lambda e: e.tensor_tensor(out=t0b[:], in0=t0b[:], in1=t1b[:], op=ALU.add), rd=[r_t0, r_t1], wr=[r_t0])
                            mb = (tb * 16 + dc) % 2
                            op("pool", lambda e, mb=mb: e.tensor_tensor(out=mst[mb][:], in0=t0b[:], in1=t2b[:], op=ALU.add), rd=[r_t0, r_t2], wr=[r_mst[mb]])
                            dma("pool", mT[dc, :, tb * 512:(tb + 1) * 512], mst[mb][:], rd=[r_mst[mb]])
                    tk.barrier()

                if stop == 6:
                    tk.barrier()
                    return nc
                with ExitStack() as s7:
                    WO = sb(s7, "WO", [128, KC, D], BF16)
                    r_WO = [Res() for _ in range(KC)]
                    wsf = [sb(s7, "wsf%d" % i, [128, D], F32) for i in range(2)]
                    r_wsf = [Res(), Res()]
                    for kc in range(KC):
                        b = kc % 2
                        dma("sp", wsf[b][:], w_out[l, kc * 128:(kc + 1) * 128, :], wr=[r_wsf[b]])
                        if kc % 2 == 0:
                            op("dve", lambda e, b=b, kc=kc: e.tensor_copy(out=WO[:, kc, :], in_=wsf[b][:]), rd=[r_wsf[b]], wr=[r_WO[kc]])
                        else:
                            op("act", lambda e, b=b, kc=kc: e.activation(out=WO[:, kc, :], in_=wsf[b][:], func=AF.Copy), rd=[r_wsf[b]], wr=[r_WO[kc]])
                    pg = sb(s7, "pg", [128, D], F32)
                    r_pg = Res()
                    dma("sp", pg[:], postg[l], wr=[r_pg])
                    mtb = [sb(s7, "mtb%d" % i, [128, KC, 128], BF16) for i in range(2)]
                    xr = [sb(s7, "xr%d" % i, [128, D], F32) for i in range(2)]
                    r_mtb = [Res(), Res()]
                    r_xr = [Res(), Res()]
                    ot = [sb(s7, "ot%d" % i, [128, D], F32) for i in range(2)]
                    r_ot = [Res(), Res()]
                    jq = sb(s7, "jq", [128, 512], BF16)
                    r_jq = Res()
                    q4 = [sb(s7, "q4_%d" % i, [128, 8], F32) for i in range(2)]
                    r_q4 = [Res(), Res()]

                    def o_load(tt):
                        b = tt % 2
                        t0 = tt * 128
                        dma("sp", mtb[b][:], mT[:, :, t0:t0 + 128].rearrange("c p s -> p c s"), wr=[r_mtb[b]])
                        dma("sp", xr[b][:], x_src[t0:t0 + 128, :], wr=[r_xr[b]])

                    o_load(0)
                    for tt in range(NT):
                        b = tt % 2
                        if tt + 1 < NT:
                            o_load(tt + 1)
                        base = 4 * (tt % 2)
                        for eb in range(4):
                            pk = base + eb
                            for dc in range(KC):
                                op("pe", lambda e, pk=pk, dc=dc, eb=eb, b=b: e.matmul(psb[pk][:, 0:512], lhsT=mtb[b][:, dc, :], rhs=WO[:, dc, eb * 512:(eb + 1) * 512], start=(dc == 0), stop=(dc == KC - 1)),
                                   rd=[r_mtb[b], r_WO[dc]], wr=[psr[pk]])
                            op("act", lambda e, pk=pk, eb=eb, b=b: e.activation(out=jq[:], in_=psb[pk][:, 0:512], func=AF.Square, accum_out=q4[b][:, eb:eb + 1]), rd=[psr[pk]], wr=[r_jq, r_q4[b]])
                        op("dve", lambda e, b=b: e.tensor_reduce(out=q4[b][:, 4:5], in_=q4[b][:, 0:4], axis=AX.X, op=ALU.add), rd=[r_q4[b]], wr=[r_q4[b]])
                        op("act", lambda e, b=b: e.activation(out=q4[b][:, 5:6], in_=q4[b][:, 4:5], func=AF.Sqrt, scale=1.0 / D, bias=EPS), rd=[r_q4[b]], wr=[r_q4[b]])
                        op("dve", lambda e, b=b: e.reciprocal(out=q4[b][:, 6:7], in_=q4[b][:, 5:6]), rd=[r_q4[b]], wr=[r_q4[b]])
                        for eb in range(4):
                            pk = base + eb
                            es_ = slice(eb * 512, (eb + 1) * 512)
                            op("dve", lambda e, pk=pk, es_=es_, b=b: e.scalar_tensor_tensor(out=ot[b][:, es_], in0=psb[pk][:, 0:512], scalar=q4[b][:, 6:7], in1=pg[:, es_], op0=ALU.mult, op1=ALU.mult),
                               rd=[psr[pk], r_q4[b], r_pg], wr=[r_ot[b]])
                        op("pool", lambda e, b=b: e.tensor_tensor(out=ot[b][:], in0=ot[b][:], in1=xr[b][:], op=ALU.add), rd=[r_ot[b], r_xr[b]], wr=[r_ot[b]])
                        dma("pool", x_dst[tt * 128:(tt + 1) * 128, :], ot[b][:], rd=[r_ot[b]])
                    tk.barrier()
        tk.barrier()
    return nc


def _const_tables(S):
    f32 = np.float32
    pos = np.arange(S, dtype=f32)
    ret_freq = (1.0 / (f32(10000.0) ** np.linspace(0.0, 1.0, 64, dtype=f32))).astype(f32)
    ang = (pos[:, None] * ret_freq[None, :]).astype(f32)
    c, s = np.cos(ang).astype(f32), np.sin(ang).astype(f32)
    Cq = np.repeat(c, 2, axis=1)
    Sq = np.stack([-s, s], axis=-1).reshape(S, 128)
    ks = f32(128.0 ** -0.5)
    ropeR = np.tile(np.stack([Cq, Sq, Cq * ks, Sq * ks]).astype(f32), (1, 1, 2))
    inv = (f32(500000.0) ** (-np.arange(0, 16, 2, dtype=f32) / f32(16))).astype(f32)
    angd = (pos[:, None] * inv[None, :]).astype(f32)
    cd, sd = np.cos(angd).astype(f32), np.sin(angd).astype(f32)
    Cf = np.ones((S, 256), f32)
    Sa = np.zeros((S, 256), f32)
    Sb = np.zeros((S, 256), f32)
    for blk in range(4):
        o = blk * 64
        Cf[:, o:o + 8] = cd
        Cf[:, o + 8:o + 16] = cd
        Sa[:, o:o + 8] = -sd
        Sb[:, o + 8:o + 16] = sd
    ropeD = np.stack([Cf, Sa, Sb]).astype(f32)
    H = 8
    log_g = np.log1p(-np.exp2(-5.0 - np.arange(H, dtype=f32))).astype(f32)
    idx = np.arange(128, dtype=f32)
    rel = idx[:, None] - idx[None, :]
    intra = np.where(rel[None] >= 0, np.exp(log_g[:, None, None] * np.maximum(rel, 0.0)[None]), 0.0).astype(f32)
    intraT = np.ascontiguousarray(intra.transpose(2, 0, 1))
    kdec = np.ascontiguousarray(np.exp(log_g[:, None] * (127.0 - idx)[None, :]).astype(f32).T)
    qd = np.exp(log_g[:, None] * (idx + 1.0)[None, :]).astype(f32)
    qdec = np.ascontiguousarray(np.broadcast_to(np.tile(qd, (1, 4))[None], (128, 8, 512))).astype(f32)
    ident = np.eye(128, dtype=f32)
    tri = (idx[None, :] >= idx[:, None]).astype(f32)
    return dict(ropeR=ropeR, ropeD=ropeD, intraT=intraT, kdec=kdec, qdec=qdec, ident=ident, tri=tri)


def _prep_weights(inp, depth):
    f32 = np.float32
    cols = np.zeros((depth, 128, 80), f32)
    lruw = np.zeros((depth, 2, 8, 128, 128), f32)
    for l in range(depth):
        cols[l, :, 0:16] = inp["pre_norm"][l].reshape(16, 128).T
        cols[l, :, 16:48] = inp["conv_w"][l].reshape(4, 8, 128).transpose(2, 0, 1).reshape(128, 32)
        cols[l, :, 48:56] = inp["conv_b"][l].reshape(8, 128).T
        cols[l, :, 56:64] = inp["lru_ba"][l].reshape(8, 128).T
        cols[l, :, 64:72] = inp["lru_bx"][l].reshape(8, 128).T
        cols[l, :, 72:80] = inp["lru_lambda"][l].reshape(8, 128).T
        for wi, name in enumerate(("lru_wa", "lru_wx")):
            w = inp[name][l]
            for cc in range(8):
                for j in range(2):
                    lruw[l, wi, cc, j * 64:(j + 1) * 64, j * 64:(j + 1) * 64] = w[cc * 2 + j]
    postg = np.ascontiguousarray(np.broadcast_to(inp["post_norm"][:depth, None, :], (depth, 128, D))).astype(f32)
    subg = np.ascontiguousarray(np.broadcast_to(inp["diff_subln"][:depth, None, :], (depth, 128, 128))).astype(f32)
    dlam = np.ascontiguousarray(np.broadcast_to(inp["diff_lambda"][:depth].reshape(depth, 1, 256), (depth, 128, 256))).astype(f32)
    w_br = np.ascontiguousarray(np.stack([inp["w_branch_a"][:depth], inp["w_branch_b"][:depth], inp["w_branch_c"][:depth]], axis=1)).astype(f32)
    return dict(cols=cols, lruw=lruw, postg=postg, subg=subg, dlam=dlam, w_br=w_br,
                w_in=np.ascontiguousarray(inp["w_in"][:depth]), w_out=np.ascontiguousarray(inp["w_out"][:depth]))


def kernel(**inputs):
    x = np.asarray(inputs["x"], dtype=np.float32)
    B, S, _ = x.shape
    depth = inputs["w_in"].shape[0]
    nc = build(S, depth)
    shared = _prep_weights({k: np.asarray(v) for k, v in inputs.items()}, depth)
    shared.update(_const_tables(S))
    in_maps = []
    for b in range(B):
        m = dict(shared)
        m["x"] = np.ascontiguousarray(x[b])
        in_maps.append(m)
    res = run_bass_kernel_spmd(nc, in_maps, core_ids=list(range(B)))
    return np.stack([np.asarray(r["y"], dtype=np.float32) for r in res.results], axis=0)
```
